# Optimizing a Trainium2 kernel written in Bass

```python
import math
import jax, jax.numpy as jnp
from jax import lax
import numpy as np

D_MODEL = 1024
BATCH = 8
SEQ = 4096
DEPTH = 4

CHUNK = 64
QBLOCK = 64
MIX_W = D_MODEL
N_HEADS = 8
HEAD_DIM = 64
ATTN_W = N_HEADS * HEAD_DIM
CONV_CH = MIX_W - ATTN_W
CONV_K = 31
IDX_HEADS = 8
IDX_DIM = 64
TOPK_MAX = 256
NUM_BUCKETS = 32
MAX_DISTANCE = 128
D_FF = 2816
N_EXPERTS = 8
TOP_K = 2
D_FF_EXPERT = 3584
N_DENSE = (DEPTH + 1) // 2
N_MOE = DEPTH // 2
OFF_Q = 0
OFF_K = OFF_Q + ATTN_W
OFF_V = OFF_K + ATTN_W
OFF_QI = OFF_V + ATTN_W
OFF_KI = OFF_QI + IDX_HEADS * IDX_DIM
OFF_WI = OFF_KI + IDX_DIM
OFF_GLU = OFF_WI + IDX_HEADS
IN_COLS = OFF_GLU + 2 * CONV_CH
DN_ALPHA = (2.0 * DEPTH) ** 0.25
DN_BETA = (8.0 * DEPTH) ** -0.25
LN_EPS = 1e-5
NEG = -1e30

kernel_name = "hymba_dsa_conformer_moe_deepnorm"


def layer_norm(x, g, b):
    xf = x.astype(jnp.float32)
    mu = jnp.mean(xf, axis=-1, keepdims=True)
    var = jnp.mean(jnp.square(xf - mu), axis=-1, keepdims=True)
    y = (xf - mu) * lax.rsqrt(var + LN_EPS)
    return (y * g.astype(jnp.float32) + b.astype(jnp.float32)).astype(x.dtype)


def t5_bucket(rel):
    nb = NUM_BUCKETS // 2
    max_exact = nb // 2
    ret = jnp.where(rel > 0, nb, 0).astype(jnp.int32)
    n = jnp.abs(rel)
    nf = jnp.maximum(n, 1).astype(jnp.float32)
    large = max_exact + (jnp.log(nf / max_exact) / math.log(MAX_DISTANCE / max_exact)
                         * (nb - max_exact)).astype(jnp.int32)
    large = jnp.minimum(large, nb - 1)
    return ret + jnp.where(n < max_exact, n, large)


def sparse_attention(q, k, v, q_idx, k_idx, w_idx, rel_bias):
    B, S = q.shape[0], q.shape[1]
    nb = S // QBLOCK
    top_k = min(TOPK_MAX, S // 4)
    key_chunk = jnp.arange(S, dtype=jnp.int32) // CHUNK
    k_flat = k.reshape(B, S, ATTN_W)
    v_flat = v.reshape(B, S, ATTN_W)
    gather = jax.vmap(lambda a, i: a[i])

    def to_blocks(a):
        return jnp.moveaxis(a.reshape((B, nb, QBLOCK) + a.shape[2:]), 1, 0)

    def block_fn(args):
        blk_id, qb, qib, wb = args
        t = blk_id * QBLOCK + jnp.arange(QBLOCK, dtype=jnp.int32)
        t_chunk = t // CHUNK
        dots = jnp.einsum('bthd,bsd->bths', qib, k_idx).astype(jnp.float32) * (IDX_DIM ** -0.5)
        wf = wb.astype(jnp.float32) * (IDX_HEADS ** -0.5)
        score = jnp.einsum('bths,bth->bts', jax.nn.relu(dots), wf)
        admissible = key_chunk[None, :] <= t_chunk[:, None]
        score = jnp.where(admissible[None], score, NEG)
        _, idx = lax.top_k(score, top_k)
        valid = (idx // CHUNK) <= t_chunk[None, :, None]
        flat = idx.reshape(B, QBLOCK * top_k)
        kg = gather(k_flat, flat).reshape(B, QBLOCK, top_k, N_HEADS, HEAD_DIM)
        vg = gather(v_flat, flat).reshape(B, QBLOCK, top_k, N_HEADS, HEAD_DIM)
        logits = jnp.einsum('bthd,btkhd->bthk', qb, kg).astype(jnp.float32) * (HEAD_DIM ** -0.5)
        bucket = t5_bucket(idx - t[None, :, None])
        bias = jnp.moveaxis(rel_bias[bucket], -1, 2)
        logits = logits + bias.astype(jnp.float32)
        logits = jnp.where(valid[:, :, None, :], logits, NEG)
        p = jax.nn.softmax(logits, axis=-1).astype(v.dtype)
        return jnp.einsum('bthk,btkhd->bthd', p, vg)

    out = lax.map(block_fn, (jnp.arange(nb, dtype=jnp.int32), to_blocks(q),
                             to_blocks(q_idx), to_blocks(w_idx)))
    return jnp.moveaxis(out, 0, 1).reshape(B, S, ATTN_W)


def conformer_conv(glu_in, conv_w, conv_b, ln_g, ln_b):
    a, g = glu_in[..., :CONV_CH], glu_in[..., CONV_CH:]
    u = a * jax.nn.sigmoid(g)
    y = lax.conv_general_dilated(u, conv_w[:, None, :], window_strides=(1,),
                                 padding=[(CONV_K - 1, 0)],
                                 dimension_numbers=('NWC', 'WIO', 'NWC'),
                                 feature_group_count=CONV_CH) + conv_b
    y = layer_norm(y, ln_g, ln_b)
    return jax.nn.silu(y)


def swiglu(x, w_gate, w_up, w_down):
    return (jax.nn.silu(x @ w_gate) * (x @ w_up)) @ w_down


def moe_swiglu(x, router, w_gate, w_up, w_down):
    logits = (x @ router).astype(jnp.float32)
    top_vals, top_idx = lax.top_k(logits, TOP_K)
    sel = jax.nn.softmax(top_vals, axis=-1)
    gates = jnp.sum(jax.nn.one_hot(top_idx, N_EXPERTS, dtype=jnp.float32) * sel[..., None], axis=-2)
    gates = gates.astype(x.dtype)
    out = jnp.zeros_like(x)
    for e in range(N_EXPERTS):
        out = out + gates[..., e:e + 1] * swiglu(x, w_gate[e], w_up[e], w_down[e])
    return out


def setup_inputs(seed: int = 0) -> dict:
    key = jax.random.key(seed)
    ks = jax.random.split(key, 24)
    f32 = jnp.float32

    def nrm(k, shape, scale):
        return jax.random.normal(k, shape, f32) * scale

    def gain(k, shape):
        return 1.0 + 0.05 * jax.random.normal(k, shape, f32)

    return {
        "x": nrm(ks[0], (BATCH, SEQ, D_MODEL), 1.0),
        "w_in": nrm(ks[1], (DEPTH, D_MODEL, IN_COLS), D_MODEL ** -0.5),
        "conv_w": nrm(ks[2], (DEPTH, CONV_K, CONV_CH), CONV_K ** -0.5),
        "conv_b": nrm(ks[3], (DEPTH, CONV_CH), 0.02),
        "conv_ln_g": gain(ks[4], (DEPTH, CONV_CH)),
        "conv_ln_b": nrm(ks[5], (DEPTH, CONV_CH), 0.02),
        "mix_scale": gain(ks[6], (DEPTH, MIX_W)),
        "rel_bias": nrm(ks[7], (NUM_BUCKETS, N_HEADS), 0.5),
        "w_out": nrm(ks[8], (DEPTH, MIX_W, D_MODEL), DN_BETA * MIX_W ** -0.5),
        "ln1_g": gain(ks[9], (DEPTH, D_MODEL)),
        "ln1_b": nrm(ks[10], (DEPTH, D_MODEL), 0.02),
        "ln2_g": gain(ks[11], (DEPTH, D_MODEL)),
        "ln2_b": nrm(ks[12], (DEPTH, D_MODEL), 0.02),
        "ffn_w_gate": nrm(ks[13], (N_DENSE, D_MODEL, D_FF), D_MODEL ** -0.5),
        "ffn_w_up": nrm(ks[14], (N_DENSE, D_MODEL, D_FF), D_MODEL ** -0.5),
        "ffn_w_down": nrm(ks[15], (N_DENSE, D_FF, D_MODEL), DN_BETA * D_FF ** -0.5),
        "moe_router": nrm(ks[16], (N_MOE, D_MODEL, N_EXPERTS), D_MODEL ** -0.5),
        "moe_w_gate": nrm(ks[17], (N_MOE, N_EXPERTS, D_MODEL, D_FF_EXPERT), D_MODEL ** -0.5),
        "moe_w_up": nrm(ks[18], (N_MOE, N_EXPERTS, D_MODEL, D_FF_EXPERT), D_MODEL ** -0.5),
        "moe_w_down": nrm(ks[19], (N_MOE, N_EXPERTS, D_FF_EXPERT, D_MODEL), DN_BETA * D_FF_EXPERT ** -0.5),
    }


def reference(x, w_in, conv_w, conv_b, conv_ln_g, conv_ln_b, mix_scale, rel_bias, w_out,
              ln1_g, ln1_b, ln2_g, ln2_b, ffn_w_gate, ffn_w_up, ffn_w_down,
              moe_router, moe_w_gate, moe_w_up, moe_w_down):
    B, S = x.shape[0], x.shape[1]
    for l in range(DEPTH):
        h = x @ w_in[l]
        q = h[..., OFF_Q:OFF_K].reshape(B, S, N_HEADS, HEAD_DIM)
        k = h[..., OFF_K:OFF_V].reshape(B, S, N_HEADS, HEAD_DIM)
        v = h[..., OFF_V:OFF_QI].reshape(B, S, N_HEADS, HEAD_DIM)
        q_idx = h[..., OFF_QI:OFF_KI].reshape(B, S, IDX_HEADS, IDX_DIM)
        k_idx = h[..., OFF_KI:OFF_WI]
        w_idx = h[..., OFF_WI:OFF_GLU]
        glu_in = h[..., OFF_GLU:]
        attn = sparse_attention(q, k, v, q_idx, k_idx, w_idx, rel_bias)
        conv = conformer_conv(glu_in, conv_w[l], conv_b[l], conv_ln_g[l], conv_ln_b[l])
        mixed = jnp.concatenate([attn, conv], axis=-1) * mix_scale[l]
        x = layer_norm(DN_ALPHA * x + mixed @ w_out[l], ln1_g[l], ln1_b[l])
        if l % 2 == 0:
            f = swiglu(x, ffn_w_gate[l // 2], ffn_w_up[l // 2], ffn_w_down[l // 2])
        else:
            m = l // 2
            f = moe_swiglu(x, moe_router[m], moe_w_gate[m], moe_w_up[m], moe_w_down[m])
        x = layer_norm(DN_ALPHA * x + f, ln2_g[l], ln2_b[l])
    return x
```

```python
import math
from contextlib import ExitStack

import numpy as np
import concourse.bass as bass
import concourse.mybir as mybir
from concourse.bass_utils import run_bass_kernel_spmd

F32 = mybir.dt.float32
BF16 = mybir.dt.bfloat16
AF = mybir.ActivationFunctionType
ALU = mybir.AluOpType
AX = mybir.AxisListType

D = 1024
S = 4096
DEPTH = 4
NT = S // 128
TT = S // 512
IN_COLS = 3144
OFF_Q, OFF_K, OFF_V, OFF_QI, OFF_KI, OFF_WI, OFF_A, OFF_G = 0, 512, 1024, 1536, 2048, 2112, 2120, 2632
D_FF = 2816
D_FFE = 3584
NE = 8
CONV_K = 31
DN_ALPHA = (2.0 * DEPTH) ** 0.25
LN_EPS = 1e-5
NEGM = -30000.0
NEG = -1e30
N_BISECT = 20
NV = 56

EPOCH = 30000
DMA_RING = 8


class KB:
    def __init__(self, nc, es):
        self.nc = nc
        self.es = es
        self.eng = {"pe": nc.tensor, "act": nc.scalar, "dve": nc.vector, "pool": nc.gpsimd, "sp": nc.sync}
        self.count = {e: 0 for e in self.eng}
        self.dma_n = {}
        self.waited = {e: {} for e in self.eng}
        self.last_write = {}
        self.readers = {}
        self._semh = {}
        self.n_inst = 0

    def _sem(self, key):
        if key not in self._semh:
            name = "s_" + "_".join(str(k) for k in key)
            self._semh[key] = self.es.enter_context(self.nc.semaphore(name))
        return self._semh[key]

    def _wait(self, eng, token):
        key, val = token
        if self.waited[eng].get(key, 0) >= val:
            return
        self.waited[eng][key] = val
        self.eng[eng].wait_ge(self._sem(key), val)

    def _deps(self, eng, reads, writes):
        toks = []
        for b in reads:
            t = self.last_write.get(b)
            if t is not None:
                toks.append(t)
        for b in writes:
            t = self.last_write.get(b)
            if t is not None:
                toks.append(t)
            toks.extend(self.readers.get(b, {}).values())
        for t in toks:
            if eng == "pe" and t[0][0] == "c" and t[0][1] == "pe":
                continue
            self._wait(eng, t)

    def _record(self, token, reads, writes):
        for b in writes:
            self.last_write[b] = token
            self.readers[b] = {}
        for b in reads:
            self.readers.setdefault(b, {})[token[0]] = token

    def op(self, eng, ins_fn, reads=(), writes=()):
        self._deps(eng, reads, writes)
        ins = ins_fn()
        n = self.count[eng]
        self.count[eng] = n + 1
        key = ("c", eng, n // EPOCH)
        ins.then_inc(self._sem(key), 1)
        self._record((key, n % EPOCH + 1), reads, writes)
        self.n_inst += 1
        return ins

    def dma(self, q, out, in_, reads=(), writes=()):
        n = self.dma_n.get(q, 0)
        self.dma_n[q] = n + 1
        slot = n % DMA_RING
        key = ("d", q, slot)
        rnd = n // DMA_RING
        if rnd > 0:
            self._wait(q, (key, 16 * rnd))
        self._deps(q, reads, writes)
        ins = self.eng[q].dma_start(out=out, in_=in_)
        ins.then_inc(self._sem(key), 16)
        self._record((key, 16 * (rnd + 1)), reads, writes)
        self.n_inst += 1
        return ins

    def barrier(self):
        toks = []
        for e in self.eng:
            n = self.count[e]
            if n > 0:
                toks.append((("c", e, (n - 1) // EPOCH), (n - 1) % EPOCH + 1))
        for q, n in self.dma_n.items():
            for slot in range(min(n, DMA_RING)):
                toks.append((("d", q, slot), 16 * ((n - 1 - slot) // DMA_RING + 1)))
        for e in self.eng:
            for t in toks:
                self._wait(e, t)
        self.last_write = {}
        self.readers = {}


def bcast_mid(a, n):
    return bass.AP(a.tensor, a.offset, [list(a.ap[0]), [0, n], list(a.ap[1])])


def build_program(n_layers=DEPTH, debug=False, stop_after=None):
    nc = bass.Bass("TRN2", target_bir_lowering=False)
    dt_in = lambda name, shape: nc.dram_tensor(name, shape, F32, kind="ExternalInput").ap()
    xT_in = dt_in("xT", [D, S])
    w_in = dt_in("w_in", [DEPTH, D, IN_COLS])
    conv_wT = dt_in("conv_wT", [DEPTH, 512, CONV_K])
    vecs = dt_in("vecs", [128, DEPTH, NV])
    rel_bias = dt_in("rel_bias", [32, 8])
    ohb = dt_in("ohb", [128, 32, 2, 128])
    w_out = dt_in("w_out", [DEPTH, D, D])
    ffn_wg = dt_in("ffn_w_gate", [2, D, D_FF])
    ffn_wu = dt_in("ffn_w_up", [2, D, D_FF])
    ffn_wd = dt_in("ffn_w_down", [2, D_FF, D])
    moe_r = dt_in("moe_router", [2, D, NE])
    moe_wg = dt_in("moe_w_gate", [2, NE, D, D_FFE])
    moe_wu = dt_in("moe_w_up", [2, NE, D, D_FFE])
    moe_wd = dt_in("moe_w_down", [2, NE, D_FFE, D])
    outT = nc.dram_tensor("outT", [D, S], F32, kind="ExternalOutput").ap()

    skind = "ExternalOutput" if debug else "Internal"
    scr = lambda name, shape, dt: nc.dram_tensor(name, shape, dt, kind=skind).ap()
    xm = scr("xm", [D, S], F32)
    fsc = scr("fsc", [D, S], F32)
    hq = scr("hq", [512, S], BF16)
    hk = scr("hk", [512, S], BF16)
    hqi = scr("hqi", [512, S], BF16)
    hki = scr("hki", [64, S], BF16)
    hu = scr("hu", [512, S], BF16)
    hv = scr("hv", [S, 512], BF16)
    hw = scr("hw", [S, 8], F32)
    hma = scr("hma", [8, 64, S], BF16)
    hmc = scr("hmc", [512, S], BF16)
    hg = scr("hg", [8, S], F32)

    def cm(a):
        return a.rearrange("(c p) s -> p c s", p=128)

    with ExitStack() as es:
        kb = KB(nc, es)
        uid = [0]

        def SB(st, name, shape, dt):
            uid[0] += 1
            return st.enter_context(nc.sbuf_tensor(f"{name}_{uid[0]}", shape, dt))

        def PS(st, name, shape, dt):
            uid[0] += 1
            return st.enter_context(nc.psum_tensor(f"{name}_{uid[0]}", shape, dt))

        identb = SB(es, "identb", [128, 128], BF16)
        ident32 = SB(es, "ident32", [128, 128], F32)
        onesdiv = {}
        ones32 = SB(es, "ones32", [128, 64], F32)
        vec = SB(es, "vec", [128, DEPTH, NV], F32)
        rbb = SB(es, "rbb", [128, 256], F32)
        sel8 = SB(es, "sel8", [8, 8, 128], F32)
        pow2 = SB(es, "pow2", [128, N_BISECT + 1], F32)
        bias_hi = SB(es, "bias_hi", [128, 8, 2, 128], BF16)
        bias_lo = SB(es, "bias_lo", [128, 8, 2, 128], BF16)
        negc = SB(es, "negc", [128, 8], F32)

        kb.op("pool", lambda: nc.gpsimd.memset(ident32[:], 0.0), writes=["ident32"])
        kb.op("pool", lambda: nc.gpsimd.affine_select(out=ident32[:], in_=ident32[:], pattern=[[-1, 128]],
                                                      compare_op=ALU.not_equal, fill=1.0, base=0,
                                                      channel_multiplier=1), reads=["ident32"], writes=["ident32"])
        kb.op("dve", lambda: nc.vector.tensor_copy(identb[:], ident32[:]), reads=["ident32"], writes=["identb"])
        kb.op("pool", lambda: nc.gpsimd.memset(sel8[:], 0.0), writes=["sel8"])
        kb.op("pool", lambda: nc.gpsimd.affine_select(out=sel8[:], in_=sel8[:], pattern=[[-1, 8], [0, 128]],
                                                      compare_op=ALU.not_equal, fill=1.0, base=0,
                                                      channel_multiplier=1), reads=["sel8"], writes=["sel8"])
        kb.op("dve", lambda: nc.vector.memset(ones32[:], 1.0), writes=["ones32"])
        for j in range(N_BISECT + 1):
            kb.op("dve", lambda j=j: nc.vector.memset(pow2[:, j:j + 1], 2.0 ** -(j + 1)), writes=["pow2"])
        for nm_, val in (("od512", 1.0 / 512), ("od1024", 1.0 / 1024)):
            t_ = SB(es, nm_, [128, 128], BF16)
            kb.op("dve", lambda t_=t_, val=val: nc.vector.memset(t_[:], val), writes=[nm_])
            onesdiv[nm_] = t_
        kb.dma("sp", vec[:], vecs, writes=["vec"])
        rb_flat = rel_bias.rearrange("b h -> (b h)")
        kb.dma("sp", rbb[:], bass.AP(rb_flat.tensor, rb_flat.offset, [[0, 128], [1, 256]]), writes=["rbb"])
        with ExitStack() as st:
            oh = SB(st, "oh", [128, 32, 2, 128], F32)
            acc = SB(st, "bacc", [128, 8, 2, 128], F32)
            tmp = SB(st, "btmp", [128, 8, 2, 128], F32)
            kb.dma("sp", oh[:], ohb, writes=["oh"])
            kb.op("dve", lambda: nc.vector.tensor_scalar(out=negc[:], in0=rbb[:, 15 * 8:16 * 8], scalar1=-1.0,
                                                         scalar2=None, op0=ALU.mult), reads=["rbb"], writes=["negc"])
            kb.op("dve", lambda: nc.vector.memset(acc[:], 0.0), writes=["bacc"])
            for h in range(8):
                for b in range(32):
                    kb.op("dve", lambda h=h, b=b: nc.vector.scalar_tensor_tensor(
                        out=acc[:, h], in0=oh[:, b], scalar=rbb[:, b * 8 + h:b * 8 + h + 1], in1=acc[:, h],
                        op0=ALU.mult, op1=ALU.add), reads=["oh", "rbb", "bacc"], writes=["bacc"])
                kb.op("dve", lambda h=h: nc.vector.tensor_scalar(
                    out=acc[:, h], in0=acc[:, h], scalar1=negc[:, h:h + 1], scalar2=8.0, op0=ALU.add, op1=ALU.mult),
                    reads=["bacc", "negc"], writes=["bacc"])
            kb.op("dve", lambda: nc.vector.tensor_copy(bias_hi[:], acc[:]), reads=["bacc"], writes=["bias_hi"])
            kb.op("dve", lambda: nc.vector.tensor_tensor(out=tmp[:], in0=acc[:], in1=bias_hi[:], op=ALU.subtract),
                  reads=["bacc", "bias_hi"], writes=["btmp"])
            kb.op("dve", lambda: nc.vector.tensor_copy(bias_lo[:], tmp[:]), reads=["btmp"], writes=["bias_lo"])
            kb.barrier()

        with ExitStack() as st:
            xt = [SB(st, f"x0_{i}", [128, S], F32) for i in range(2)]
            for c in range(8):
                kb.dma("sp", xt[c % 2][:], cm(xT_in)[:, c, :], writes=[f"x0_{c % 2}"])
                kb.dma("act", cm(xm)[:, c, :], xt[c % 2][:], reads=[f"x0_{c % 2}"])
            kb.barrier()

        def phase_proj(l):
            with ExitStack() as st:
                xb = SB(st, "xb", [128, 8, S], BF16)
                wb = [SB(st, f"wb{i}", [128, 8, 512], BF16) for i in range(2)]
                wkw = SB(st, "wkw", [128, 8, 72], BF16)
                stg = [SB(st, f"stg{i}", [128, S], BF16) for i in range(2)]
                vst = [SB(st, f"vst{i}", [128, 4, 512], BF16) for i in range(2)]
                wst = SB(st, "wst", [128, NT, 8], F32)
                sg = [SB(st, f"sg{i}", [128, 512], F32) for i in range(2)]
                pa = [PS(st, f"pa{i}", [128, 512], F32) for i in range(3)]
                pg = [PS(st, f"pg{i}", [128, 512], F32) for i in range(2)]
                pw = PS(st, "pw", [128, 8], F32)
                for c in range(8):
                    kb.dma("pool", xb[:, c, :], cm(xm)[:, c, :], writes=[("xb", c)])
                xbk = [("xb", c) for c in range(8)]
                wv = cm(w_in[l])
                nslot = [0]

                def loadw(col0, ncol=512):
                    i = nslot[0] % 2
                    nslot[0] += 1
                    kb.dma("pool", wb[i][:, :, 0:ncol], wv[:, :, col0:col0 + ncol], writes=[f"wb{i}"])
                    return i

                nev = [0]

                def evac(dst, src, reads, writes):
                    if nev[0] % 2 == 0:
                        kb.op("act", lambda: nc.scalar.copy(dst, src), reads=reads, writes=writes)
                    else:
                        kb.op("dve", lambda: nc.vector.tensor_copy(dst, src), reads=reads, writes=writes)
                    nev[0] += 1

                nst = [0]
                npa = [0]
                for (col0, dst) in ((OFF_Q, hq), (OFF_K, hk), (OFF_QI, hqi)):
                    wi = loadw(col0)
                    for blk in range(4):
                        si = nst[0] % 2
                        nst[0] += 1
                        for tt in range(TT):
                            p = npa[0] % 3
                            npa[0] += 1
                            for c in range(8):
                                kb.op("pe", lambda c=c, p=p, wi=wi, blk=blk, tt=tt: nc.tensor.matmul(
                                    pa[p][:, :], wb[wi][:, c, blk * 128:(blk + 1) * 128], xb[:, c, tt * 512:(tt + 1) * 512],
                                    start=(c == 0), stop=(c == 7)), reads=[f"wb{wi}", ("xb", c)], writes=[f"pa{p}"])
                            evac(stg[si][:, tt * 512:(tt + 1) * 512], pa[p][:, :], [f"pa{p}"], [(f"stg{si}", tt)])
                        kb.dma("sp", dst[blk * 128:(blk + 1) * 128, :], stg[si][:],
                               reads=[(f"stg{si}", tt) for tt in range(TT)], writes=[])
                        for tt in range(TT):
                            kb.readers.setdefault((f"stg{si}", tt), {}).update(kb.readers.get((f"stg{si}", 0), {}))
                wa = loadw(OFF_A)
                wg_ = loadw(OFF_G)
                for blk in range(4):
                    si = nst[0] % 2
                    nst[0] += 1
                    for tt in range(TT):
                        p = npa[0] % 3
                        npa[0] += 1
                        q = tt % 2
                        for c in range(8):
                            kb.op("pe", lambda c=c, p=p, blk=blk, tt=tt: nc.tensor.matmul(
                                pa[p][:, :], wb[wa][:, c, blk * 128:(blk + 1) * 128], xb[:, c, tt * 512:(tt + 1) * 512],
                                start=(c == 0), stop=(c == 7)), reads=[f"wb{wa}", ("xb", c)], writes=[f"pa{p}"])
                        for c in range(8):
                            kb.op("pe", lambda c=c, q=q, blk=blk, tt=tt: nc.tensor.matmul(
                                pg[q][:, :], wb[wg_][:, c, blk * 128:(blk + 1) * 128], xb[:, c, tt * 512:(tt + 1) * 512],
                                start=(c == 0), stop=(c == 7)), reads=[f"wb{wg_}", ("xb", c)], writes=[f"pg{q}"])
                        kb.op("act", lambda q=q: nc.scalar.activation(out=sg[q][:], in_=pg[q][:, :], func=AF.Sigmoid),
                              reads=[f"pg{q}"], writes=[f"sg{q}"])
                        kb.op("dve", lambda q=q, p=p, si=si, tt=tt: nc.vector.tensor_tensor(
                            out=stg[si][:, tt * 512:(tt + 1) * 512], in0=pa[p][:, :], in1=sg[q][:], op=ALU.mult),
                            reads=[f"pa{p}", f"sg{q}"], writes=[(f"stg{si}", tt)])
                    kb.dma("sp", hu[blk * 128:(blk + 1) * 128, :], stg[si][:],
                           reads=[(f"stg{si}", tt) for tt in range(TT)], writes=[])
                    for tt in range(TT):
                        kb.readers.setdefault((f"stg{si}", tt), {}).update(kb.readers.get((f"stg{si}", 0), {}))
                wvv = loadw(OFF_V)
                kb.dma("pool", wkw[:], wv[:, :, OFF_KI:OFF_KI + 72], writes=["wkw"])
                si = nst[0] % 2
                nst[0] += 1
                for tt in range(TT):
                    p = npa[0] % 3
                    npa[0] += 1
                    for c in range(8):
                        kb.op("pe", lambda c=c, p=p, tt=tt: nc.tensor.matmul(
                            pa[p][0:64, :], wkw[:, c, 0:64], xb[:, c, tt * 512:(tt + 1) * 512],
                            start=(c == 0), stop=(c == 7)), reads=["wkw", ("xb", c)], writes=[f"pa{p}"])
                    evac(stg[si][0:64, tt * 512:(tt + 1) * 512], pa[p][0:64, :], [f"pa{p}"], [(f"stg{si}", tt)])
                kb.dma("sp", hki[:, :], stg[si][0:64, :], reads=[(f"stg{si}", tt) for tt in range(TT)])
                for tt in range(TT):
                    kb.readers.setdefault((f"stg{si}", tt), {}).update(kb.readers.get((f"stg{si}", 0), {}))
                hv4 = hv.rearrange("(n p) c -> p n c", p=128)
                for i in range(NT):
                    p = npa[0] % 3
                    npa[0] += 1
                    vi = (i // 4) % 2
                    for c in range(8):
                        kb.op("pe", lambda c=c, p=p, i=i: nc.tensor.matmul(
                            pa[p][:, :], xb[:, c, i * 128:(i + 1) * 128], wb[wvv][:, c, :],
                            start=(c == 0), stop=(c == 7)), reads=[f"wb{wvv}", ("xb", c)], writes=[f"pa{p}"])
                    evac(vst[vi][:, i % 4, :], pa[p][:, :], [f"pa{p}"], [(f"vst{vi}", i % 4)])
                    for c in range(8):
                        kb.op("pe", lambda c=c, i=i: nc.tensor.matmul(
                            pw[:, :], xb[:, c, i * 128:(i + 1) * 128], wkw[:, c, 64:72],
                            start=(c == 0), stop=(c == 7)), reads=["wkw", ("xb", c)], writes=["pw"])
                    kb.op("dve", lambda i=i: nc.vector.tensor_copy(wst[:, i, :], pw[:, :]), reads=["pw"], writes=["wst"])
                    if i % 4 == 3:
                        g = i // 4
                        kb.dma("sp", hv4[:, 4 * g:4 * g + 4, :], vst[vi][:], reads=[(f"vst{vi}", k) for k in range(4)])
                        for k in range(1, 4):
                            kb.readers.setdefault((f"vst{vi}", k), {}).update(kb.readers.get((f"vst{vi}", 0), {}))
                kb.dma("sp", hw.rearrange("(n p) e -> p n e", p=128), wst[:], reads=["wst"])
                kb.barrier()

        def phase_attn(l):
            with ExitStack() as st:
                kT = SB(st, "kT", [128, 4, S], BF16)
                kiT = SB(st, "kiT", [128, S], BF16)
                va = SB(st, "va", [128, NT, 8, 65], BF16)
                qT = [SB(st, f"qT{i}", [128, 4, 128], BF16) for i in range(4)]
                qiT = [SB(st, f"qiT{i}", [128, 4, 128], BF16) for i in range(2)]
                wt = [SB(st, f"wt{i}", [128, 8], F32) for i in range(2)]
                sgn = [SB(st, f"sgn{i}", [128, 8], F32) for i in range(2)]
                absw = [SB(st, f"absw{i}", [128, 8], F32) for i in range(2)]
                dg = [SB(st, f"dg{i}", [128, 8, 128], BF16) for i in range(2)]
                sc = [SB(st, f"sc{i}", [128, S], F32) for i in range(3)]
                rl = [SB(st, f"rl{i}", [128, 512], BF16) for i in range(3)]
                junk = SB(st, "junk", [128, S], BF16)
                nm = SB(st, "nm", [128, S], BF16)
                nmT = [SB(st, f"nmT{i}", [128, NT, 128], BF16) for i in range(2)]
                bs = [SB(st, f"bs{i}", [128, 8], F32) for i in range(2)]
                hta = [SB(st, f"hta{i}", [128, N_BISECT + 1], F32) for i in range(2)]
                htb = [SB(st, f"htb{i}", [128, N_BISECT + 1], F32) for i in range(2)]
                pt = [SB(st, f"pt{i}", [128, 4, 128], BF16) for i in range(3)]
                rec = SB(st, "rec", [128, 512], F32)
                num = SB(st, "num", [128, 512], F32)
                ao = [SB(st, f"ao{i}", [128, 8, 128], BF16) for i in range(2)]
                psD = [PS(st, f"psD{i}", [128, 512], F32) for i in range(2)]
                psS = PS(st, "psS", [128, 512], F32)
                ptr = PS(st, "ptr", [128, 4, 128], BF16)
                psL = [PS(st, f"psL{i}", [128, 4, 128], F32) for i in range(2)]
                psO = PS(st, "psO", [128, 4, 128], F32)
                psR = PS(st, "psR", [128, 512], F32)

                for c in range(4):
                    kb.dma("sp", kT[:, c, :], cm(hk)[:, c, :], writes=["kT"])
                kb.dma("sp", kiT[0:64, :], hki[:, :], writes=["kiT"])
                kb.dma("sp", kiT[64:128, :], hki[:, :], writes=["kiT"])
                kb.op("pool", lambda: nc.gpsimd.memset(va[:], 1.0), writes=["va"])
                hv5 = hv.rearrange("(n p) (h d) -> p n h d", p=128, d=64)
                for n_ in range(NT):
                    kb.dma("sp", va[:, n_, :, 0:64], hv5[:, n_, :, :], writes=["va"])
                hw3 = hw.rearrange("(n p) e -> n p e", p=128)
                hma_v = hma.rearrange("h d t -> d h t")
                nrl = [0]
                npt = [0]
                nL = [0]

                def gen_score(i):
                    T0 = i * 128
                    L = T0 + 128
                    b = i % 2
                    b3 = i % 3
                    q4 = i % 4
                    kb.dma("sp", qT[q4][:], cm(hq)[:, :, T0:T0 + 128], writes=[f"qT{q4}"])
                    if i < 2:
                        return
                    kb.dma("sp", qiT[b][:], cm(hqi)[:, :, T0:T0 + 128], writes=[f"qiT{b}"])
                    kb.dma("sp", wt[b][:], hw3[i], writes=[f"wt{b}"])
                    kb.op("act", lambda: nc.scalar.activation(out=sgn[b][:], in_=wt[b][:], func=AF.Sign),
                          reads=[f"wt{b}"], writes=[f"sgn{b}"])
                    kb.op("dve", lambda: nc.vector.tensor_tensor(out=absw[b][:], in0=wt[b][:], in1=sgn[b][:], op=ALU.mult),
                          reads=[f"wt{b}", f"sgn{b}"], writes=[f"absw{b}"])
                    for h in range(8):
                        kb.op("dve", lambda h=h: nc.vector.tensor_scalar(
                            out=dg[b][:, h, :], in0=identb[:], scalar1=sgn[b][:, h:h + 1], scalar2=None,
                            op0=ALU.mult), reads=["identb", f"sgn{b}"], writes=[(f"dg{b}", h)])
                    yield
                    nsb = (L + 511) // 512
                    units = [(sbk, h) for sbk in range(nsb) for h in range(8)]
                    pend = None

                    def flush(pend):
                        sbk, h, r, wd, c0 = pend
                        kb.op("pe", lambda: nc.tensor.matmul(
                            psS[:, 0:wd], dg[b][:, h, :], rl[r][:, 0:wd], start=(h == 0), stop=(h == 7)),
                            reads=[(f"dg{b}", h), f"rl{r}"], writes=["psS"])
                        if h == 7:
                            kb.op("dve", lambda: nc.vector.tensor_copy(sc[b3][:, c0:c0 + wd], psS[:, 0:wd]),
                                  reads=["psS"], writes=[f"sc{b3}"])

                    for (sbk, h) in units:
                        c0 = sbk * 512
                        wd = min(512, L - c0)
                        hp = (h % 2) * 64
                        d = nrl[0] % 2
                        r = nrl[0] % 3
                        nrl[0] += 1
                        kb.op("pe", lambda: nc.tensor.matmul(
                            psD[d][:, 0:wd], qiT[b][hp:hp + 64, h // 2, :], kiT[hp:hp + 64, c0:c0 + wd],
                            start=True, stop=True), reads=[f"qiT{b}", "kiT"], writes=[f"psD{d}"])
                        kb.op("act", lambda: nc.scalar.activation(
                            out=rl[r][:, 0:wd], in_=psD[d][:, 0:wd], func=AF.Relu, scale=absw[b][:, h:h + 1]),
                            reads=[f"psD{d}", f"absw{b}"], writes=[f"rl{r}"])
                        if pend is not None:
                            flush(pend)
                        pend = (sbk, h, r, wd, c0)
                        yield
                    flush(pend)
                    kb.op("dve", lambda: nc.vector.memset(sc[b3][0:64, L - 64:L], NEG), writes=[f"sc{b3}"])
                    yield

                def gen_bisect(i):
                    T0 = i * 128
                    L = T0 + 128
                    b3 = i % 3
                    nb_ = i % 2
                    c_ = i % 2
                    bsk, htak, htbk = f"bs{c_}", f"hta{c_}", f"htb{c_}"
                    bs_, hta_, htb_ = bs[c_], hta[c_], htb[c_]
                    if i >= 2:
                        use_act = (i % 2 == 0)
                        sg_ = -1.0 if use_act else 1.0
                        kb.op("dve", lambda: nc.vector.tensor_reduce(out=bs_[:, 0:1], in_=sc[b3][:, 0:L], axis=AX.X, op=ALU.max),
                              reads=[f"sc{b3}"], writes=[bsk])
                        kb.op("dve", lambda: nc.vector.tensor_reduce(out=bs_[:, 1:2], in_=sc[b3][:, 0:L - 64], axis=AX.X, op=ALU.min),
                              reads=[f"sc{b3}", bsk], writes=[bsk])
                        kb.op("dve", lambda: nc.vector.tensor_tensor(out=bs_[:, 2:3], in0=bs_[:, 0:1], in1=bs_[:, 1:2], op=ALU.subtract),
                              reads=[bsk], writes=[bsk])
                        kb.op("dve", lambda: nc.vector.tensor_scalar(out=hta_[:], in0=pow2[:], scalar1=bs_[:, 2:3], scalar2=2.0 * sg_,
                                                                     op0=ALU.mult, op1=ALU.mult), reads=[bsk, "pow2"], writes=[htak])
                        kb.op("dve", lambda: nc.vector.tensor_scalar(out=htb_[:], in0=pow2[:], scalar1=bs_[:, 2:3], scalar2=-sg_,
                                                                     op0=ALU.mult, op1=ALU.mult), reads=[bsk, "pow2"], writes=[htbk])
                        kb.op("dve", lambda: nc.vector.scalar_tensor_tensor(out=bs_[:, 5:6], in0=bs_[:, 1:2], scalar=sg_, in1=htb_[:, 0:1],
                                                                            op0=ALU.mult, op1=ALU.subtract),
                              reads=[bsk, htbk], writes=[bsk])
                        yield
                        for it in range(N_BISECT):
                            if use_act:
                                kb.op("act", lambda: nc.scalar.activation(
                                    out=junk[:, 0:L], in_=sc[b3][:, 0:L], func=AF.Sign, bias=bs_[:, 5:6], scale=1.0,
                                    accum_out=bs_[:, 3:4]), reads=[f"sc{b3}", bsk], writes=[bsk])
                                Kp = float(512 - L)
                            else:
                                kb.op("dve", lambda: nc.vector.tensor_scalar(
                                    out=junk[:, 0:L], in0=sc[b3][:, 0:L], scalar1=bs_[:, 5:6], scalar2=None, op0=ALU.is_ge,
                                    op1=ALU.add, accum_out=bs_[:, 3:4]), reads=[f"sc{b3}", bsk], writes=[bsk])
                                Kp = 256.0
                            kb.op("dve", lambda it=it, Kp=Kp: nc.vector.scalar_tensor_tensor(
                                out=bs_[:, 4:5], in0=bs_[:, 3:4], scalar=Kp, in1=hta_[:, it + 1:it + 2], op0=ALU.is_ge, op1=ALU.mult),
                                reads=[bsk, htak], writes=[bsk])
                            kb.op("dve", lambda it=it: nc.vector.scalar_tensor_tensor(
                                out=bs_[:, 5:6], in0=bs_[:, 5:6], scalar=htb_[:, it + 1:it + 2], in1=bs_[:, 4:5], op0=ALU.add, op1=ALU.add),
                                reads=[bsk, htbk], writes=[bsk])
                            yield
                        kb.op("dve", lambda: nc.vector.tensor_scalar(out=bs_[:, 4:5], in0=bs_[:, 5:6], scalar1=sg_, scalar2=None, op0=ALU.mult),
                              reads=[bsk], writes=[bsk])
                        kb.op("dve", lambda: nc.vector.scalar_tensor_tensor(
                            out=bs_[:, 6:7], in0=bs_[:, 2:3], scalar=-(2.0 ** -(N_BISECT + 1)), in1=bs_[:, 4:5], op0=ALU.mult, op1=ALU.add),
                            reads=[bsk], writes=[bsk])
                        kb.op("dve", lambda: nc.vector.tensor_scalar(
                            out=nm[:, 0:L], in0=sc[b3][:, 0:L], scalar1=bs_[:, 6:7], scalar2=NEGM, op0=ALU.is_lt, op1=ALU.mult),
                            reads=[f"sc{b3}", bsk], writes=["nm"])
                    else:
                        kb.op("dve", lambda: nc.vector.memset(nm[:, 0:L], 0.0), writes=["nm"])
                        kb.op("dve", lambda: nc.vector.memset(nm[0:64, L - 64:L], NEGM), writes=["nm"])
                    yield
                    for j0 in range(0, i + 1, 4):
                        cnt = min(4, i + 1 - j0)
                        for jj in range(cnt):
                            j = j0 + jj
                            kb.op("pe", lambda j=j, jj=jj: nc.tensor.transpose(ptr[:, jj, :], nm[:, j * 128:(j + 1) * 128], identb[:]),
                                  reads=["nm", "identb"], writes=["ptr"])
                        kb.op("dve", lambda: nc.vector.tensor_copy(nmT[nb_][:, j0:j0 + cnt, :], ptr[:, 0:cnt, :]),
                              reads=["ptr"], writes=[f"nmT{nb_}"])
                        yield

                def gen_attend(i):
                    T0 = i * 128
                    q4 = i % 4
                    nb_ = i % 2
                    ab = i % 2
                    for half in range(2):
                        pend = None

                        def flush(pend):
                            hh, h, j0, cnt, p_ = pend
                            for jj in range(cnt):
                                j = j0 + jj
                                kb.op("pe", lambda j=j, jj=jj: nc.tensor.matmul(
                                    psO[0:65, hh, :], va[:, j, h, :], pt[p_][:, jj, :], start=(j == 0), stop=(j == i)),
                                    reads=["va", f"pt{p_}"], writes=["psO"])

                        for hh in range(4):
                            h = half * 4 + hh
                            hp = (h % 2) * 64
                            for j0 in range(0, i + 1, 4):
                                cnt = min(4, i + 1 - j0)
                                Lb = nL[0] % 2
                                nL[0] += 1
                                p_ = npt[0] % 3
                                npt[0] += 1
                                kb.op("pe", lambda: nc.tensor.matmul(
                                    psL[Lb][:, 0:cnt, :], identb[:], nmT[nb_][:, j0:j0 + cnt, :], start=True, stop=False),
                                    reads=["identb", f"nmT{nb_}"], writes=[f"psL{Lb}"])
                                for jj in range(cnt):
                                    j = j0 + jj
                                    near = j >= i - 1
                                    lastj = (jj == cnt - 1)
                                    kb.op("pe", lambda j=j, jj=jj, near=near, lastj=lastj: nc.tensor.matmul(
                                        psL[Lb][:, jj, :], kT[hp:hp + 64, h // 2, j * 128:(j + 1) * 128],
                                        qT[q4][hp:hp + 64, h // 2, :], start=False, stop=(lastj and not near)),
                                        reads=["kT", f"qT{q4}"], writes=[f"psL{Lb}"])
                                    if near:
                                        o = i - j
                                        kb.op("pe", lambda o=o, jj=jj: nc.tensor.matmul(
                                            psL[Lb][:, jj, :], identb[:], bias_hi[:, h, o, :], start=False, stop=False),
                                            reads=["identb", "bias_hi"], writes=[f"psL{Lb}"])
                                        kb.op("pe", lambda o=o, jj=jj, lastj=lastj: nc.tensor.matmul(
                                            psL[Lb][:, jj, :], identb[:], bias_lo[:, h, o, :], start=False, stop=lastj),
                                            reads=["identb", "bias_lo"], writes=[f"psL{Lb}"])
                                kb.op("act", lambda: nc.scalar.activation(
                                    out=pt[p_][:, 0:cnt, :], in_=psL[Lb][:, 0:cnt, :], func=AF.Exp,
                                    bias=rbb[:, 15 * 8 + h:15 * 8 + h + 1], scale=0.125),
                                    reads=[f"psL{Lb}", "rbb"], writes=[f"pt{p_}"])
                                if pend is not None:
                                    flush(pend)
                                pend = (hh, h, j0, cnt, p_)
                                yield
                        flush(pend)
                        kb.op("dve", lambda: nc.vector.reciprocal(out=rec[64:65, :], in_=psO[64:65, :, :].rearrange("p a b -> p (a b)")),
                              reads=["psO"], writes=["rec"])
                        kb.op("pe", lambda: nc.tensor.matmul(psR[0:64, :], ones32[64:65, :], rec[64:65, :], start=True, stop=True),
                              reads=["ones32", "rec"], writes=["psR"])
                        kb.op("act", lambda: nc.scalar.copy(num[0:64, :], psO[0:64, :, :].rearrange("p a b -> p (a b)")),
                              reads=["psO"], writes=["num"])
                        for hh in range(4):
                            h = half * 4 + hh
                            kb.op("dve", lambda h=h, hh=hh: nc.vector.scalar_tensor_tensor(
                                out=ao[ab][0:64, h, :], in0=num[0:64, hh * 128:(hh + 1) * 128],
                                scalar=vec[0:64, l, 48 + h:48 + h + 1], in1=psR[0:64, hh * 128:(hh + 1) * 128],
                                op0=ALU.mult, op1=ALU.mult), reads=["num", "psR", "vec"], writes=[(f"ao{ab}", h)])
                        yield
                    kb.dma("act", hma_v[:, :, T0:T0 + 128], ao[ab][0:64, :, :], reads=[(f"ao{ab}", h) for h in range(8)])

                def run_step(must, carry, cap=12):
                    must = [g for g in must if g is not None]
                    ncar = 0
                    while must:
                        nxt = []
                        for g in must:
                            try:
                                next(g)
                                nxt.append(g)
                            except StopIteration:
                                pass
                        must = nxt
                        if carry is not None and ncar < cap:
                            ncar += 1
                            try:
                                next(carry)
                            except StopIteration:
                                carry = None
                    return carry

                run_step([gen_score(0)], None)
                old = None
                for k in range(NT + 2):
                    new = gen_bisect(k) if k < NT else None
                    must = [gen_score(k + 1) if k + 1 < NT else None, old, gen_attend(k - 2) if k >= 2 else None]
                    if all(g is None for g in must):
                        must = [new]
                        new = None
                        old = run_step(must, None)
                    else:
                        old = run_step(must, new)
                kb.barrier()

        def phase_conv(l):
            with ExitStack() as st:
                u = SB(st, "u", [128, 4, S + 32], BF16)
                cw = SB(st, "cw", [128, 4, CONV_K], F32)
                dgc = SB(st, "dgc", [128, 4, CONV_K, 128], BF16)
                y32 = SB(st, "y32", [128, 4, 512], F32)
                yb = SB(st, "yb", [128, 4, 512], BF16)
                ysq = SB(st, "ysq", [128, 4, 512], BF16)
                mean = SB(st, "cmean", [128, 512], F32)
                rstd = SB(st, "crstd", [128, 512], F32)
                t1 = SB(st, "ct1", [128, 4, 512], F32)
                so = [SB(st, f"cso{i}", [128, 4, 512], BF16) for i in range(2)]
                pc = [PS(st, f"pc{i}", [128, 512], F32) for i in range(2)]
                pm = PS(st, "pm", [128, 512], F32)
                pq = PS(st, "pq", [128, 512], F32)
                kb.op("pool", lambda: nc.gpsimd.memset(u[:, :, 0:32], 0.0), writes=["u"])
                kb.dma("sp", u[:, :, 32:], cm(hu), writes=["u"])
                kb.dma("sp", cw[:], conv_wT[l].rearrange("(c p) j -> p c j", p=128), writes=["cw"])
                for c in range(4):
                    for j in range(CONV_K):
                        kb.op("dve", lambda c=c, j=j: nc.vector.tensor_scalar(
                            out=dgc[:, c, j, :], in0=identb[:], scalar1=cw[:, c, j:j + 1], scalar2=None, op0=ALU.mult),
                            reads=["identb", "cw"], writes=[("dgc", c)])
                od = onesdiv["od512"]
                for tt in range(TT):
                    for c in range(4):
                        p = c % 2
                        for j in range(CONV_K):
                            off = 2 + tt * 512 + j
                            kb.op("pe", lambda c=c, j=j, p=p, off=off: nc.tensor.matmul(
                                pc[p][:, :], dgc[:, c, j, :], u[:, c, off:off + 512], start=(j == 0), stop=(j == CONV_K - 1)),
                                reads=[("dgc", c), "u"], writes=[f"pc{p}"])
                        kb.op("act", lambda c=c, p=p: nc.scalar.activation(out=y32[:, c, :], in_=pc[p][:, :], func=AF.Identity,
                                                                           bias=vec[:, l, c:c + 1], scale=1.0),
                              reads=[f"pc{p}", "vec"], writes=[("y32", c)])
                        kb.op("act", lambda c=c: nc.scalar.activation(out=ysq[:, c, :], in_=y32[:, c, :], func=AF.Square),
                              reads=[("y32", c)], writes=[("ysq", c)])
                        kb.op("dve", lambda c=c: nc.vector.tensor_copy(yb[:, c, :], y32[:, c, :]), reads=[("y32", c)],
                              writes=[("yb", c)])
                    for c in range(4):
                        kb.op("pe", lambda c=c: nc.tensor.matmul(pm[:, :], od[:], yb[:, c, :], start=(c == 0), stop=(c == 3)),
                              reads=["od512", ("yb", c)], writes=["pm"])
                    for c in range(4):
                        kb.op("pe", lambda c=c: nc.tensor.matmul(pq[:, :], od[:], ysq[:, c, :], start=(c == 0), stop=(c == 3)),
                              reads=["od512", ("ysq", c)], writes=["pq"])
                    ln_tail(mean, rstd, pm, pq, "c")
                    sb_ = tt % 2
                    kb.op("dve", lambda: nc.vector.tensor_tensor(out=t1[:], in0=y32[:], in1=bcast_mid(mean[:], 4), op=ALU.subtract),
                          reads=[("y32", c) for c in range(4)] + ["cmean"], writes=["ct1"])
                    kb.op("dve", lambda: nc.vector.tensor_tensor(out=t1[:], in0=t1[:], in1=bcast_mid(rstd[:], 4), op=ALU.mult),
                          reads=["ct1", "crstd"], writes=["ct1"])
                    for c in range(4):
                        kb.op("act", lambda c=c: nc.scalar.activation(out=t1[:, c, :], in_=t1[:, c, :], func=AF.Silu,
                                                                      bias=vec[:, l, 8 + c:9 + c], scale=vec[:, l, 4 + c:5 + c]),
                              reads=["ct1", "vec"], writes=["ct1"])
                        kb.op("dve", lambda c=c, sb_=sb_: nc.vector.tensor_scalar(
                            out=so[sb_][:, c, :], in0=t1[:, c, :], scalar1=vec[:, l, 12 + c:13 + c], scalar2=None, op0=ALU.mult),
                            reads=["ct1", "vec"], writes=[f"cso{sb_}"])
                    kb.dma("act", cm(hmc)[:, :, tt * 512:(tt + 1) * 512], so[sb_][:], reads=[f"cso{sb_}"])
                kb.barrier()

        def ln_tail(mean, rstd, pm, pq, tag):
            mk, rk = f"{tag}mean", f"{tag}rstd"
            kb.op("act", lambda: nc.scalar.copy(mean[:], pm[:, :]), reads=["pm"], writes=[mk])
            kb.op("dve", lambda: nc.vector.tensor_tensor(out=rstd[:], in0=mean[:], in1=mean[:], op=ALU.mult),
                  reads=[mk], writes=[rk])
            kb.op("dve", lambda: nc.vector.tensor_tensor(out=rstd[:], in0=pq[:, :], in1=rstd[:], op=ALU.subtract),
                  reads=["pq", rk], writes=[rk])
            kb.op("dve", lambda: nc.vector.tensor_scalar(out=rstd[:], in0=rstd[:], scalar1=LN_EPS, scalar2=None, op0=ALU.add),
                  reads=[rk], writes=[rk])
            kb.op("act", lambda: nc.scalar.activation(out=rstd[:], in_=rstd[:], func=AF.Sqrt), reads=[rk], writes=[rk])
            kb.op("dve", lambda: nc.vector.reciprocal(out=rstd[:], in_=rstd[:]), reads=[rk], writes=[rk])

        def phase_outproj(l):
            with ExitStack() as st:
                woA = SB(st, "woA", [128, 8, D], BF16)
                woC = SB(st, "woC", [128, 4, D], BF16)
                ma = [SB(st, f"ma{i}", [128, 8, 512], BF16) for i in range(2)]
                mc = [SB(st, f"mc{i}", [128, 4, 512], BF16) for i in range(2)]
                fo = [SB(st, f"fo{i}", [128, 8, 512], F32) for i in range(2)]
                pp = [PS(st, f"pp{i}", [128, 512], F32) for i in range(3)]
                kb.dma("pool", woA[0:64, :, :], w_out[l][0:512, :].rearrange("(h d) n -> d h n", d=64), writes=["woA"])
                kb.dma("pool", woC[:], w_out[l][512:1024, :].rearrange("(c p) n -> p c n", p=128), writes=["woC"])
                hma_v = hma.rearrange("h d t -> d h t")
                n = 0
                for tt in range(TT):
                    b = tt % 2
                    kb.dma("sp", ma[b][0:64, :, :], hma_v[:, :, tt * 512:(tt + 1) * 512], writes=[f"ma{b}"])
                    kb.dma("sp", mc[b][:], cm(hmc)[:, :, tt * 512:(tt + 1) * 512], writes=[f"mc{b}"])
                    for dc in range(8):
                        p = n % 3
                        n += 1
                        for h in range(8):
                            kb.op("pe", lambda h=h, dc=dc, p=p, b=b: nc.tensor.matmul(
                                pp[p][:, :], woA[0:64, h, dc * 128:(dc + 1) * 128], ma[b][0:64, h, :], start=(h == 0), stop=False),
                                reads=["woA", f"ma{b}"], writes=[f"pp{p}"])
                        for c in range(4):
                            kb.op("pe", lambda c=c, dc=dc, p=p, b=b: nc.tensor.matmul(
                                pp[p][:, :], woC[:, c, dc * 128:(dc + 1) * 128], mc[b][:, c, :], start=False, stop=(c == 3)),
                                reads=["woC", f"mc{b}"], writes=[f"pp{p}"])
                        if dc % 2 == 0:
                            kb.op("act", lambda dc=dc, p=p, b=b: nc.scalar.copy(fo[b][:, dc, :], pp[p][:, :]),
                                  reads=[f"pp{p}"], writes=[(f"fo{b}", dc)])
                        else:
                            kb.op("dve", lambda dc=dc, p=p, b=b: nc.vector.tensor_copy(fo[b][:, dc, :], pp[p][:, :]),
                                  reads=[f"pp{p}"], writes=[(f"fo{b}", dc)])
                    kb.dma("act", cm(fsc)[:, :, tt * 512:(tt + 1) * 512], fo[b][:], reads=[(f"fo{b}", dc) for dc in range(8)])
                    for dc in range(1, 8):
                        kb.readers.setdefault((f"fo{b}", dc), {}).update(kb.readers.get((f"fo{b}", 0), {}))
                kb.barrier()

        def phase_ln(l, which, dst, router=None):
            gcol = 16 + 16 * which
            with ExitStack() as st:
                x32 = [SB(st, f"lx{i}", [128, 8, 512], F32) for i in range(2)]
                f32_ = [SB(st, f"lf{i}", [128, 8, 512], F32) for i in range(2)]
                zb = SB(st, "zb", [128, 8, 512], BF16)
                zsq = SB(st, "zsq", [128, 8, 512], BF16)
                mean = SB(st, "lmean", [128, 512], F32)
                rstd = SB(st, "lrstd", [128, 512], F32)
                xo = [SB(st, f"lo{i}", [128, 8, 512], F32) for i in range(2)]
                pm = PS(st, "pm", [128, 512], F32)
                pq = PS(st, "pq", [128, 512], F32)
                od = onesdiv["od1024"]
                if router is not None:
                    rw = SB(st, "rw", [128, 8, NE], F32)
                    lg = SB(st, "lg", [128, NE], F32)
                    mx8 = SB(st, "mx8", [128, 8], F32)
                    ex = SB(st, "ex", [128, NE], F32)
                    gs = SB(st, "gs", [128, 4], F32)
                    gt = SB(st, "gt", [128, NE], F32)
                    gT = [SB(st, f"gT{i}", [8, 512], F32) for i in range(2)]
                    pr = PS(st, "pr", [128, NE], F32)
                    pgt = PS(st, "pgt", [8, 512], F32)
                    kb.dma("sp", rw[:], router.rearrange("(c p) e -> p c e", p=128), writes=["rw"])
                for tt in range(TT):
                    b = tt % 2
                    ts_ = slice(tt * 512, (tt + 1) * 512)
                    kb.dma("sp", x32[b][:], cm(xm)[:, :, ts_], writes=[f"lx{b}"])
                    kb.dma("sp", f32_[b][:], cm(fsc)[:, :, ts_], writes=[f"lf{b}"])
                    kb.op("dve", lambda b=b: nc.vector.scalar_tensor_tensor(
                        out=x32[b][:], in0=x32[b][:], scalar=DN_ALPHA, in1=f32_[b][:], op0=ALU.mult, op1=ALU.add),
                        reads=[f"lx{b}", f"lf{b}"], writes=[f"lx{b}"])
                    kb.op("act", lambda b=b: nc.scalar.copy(zb[:], x32[b][:]), reads=[f"lx{b}"], writes=["zb"])
                    kb.op("act", lambda b=b: nc.scalar.activation(out=zsq[:], in_=x32[b][:], func=AF.Square),
                          reads=[f"lx{b}"], writes=["zsq"])
                    for c in range(8):
                        kb.op("pe", lambda c=c: nc.tensor.matmul(pm[:, :], od[:], zb[:, c, :], start=(c == 0), stop=(c == 7)),
                              reads=["od1024", "zb"], writes=["pm"])
                    for c in range(8):
                        kb.op("pe", lambda c=c: nc.tensor.matmul(pq[:, :], od[:], zsq[:, c, :], start=(c == 0), stop=(c == 7)),
                              reads=["od1024", "zsq"], writes=["pq"])
                    ln_tail(mean, rstd, pm, pq, "l")
                    kb.op("dve", lambda b=b: nc.vector.tensor_tensor(out=x32[b][:], in0=x32[b][:], in1=bcast_mid(mean[:], 8),
                                                                     op=ALU.subtract),
                          reads=[f"lx{b}", "lmean"], writes=[f"lx{b}"])
                    kb.op("dve", lambda b=b: nc.vector.tensor_tensor(out=x32[b][:], in0=x32[b][:], in1=bcast_mid(rstd[:], 8),
                                                                     op=ALU.mult),
                          reads=[f"lx{b}", "lrstd"], writes=[f"lx{b}"])
                    for c in range(8):
                        kb.op("act", lambda c=c, b=b: nc.scalar.activation(
                            out=xo[b][:, c, :], in_=x32[b][:, c, :], func=AF.Identity,
                            bias=vec[:, l, gcol + 8 + c:gcol + 9 + c], scale=vec[:, l, gcol + c:gcol + c + 1]),
                            reads=[f"lx{b}", "vec"], writes=[(f"lo{b}", c)])
                    kb.dma("act", cm(dst)[:, :, ts_], xo[b][:], reads=[(f"lo{b}", c) for c in range(8)])
                    for c in range(1, 8):
                        kb.readers.setdefault((f"lo{b}", c), {}).update(kb.readers.get((f"lo{b}", 0), {}))
                    if router is not None:
                        for sub in range(4):
                            for c in range(8):
                                kb.op("pe", lambda c=c, b=b, sub=sub: nc.tensor.matmul(
                                    pr[:, :], xo[b][:, c, sub * 128:(sub + 1) * 128], rw[:, c, :], start=(c == 0), stop=(c == 7)),
                                    reads=[(f"lo{b}", c), "rw"], writes=["pr"])
                            kb.op("dve", lambda: nc.vector.tensor_copy(lg[:], pr[:, :]), reads=["pr"], writes=["lg"])
                            kb.op("dve", lambda: nc.vector.max(out=mx8[:], in_=lg[:]), reads=["lg"], writes=["mx8"])
                            kb.op("dve", lambda: nc.vector.tensor_scalar(out=gs[:, 0:1], in0=mx8[:, 0:1], scalar1=-1.0, scalar2=None,
                                                                         op0=ALU.mult), reads=["mx8"], writes=["gs"])
                            kb.op("act", lambda: nc.scalar.activation(out=ex[:], in_=lg[:], func=AF.Exp, bias=gs[:, 0:1], scale=1.0),
                                  reads=["lg", "gs"], writes=["ex"])
                            kb.op("dve", lambda: nc.vector.scalar_tensor_tensor(
                                out=ex[:], in0=lg[:], scalar=mx8[:, 1:2], in1=ex[:], op0=ALU.is_ge, op1=ALU.mult),
                                reads=["lg", "mx8", "ex"], writes=["ex"])
                            kb.op("dve", lambda: nc.vector.tensor_reduce(out=gs[:, 1:2], in_=ex[:], axis=AX.X, op=ALU.add),
                                  reads=["ex", "gs"], writes=["gs"])
                            kb.op("dve", lambda: nc.vector.reciprocal(out=gs[:, 2:3], in_=gs[:, 1:2]), reads=["gs"], writes=["gs"])
                            kb.op("dve", lambda: nc.vector.tensor_scalar(out=gt[:], in0=ex[:], scalar1=gs[:, 2:3], scalar2=None,
                                                                         op0=ALU.mult), reads=["ex", "gs"], writes=["gt"])
                            kb.op("pe", lambda sub=sub: nc.tensor.transpose(pgt[0:8, sub * 128:(sub + 1) * 128], gt[:, :], ident32[:]),
                                  reads=["gt", "ident32"], writes=["pgt"])
                        kb.op("dve", lambda b=b: nc.vector.tensor_copy(gT[b][:], pgt[:, :]), reads=["pgt"], writes=[f"gT{b}"])
                        kb.dma("act", hg[:, ts_], gT[b][:], reads=[f"gT{b}"])
                kb.barrier()

        def phase_ffn(l):
            moe = (l % 2 == 1)
            m = l // 2
            HW = D_FFE if moe else D_FF
            nexp = NE if moe else 1
            groups = [(c0, min(512, HW - c0)) for c0 in range(0, HW, 512)]
            HT = 2048
            with ExitStack() as st:
                xb = SB(st, "fxb", [128, 8, HT], BF16)
                yacc = SB(st, "yacc", [128, 8, HT], F32)
                wg = [SB(st, f"fwg{i}", [128, 8, 512], BF16) for i in range(2)]
                wu = [SB(st, f"fwu{i}", [128, 8, 512], BF16) for i in range(2)]
                wd = [SB(st, f"fwd{i}", [128, 4, D], BF16) for i in range(2)]
                sgm = [SB(st, f"fsg{i}", [128, 512], BF16) for i in range(2)]
                hT = [SB(st, f"fhT{i}", [128, 4, 512], BF16) for i in range(2)]
                htmp = [SB(st, f"fht{i}", [128, 512], F32) for i in range(2)]
                pA = [PS(st, f"pA{i}", [128, 512], F32) for i in range(2)]
                pB = [PS(st, f"pB{i}", [128, 512], F32) for i in range(2)]
                pY = [PS(st, f"pY{i}", [128, 512], F32) for i in range(2)]
                if moe:
                    gTs = SB(st, "gTs", [8, HT], F32)
                    gbc = SB(st, "gbc", [128, 4, 512], BF16)
                    pG = PS(st, "pG", [128, 512], F32)
                nab = 0
                ny = 0
                nh = 0
                items = []
                for half in range(S // HT):
                    for e in range(nexp):
                        for gi, (c0, wdt) in enumerate(groups):
                            items.append((half, e, gi, c0, wdt))

                def issue_w(k):
                    half, e, gi, c0, wdt = items[k]
                    ws = k % 2
                    if moe:
                        Wg, Wu, Wd = moe_wg[m, e], moe_wu[m, e], moe_wd[m, e]
                    else:
                        Wg, Wu, Wd = ffn_wg[m], ffn_wu[m], ffn_wd[m]
                    nfc = wdt // 128
                    kb.dma("pool", wg[ws][:, :, 0:wdt], cm(Wg)[:, :, c0:c0 + wdt], writes=[f"fwg{ws}"])
                    kb.dma("pool", wu[ws][:, :, 0:wdt], cm(Wu)[:, :, c0:c0 + wdt], writes=[f"fwu{ws}"])
                    kb.dma("pool", wd[ws][:, 0:nfc, :], Wd[c0:c0 + wdt, :].rearrange("(c p) n -> p c n", p=128),
                           writes=[f"fwd{ws}"])

                issue_w(0)
                for k, (half, e, gi, c0, wdt) in enumerate(items):
                    hs = slice(half * HT, (half + 1) * HT)
                    ws = k % 2
                    nfc = wdt // 128
                    if e == 0 and gi == 0:
                        for c in range(8):
                            kb.dma("pool", xb[:, c, :], cm(xm)[:, c, hs], writes=[("fxb", c)])
                        if moe:
                            kb.dma("sp", gTs[:], hg[:, hs], writes=["gTs"])
                    if k + 1 < len(items):
                        issue_w(k + 1)
                    first = (e == 0 and gi == 0)
                    if moe and gi == 0:
                        for tt in range(4):
                            kb.op("pe", lambda e=e, tt=tt: nc.tensor.matmul(
                                pG[:, :], sel8[0:8, e, :], gTs[0:8, tt * 512:(tt + 1) * 512], start=True, stop=True),
                                reads=["sel8", "gTs"], writes=["pG"])
                            kb.op("act", lambda tt=tt: nc.scalar.copy(gbc[:, tt, :], pG[:, :]), reads=["pG"], writes=[("gbc", tt)])
                    for tt in range(4):
                        tsl = slice(tt * 512, (tt + 1) * 512)
                        hb = nh % 2
                        nh += 1
                        for fc in range(nfc):
                            a = nab % 2
                            nab += 1
                            for c in range(8):
                                kb.op("pe", lambda c=c, a=a, ws=ws, fc=fc, tsl=tsl: nc.tensor.matmul(
                                    pA[a][:, :], wg[ws][:, c, fc * 128:(fc + 1) * 128], xb[:, c, tsl],
                                    start=(c == 0), stop=(c == 7)), reads=[f"fwg{ws}", ("fxb", c)], writes=[f"pA{a}"])
                            for c in range(8):
                                kb.op("pe", lambda c=c, a=a, ws=ws, fc=fc, tsl=tsl: nc.tensor.matmul(
                                    pB[a][:, :], wu[ws][:, c, fc * 128:(fc + 1) * 128], xb[:, c, tsl],
                                    start=(c == 0), stop=(c == 7)), reads=[f"fwu{ws}", ("fxb", c)], writes=[f"pB{a}"])
                            kb.op("act", lambda a=a: nc.scalar.activation(out=sgm[a][:], in_=pA[a][:, :], func=AF.Silu),
                                  reads=[f"pA{a}"], writes=[f"fsg{a}"])
                            if moe:
                                kb.op("dve", lambda a=a: nc.vector.tensor_tensor(out=htmp[a][:], in0=pB[a][:, :], in1=sgm[a][:],
                                                                                 op=ALU.mult),
                                      reads=[f"pB{a}", f"fsg{a}"], writes=[f"fht{a}"])
                                kb.op("dve", lambda a=a, hb=hb, fc=fc, tt=tt: nc.vector.tensor_tensor(
                                    out=hT[hb][:, fc, :], in0=htmp[a][:], in1=gbc[:, tt, :], op=ALU.mult),
                                    reads=[f"fht{a}", ("gbc", tt)], writes=[(f"fhT{hb}", fc)])
                            else:
                                kb.op("dve", lambda a=a, hb=hb, fc=fc: nc.vector.tensor_tensor(
                                    out=hT[hb][:, fc, :], in0=pB[a][:, :], in1=sgm[a][:], op=ALU.mult),
                                    reads=[f"pB{a}", f"fsg{a}"], writes=[(f"fhT{hb}", fc)])
                        for dc in range(8):
                            y = ny % 2
                            ny += 1
                            for fc in range(nfc):
                                kb.op("pe", lambda fc=fc, dc=dc, y=y, ws=ws, hb=hb: nc.tensor.matmul(
                                    pY[y][:, :], wd[ws][:, fc, dc * 128:(dc + 1) * 128], hT[hb][:, fc, :],
                                    start=(fc == 0), stop=(fc == nfc - 1)),
                                    reads=[f"fwd{ws}", (f"fhT{hb}", fc)], writes=[f"pY{y}"])
                            if first:
                                kb.op("dve", lambda dc=dc, y=y, tsl=tsl: nc.vector.tensor_copy(yacc[:, dc, tsl], pY[y][:, :]),
                                      reads=[f"pY{y}"], writes=[("yacc", dc, tsl.start)])
                            else:
                                kb.op("dve", lambda dc=dc, y=y, tsl=tsl: nc.vector.tensor_tensor(
                                    out=yacc[:, dc, tsl], in0=pY[y][:, :], in1=yacc[:, dc, tsl], op=ALU.add),
                                    reads=[f"pY{y}", ("yacc", dc, tsl.start)], writes=[("yacc", dc, tsl.start)])
                    if e == nexp - 1 and gi == len(groups) - 1:
                        kb.dma("act", cm(fsc)[:, :, hs], yacc[:],
                               reads=[("yacc", dc, t0) for dc in range(8) for t0 in range(0, HT, 512)])
                kb.barrier()

        stages = []
        for l in range(n_layers):
            last = (l == n_layers - 1)
            stages += [("proj", lambda l=l: phase_proj(l)), ("attn", lambda l=l: phase_attn(l)),
                       ("conv", lambda l=l: phase_conv(l)), ("outproj", lambda l=l: phase_outproj(l)),
                       ("ln1", lambda l=l: phase_ln(l, 0, xm, router=(moe_r[l // 2] if l % 2 == 1 else None))),
                       ("ffn", lambda l=l: phase_ffn(l)),
                       ("ln2", lambda l=l, last=last: phase_ln(l, 1, outT if last else xm))]
        for idx, (name, fn) in enumerate(stages):
            fn()
            if stop_after is not None and (idx + 1) >= stop_after:
                break
        kb.barrier()
        build_program.n_inst = kb.n_inst
    return nc


def _t5_bucket_np(rel):
    nb = 16
    max_exact = 8
    ret = np.where(rel > 0, nb, 0).astype(np.int32)
    n = np.abs(rel)
    nf = np.maximum(n, 1).astype(np.float32)
    large = max_exact + (np.log(nf / max_exact) / math.log(128 / max_exact) * (nb - max_exact)).astype(np.int32)
    large = np.minimum(large, nb - 1)
    return ret + np.where(n < max_exact, n, large)


def _const_onehot():
    s = np.arange(128)[:, None]
    t = np.arange(128)[None, :]
    oh = np.zeros((128, 32, 2, 128), np.float32)
    for o in range(2):
        rel = (s - 128 * o) - t
        bk = _t5_bucket_np(rel)
        for b in range(32):
            oh[:, b, o, :] = (bk == b)
    return oh


def _pack_vecs(inp):
    f = lambda a, n: np.asarray(a, np.float32).reshape(DEPTH, n, 128).transpose(2, 0, 1)
    v = np.zeros((128, DEPTH, NV), np.float32)
    v[:, :, 0:4] = f(inp["conv_b"], 4)
    v[:, :, 4:8] = f(inp["conv_ln_g"], 4)
    v[:, :, 8:12] = f(inp["conv_ln_b"], 4)
    ms = np.asarray(inp["mix_scale"], np.float32)
    v[:, :, 12:16] = f(ms[:, 512:], 4)
    v[:, :, 16:24] = f(inp["ln1_g"], 8)
    v[:, :, 24:32] = f(inp["ln1_b"], 8)
    v[:, :, 32:40] = f(inp["ln2_g"], 8)
    v[:, :, 40:48] = f(inp["ln2_b"], 8)
    v[0:64, :, 48:56] = ms[:, :512].reshape(DEPTH, 8, 64).transpose(2, 0, 1)
    return v


def make_in_maps(inputs, n_cores=8):
    shared = {
        "w_in": np.ascontiguousarray(inputs["w_in"], np.float32),
        "conv_wT": np.ascontiguousarray(np.asarray(inputs["conv_w"], np.float32).transpose(0, 2, 1)),
        "vecs": _pack_vecs(inputs),
        "rel_bias": np.ascontiguousarray(inputs["rel_bias"], np.float32),
        "ohb": _const_onehot(),
        "w_out": np.ascontiguousarray(inputs["w_out"], np.float32),
        "ffn_w_gate": np.ascontiguousarray(inputs["ffn_w_gate"], np.float32),
        "ffn_w_up": np.ascontiguousarray(inputs["ffn_w_up"], np.float32),
        "ffn_w_down": np.ascontiguousarray(inputs["ffn_w_down"], np.float32),
        "moe_router": np.ascontiguousarray(inputs["moe_router"], np.float32),
        "moe_w_gate": np.ascontiguousarray(inputs["moe_w_gate"], np.float32),
        "moe_w_up": np.ascontiguousarray(inputs["moe_w_up"], np.float32),
        "moe_w_down": np.ascontiguousarray(inputs["moe_w_down"], np.float32),
    }
    x = np.asarray(inputs["x"], np.float32)
    maps = []
    for b in range(n_cores):
        m = dict(shared)
        m["xT"] = np.ascontiguousarray(x[b].T)
        maps.append(m)
    return maps


def kernel(**inputs):
    nc = build_program()
    in_maps = make_in_maps(inputs, 8)
    res = run_bass_kernel_spmd(nc, in_maps, core_ids=list(range(8)))
    out = np.stack([np.ascontiguousarray(r["outT"].T) for r in res.results], axis=0)
    return out.astype(np.float32)
```

```python
import math
from contextlib import ExitStack

import numpy as np
import concourse.bass as bass
import concourse.mybir as mybir
from concourse.bass_utils import run_bass_kernel_spmd

F32 = mybir.dt.float32
BF16 = mybir.dt.bfloat16
FP8 = mybir.dt.float8e5
AF = mybir.ActivationFunctionType
ALU = mybir.AluOpType
AX = mybir.AxisListType

D = 1024
S = 4096
DEPTH = 4
NT = S // 128
TT = S // 512
IN_COLS = 3144
OFF_Q, OFF_K, OFF_V, OFF_QI, OFF_KI, OFF_WI, OFF_A, OFF_G = 0, 512, 1024, 1536, 2048, 2112, 2120, 2632
D_FF = 2816
D_FFE = 3584
NE = 8
CONV_K = 31
DN_ALPHA = (2.0 * DEPTH) ** 0.25
LN_EPS = 1e-5
NEGM = -30000.0
NEG = -1e30
N_BISECT = 16
NV = 56

EPOCH = 30000
DMA_RING = 8


class KB:
    def __init__(self, nc, es):
        self.nc = nc
        self.es = es
        self.eng = {"pe": nc.tensor, "act": nc.scalar, "dve": nc.vector, "pool": nc.gpsimd, "sp": nc.sync}
        self.count = {e: 0 for e in self.eng}
        self.dma_n = {}
        self.waited = {e: {} for e in self.eng}
        self.last_write = {}
        self.readers = {}
        self._semh = {}
        self.n_inst = 0

    def _sem(self, key):
        if key not in self._semh:
            name = "s_" + "_".join(str(k) for k in key)
            self._semh[key] = self.es.enter_context(self.nc.semaphore(name))
        return self._semh[key]

    def _wait(self, eng, token):
        key, val = token
        if self.waited[eng].get(key, 0) >= val:
            return
        self.waited[eng][key] = val
        self.eng[eng].wait_ge(self._sem(key), val)

    def _deps(self, eng, reads, writes):
        toks = []
        for b in reads:
            t = self.last_write.get(b)
            if t is not None:
                toks.append(t)
        for b in writes:
            t = self.last_write.get(b)
            if t is not None:
                toks.append(t)
            toks.extend(self.readers.get(b, {}).values())
        for t in toks:
            if eng == "pe" and t[0][0] == "c" and t[0][1] == "pe":
                continue
            self._wait(eng, t)

    def _record(self, token, reads, writes):
        for b in writes:
            self.last_write[b] = token
            self.readers[b] = {}
        for b in reads:
            self.readers.setdefault(b, {})[token[0]] = token

    def op(self, eng, ins_fn, reads=(), writes=()):
        self._deps(eng, reads, writes)
        ins = ins_fn()
        n = self.count[eng]
        self.count[eng] = n + 1
        key = ("c", eng, n // EPOCH)
        ins.then_inc(self._sem(key), 1)
        self._record((key, n % EPOCH + 1), reads, writes)
        self.n_inst += 1
        return ins

    def dma(self, q, out, in_, reads=(), writes=()):
        n = self.dma_n.get(q, 0)
        self.dma_n[q] = n + 1
        slot = n % DMA_RING
        key = ("d", q, slot)
        rnd = n // DMA_RING
        if rnd > 0:
            self._wait(q, (key, 16 * rnd))
        self._deps(q, reads, writes)
        ins = self.eng[q].dma_start(out=out, in_=in_)
        ins.then_inc(self._sem(key), 16)
        self._record((key, 16 * (rnd + 1)), reads, writes)
        self.n_inst += 1
        return ins

    def barrier(self):
        toks = []
        for e in self.eng:
            n = self.count[e]
            if n > 0:
                toks.append((("c", e, (n - 1) // EPOCH), (n - 1) % EPOCH + 1))
        for q, n in self.dma_n.items():
            for slot in range(min(n, DMA_RING)):
                toks.append((("d", q, slot), 16 * ((n - 1 - slot) // DMA_RING + 1)))
        for e in self.eng:
            for t in toks:
                self._wait(e, t)
        self.last_write = {}
        self.readers = {}


def bcast_mid(a, n):
    return bass.AP(a.tensor, a.offset, [list(a.ap[0]), [0, n], list(a.ap[1])])


def build_program(n_layers=DEPTH, debug=False, stop_after=None):
    nc = bass.Bass("TRN2", target_bir_lowering=False)
    dt_in = lambda name, shape: nc.dram_tensor(name, shape, F32, kind="ExternalInput").ap()
    xT_in = dt_in("xT", [D, S])
    w_in = dt_in("w_in", [DEPTH, D, IN_COLS])
    conv_wT = dt_in("conv_wT", [DEPTH, 512, CONV_K])
    vecs = dt_in("vecs", [128, DEPTH, NV])
    rel_bias = dt_in("rel_bias", [32, 8])
    ohb = dt_in("ohb", [128, 32, 2, 128])
    w_out = dt_in("w_out", [DEPTH, D, D])
    ffn_wg = dt_in("ffn_w_gate", [2, D, D_FF])
    ffn_wu = dt_in("ffn_w_up", [2, D, D_FF])
    ffn_wd = dt_in("ffn_w_down", [2, D_FF, D])
    moe_r = dt_in("moe_router", [2, D, NE])
    moe_wg = dt_in("moe_w_gate", [2, NE, D, D_FFE])
    moe_wu = dt_in("moe_w_up", [2, NE, D, D_FFE])
    moe_wd = dt_in("moe_w_down", [2, NE, D_FFE, D])
    outT = nc.dram_tensor("outT", [D, S], F32, kind="ExternalOutput").ap()

    skind = "ExternalOutput" if debug else "Internal"
    scr = lambda name, shape, dt: nc.dram_tensor(name, shape, dt, kind=skind).ap()
    xm = scr("xm", [D, S], F32)
    fsc = scr("fsc", [D, S], F32)
    hq = scr("hq", [512, S], BF16)
    hk = scr("hk", [512, S], BF16)
    hqi = scr("hqi", [512, S], BF16)
    hki = scr("hki", [64, S], BF16)
    hu = scr("hu", [512, S], BF16)
    hv = scr("hv", [S, 512], BF16)
    hw = scr("hw", [S, 8], F32)
    hma = scr("hma", [8, 64, S], BF16)
    hmc = scr("hmc", [512, S], BF16)
    hg = scr("hg", [8, S], F32)

    def cm(a):
        return a.rearrange("(c p) s -> p c s", p=128)

    with ExitStack() as es:
        kb = KB(nc, es)
        uid = [0]

        def SB(st, name, shape, dt):
            uid[0] += 1
            return st.enter_context(nc.sbuf_tensor(f"{name}_{uid[0]}", shape, dt))

        def PS(st, name, shape, dt):
            uid[0] += 1
            return st.enter_context(nc.psum_tensor(f"{name}_{uid[0]}", shape, dt))

        identb = SB(es, "identb", [128, 128], BF16)
        ident32 = SB(es, "ident32", [128, 128], F32)
        onesdiv = {}
        ones32 = SB(es, "ones32", [128, 64], F32)
        vec = SB(es, "vec", [128, DEPTH, NV], F32)
        rbb = SB(es, "rbb", [128, 256], F32)
        pow2 = SB(es, "pow2", [128, N_BISECT + 1], F32)
        bias_hi = SB(es, "bias_hi", [128, 8, 2, 128], BF16)
        negc = SB(es, "negc", [128, 8], F32)

        kb.op("pool", lambda: nc.gpsimd.memset(ident32[:], 0.0), writes=["ident32"])
        kb.op("pool", lambda: nc.gpsimd.affine_select(out=ident32[:], in_=ident32[:], pattern=[[-1, 128]],
                                                      compare_op=ALU.not_equal, fill=1.0, base=0,
                                                      channel_multiplier=1), reads=["ident32"], writes=["ident32"])
        kb.op("dve", lambda: nc.vector.tensor_copy(identb[:], ident32[:]), reads=["ident32"], writes=["identb"])
        kb.op("dve", lambda: nc.vector.memset(ones32[:], 1.0), writes=["ones32"])
        for j in range(N_BISECT + 1):
            kb.op("dve", lambda j=j: nc.vector.memset(pow2[:, j:j + 1], 2.0 ** -(j + 1)), writes=["pow2"])
        for nm_, val in (("od512", 1.0 / 512), ("od1024", 1.0 / 1024)):
            t_ = SB(es, nm_, [128, 128], BF16)
            kb.op("dve", lambda t_=t_, val=val: nc.vector.memset(t_[:], val), writes=[nm_])
            onesdiv[nm_] = t_
        kb.dma("sp", vec[:], vecs, writes=["vec"])
        rb_flat = rel_bias.rearrange("b h -> (b h)")
        kb.dma("sp", rbb[:], bass.AP(rb_flat.tensor, rb_flat.offset, [[0, 128], [1, 256]]), writes=["rbb"])
        with ExitStack() as st:
            oh = SB(st, "oh", [128, 32, 2, 128], F32)
            acc = SB(st, "bacc", [128, 8, 2, 128], F32)
            tmp = SB(st, "btmp", [128, 8, 2, 128], F32)
            kb.dma("sp", oh[:], ohb, writes=["oh"])
            kb.op("dve", lambda: nc.vector.tensor_scalar(out=negc[:], in0=rbb[:, 15 * 8:16 * 8], scalar1=-1.0,
                                                         scalar2=None, op0=ALU.mult), reads=["rbb"], writes=["negc"])
            kb.op("dve", lambda: nc.vector.memset(acc[:], 0.0), writes=["bacc"])
            for h in range(8):
                for b in range(32):
                    kb.op("dve", lambda h=h, b=b: nc.vector.scalar_tensor_tensor(
                        out=acc[:, h], in0=oh[:, b], scalar=rbb[:, b * 8 + h:b * 8 + h + 1], in1=acc[:, h],
                        op0=ALU.mult, op1=ALU.add), reads=["oh", "rbb", "bacc"], writes=["bacc"])
                kb.op("dve", lambda h=h: nc.vector.tensor_scalar(
                    out=acc[:, h], in0=acc[:, h], scalar1=negc[:, h:h + 1], scalar2=8.0, op0=ALU.add, op1=ALU.mult),
                    reads=["bacc", "negc"], writes=["bacc"])
            kb.op("dve", lambda: nc.vector.tensor_copy(bias_hi[:], acc[:]), reads=["bacc"], writes=["bias_hi"])
            kb.barrier()

        with ExitStack() as st:
            xt = [SB(st, f"x0_{i}", [128, S], F32) for i in range(2)]
            for c in range(8):
                kb.dma("sp", xt[c % 2][:], cm(xT_in)[:, c, :], writes=[f"x0_{c % 2}"])
                kb.dma("act", cm(xm)[:, c, :], xt[c % 2][:], reads=[f"x0_{c % 2}"])
            kb.barrier()

        def phase_proj(l):
            with ExitStack() as st:
                xb = SB(st, "xb", [128, 8, S], BF16)
                wb = [SB(st, f"wb{i}", [128, 8, 512], BF16) for i in range(2)]
                wkw = SB(st, "wkw", [128, 8, 72], BF16)
                stg = [SB(st, f"stg{i}", [128, S], BF16) for i in range(2)]
                vst = [SB(st, f"vst{i}", [128, 4, 512], BF16) for i in range(2)]
                wst = SB(st, "wst", [128, NT, 8], F32)
                sg = [SB(st, f"sg{i}", [128, 512], F32) for i in range(2)]
                pa = [PS(st, f"pa{i}", [128, 512], F32) for i in range(3)]
                pg = [PS(st, f"pg{i}", [128, 512], F32) for i in range(2)]
                pw = PS(st, "pw", [128, 8], F32)
                for c in range(8):
                    kb.dma("pool", xb[:, c, :], cm(xm)[:, c, :], writes=[("xb", c)])
                xbk = [("xb", c) for c in range(8)]
                wv = cm(w_in[l])
                nslot = [0]

                def loadw(col0, ncol=512):
                    i = nslot[0] % 2
                    nslot[0] += 1
                    kb.dma("pool", wb[i][:, :, 0:ncol], wv[:, :, col0:col0 + ncol], writes=[f"wb{i}"])
                    return i

                nev = [0]

                def evac(dst, src, reads, writes):
                    if nev[0] % 2 == 0:
                        kb.op("act", lambda: nc.scalar.copy(dst, src), reads=reads, writes=writes)
                    else:
                        kb.op("dve", lambda: nc.vector.tensor_copy(dst, src), reads=reads, writes=writes)
                    nev[0] += 1

                nst = [0]
                npa = [0]
                for (col0, dst) in ((OFF_Q, hq), (OFF_K, hk), (OFF_QI, hqi)):
                    wi = loadw(col0)
                    for blk in range(4):
                        si = nst[0] % 2
                        nst[0] += 1
                        for tt in range(TT):
                            p = npa[0] % 3
                            npa[0] += 1
                            for c in range(8):
                                kb.op("pe", lambda c=c, p=p, wi=wi, blk=blk, tt=tt: nc.tensor.matmul(
                                    pa[p][:, :], wb[wi][:, c, blk * 128:(blk + 1) * 128], xb[:, c, tt * 512:(tt + 1) * 512],
                                    start=(c == 0), stop=(c == 7)), reads=[f"wb{wi}", ("xb", c)], writes=[f"pa{p}"])
                            evac(stg[si][:, tt * 512:(tt + 1) * 512], pa[p][:, :], [f"pa{p}"], [(f"stg{si}", tt)])
                        kb.dma("sp", dst[blk * 128:(blk + 1) * 128, :], stg[si][:],
                               reads=[(f"stg{si}", tt) for tt in range(TT)], writes=[])
                        for tt in range(TT):
                            kb.readers.setdefault((f"stg{si}", tt), {}).update(kb.readers.get((f"stg{si}", 0), {}))
                wa = loadw(OFF_A)
                wg_ = loadw(OFF_G)
                for blk in range(4):
                    si = nst[0] % 2
                    nst[0] += 1
                    for tt in range(TT):
                        p = npa[0] % 3
                        npa[0] += 1
                        q = tt % 2
                        for c in range(8):
                            kb.op("pe", lambda c=c, p=p, blk=blk, tt=tt: nc.tensor.matmul(
                                pa[p][:, :], wb[wa][:, c, blk * 128:(blk + 1) * 128], xb[:, c, tt * 512:(tt + 1) * 512],
                                start=(c == 0), stop=(c == 7)), reads=[f"wb{wa}", ("xb", c)], writes=[f"pa{p}"])
                        for c in range(8):
                            kb.op("pe", lambda c=c, q=q, blk=blk, tt=tt: nc.tensor.matmul(
                                pg[q][:, :], wb[wg_][:, c, blk * 128:(blk + 1) * 128], xb[:, c, tt * 512:(tt + 1) * 512],
                                start=(c == 0), stop=(c == 7)), reads=[f"wb{wg_}", ("xb", c)], writes=[f"pg{q}"])
                        kb.op("act", lambda q=q: nc.scalar.activation(out=sg[q][:], in_=pg[q][:, :], func=AF.Sigmoid),
                              reads=[f"pg{q}"], writes=[f"sg{q}"])
                        kb.op("dve", lambda q=q, p=p, si=si, tt=tt: nc.vector.tensor_tensor(
                            out=stg[si][:, tt * 512:(tt + 1) * 512], in0=pa[p][:, :], in1=sg[q][:], op=ALU.mult),
                            reads=[f"pa{p}", f"sg{q}"], writes=[(f"stg{si}", tt)])
                    kb.dma("sp", hu[blk * 128:(blk + 1) * 128, :], stg[si][:],
                           reads=[(f"stg{si}", tt) for tt in range(TT)], writes=[])
                    for tt in range(TT):
                        kb.readers.setdefault((f"stg{si}", tt), {}).update(kb.readers.get((f"stg{si}", 0), {}))
                wvv = loadw(OFF_V)
                kb.dma("pool", wkw[:], wv[:, :, OFF_KI:OFF_KI + 72], writes=["wkw"])
                si = nst[0] % 2
                nst[0] += 1
                for tt in range(TT):
                    p = npa[0] % 3
                    npa[0] += 1
                    for c in range(8):
                        kb.op("pe", lambda c=c, p=p, tt=tt: nc.tensor.matmul(
                            pa[p][0:64, :], wkw[:, c, 0:64], xb[:, c, tt * 512:(tt + 1) * 512],
                            start=(c == 0), stop=(c == 7)), reads=["wkw", ("xb", c)], writes=[f"pa{p}"])
                    evac(stg[si][0:64, tt * 512:(tt + 1) * 512], pa[p][0:64, :], [f"pa{p}"], [(f"stg{si}", tt)])
                kb.dma("sp", hki[:, :], stg[si][0:64, :], reads=[(f"stg{si}", tt) for tt in range(TT)])
                for tt in range(TT):
                    kb.readers.setdefault((f"stg{si}", tt), {}).update(kb.readers.get((f"stg{si}", 0), {}))
                hv4 = hv.rearrange("(n p) c -> p n c", p=128)
                for i in range(NT):
                    p = npa[0] % 3
                    npa[0] += 1
                    vi = (i // 4) % 2
                    for c in range(8):
                        kb.op("pe", lambda c=c, p=p, i=i: nc.tensor.matmul(
                            pa[p][:, :], xb[:, c, i * 128:(i + 1) * 128], wb[wvv][:, c, :],
                            start=(c == 0), stop=(c == 7)), reads=[f"wb{wvv}", ("xb", c)], writes=[f"pa{p}"])
                    evac(vst[vi][:, i % 4, :], pa[p][:, :], [f"pa{p}"], [(f"vst{vi}", i % 4)])
                    for c in range(8):
                        kb.op("pe", lambda c=c, i=i: nc.tensor.matmul(
                            pw[:, :], xb[:, c, i * 128:(i + 1) * 128], wkw[:, c, 64:72],
                            start=(c == 0), stop=(c == 7)), reads=["wkw", ("xb", c)], writes=["pw"])
                    kb.op("dve", lambda i=i: nc.vector.tensor_copy(wst[:, i, :], pw[:, :]), reads=["pw"], writes=["wst"])
                    if i % 4 == 3:
                        g = i // 4
                        kb.dma("sp", hv4[:, 4 * g:4 * g + 4, :], vst[vi][:], reads=[(f"vst{vi}", k) for k in range(4)])
                        for k in range(1, 4):
                            kb.readers.setdefault((f"vst{vi}", k), {}).update(kb.readers.get((f"vst{vi}", 0), {}))
                kb.dma("sp", hw.rearrange("(n p) e -> p n e", p=128), wst[:], reads=["wst"])
                kb.barrier()

        def phase_attn(l):
            with ExitStack() as st:
                kT = SB(st, "kT", [128, 4, S], BF16)
                kiT = SB(st, "kiT", [128, S], BF16)
                va = SB(st, "va", [128, NT, 8, 65], BF16)
                qz = [SB(st, f"qz{i}", [128, 8, 512], BF16) for i in range(2)]
                qiz = [SB(st, f"qiz{i}", [128, 8, 128], BF16) for i in range(2)]
                wt = [SB(st, f"wt{i}", [128, 8], F32) for i in range(2)]
                sgn = [SB(st, f"sgn{i}", [128, 8], F32) for i in range(2)]
                absw = [SB(st, f"absw{i}", [128, 8], F32) for i in range(2)]
                dg = [SB(st, f"dg{i}", [128, 8, 128], BF16) for i in range(2)]
                sc = [SB(st, f"sc{i}", [128, S], F32) for i in range(3)]
                rl = [SB(st, f"rl{i}", [128, 512], BF16) for i in range(2)]
                junk = SB(st, "junk", [128, S], FP8)
                nm = SB(st, "nm", [128, S], BF16)
                nmTg = [SB(st, f"nmTg{i}", [128, NT, 512], FP8) for i in range(2)]
                bs = [SB(st, f"bs{i}", [128, 8], F32) for i in range(2)]
                hta = [SB(st, f"hta{i}", [128, N_BISECT + 1], F32) for i in range(2)]
                htb = [SB(st, f"htb{i}", [128, N_BISECT + 1], F32) for i in range(2)]
                pt = [SB(st, f"pt{i}", [128, 512], BF16) for i in range(3)]
                rec = SB(st, "rec", [128, 512], F32)
                ao = [SB(st, f"ao{i}", [128, 512], BF16) for i in range(2)]
                psD = [PS(st, f"psD{i}", [128, 512], F32) for i in range(2)]
                psS = PS(st, "psS", [128, 512], F32)
                ptr = PS(st, "ptr", [128, 4, 128], BF16)
                psL = [PS(st, f"psL{i}", [128, 512], F32) for i in range(2)]
                psO = PS(st, "psO", [128, 512], F32)
                psR = PS(st, "psR", [128, 512], F32)

                for c in range(4):
                    kb.dma("sp", kT[:, c, :], cm(hk)[:, c, :], writes=["kT"])
                kb.dma("sp", kiT[0:64, :], hki[:, :], writes=["kiT"])
                kb.dma("sp", kiT[64:128, :], hki[:, :], writes=["kiT"])
                kb.op("pool", lambda: nc.gpsimd.memset(va[:], 1.0), writes=["va"])
                for i_ in range(2):
                    kb.op("pool", lambda i_=i_: nc.gpsimd.memset(qz[i_][:], 0.0), writes=[f"qz{i_}"])
                    kb.op("pool", lambda i_=i_: nc.gpsimd.memset(qiz[i_][:], 0.0), writes=[f"qiz{i_}"])
                hv5 = hv.rearrange("(n p) (h d) -> p n h d", p=128, d=64)
                for n_ in range(NT):
                    kb.dma("sp", va[:, n_, :, 0:64], hv5[:, n_, :, :], writes=["va"])
                hw3 = hw.rearrange("(n p) e -> n p e", p=128)
                hma_v = hma.rearrange("h d t -> d h t")
                nrl = [0]
                npt = [0]
                nL = [0]

                def gen_score(i):
                    T0 = i * 128
                    L = T0 + 128
                    b = i % 2
                    b3 = i % 3
                    if i < 2:
                        return
                    kb.dma("sp", qiz[b][0:64, 0:8:2, :], cm(hqi)[0:64, :, T0:T0 + 128], writes=[f"qiz{b}"])
                    kb.dma("sp", qiz[b][64:128, 1:8:2, :], cm(hqi)[64:128, :, T0:T0 + 128], writes=[f"qiz{b}"])
                    kb.dma("sp", wt[b][:], hw3[i], writes=[f"wt{b}"])
                    kb.op("act", lambda: nc.scalar.activation(out=sgn[b][:], in_=wt[b][:], func=AF.Sign),
                          reads=[f"wt{b}"], writes=[f"sgn{b}"])
                    kb.op("dve", lambda: nc.vector.tensor_tensor(out=absw[b][:], in0=wt[b][:], in1=sgn[b][:], op=ALU.mult),
                          reads=[f"wt{b}", f"sgn{b}"], writes=[f"absw{b}"])
                    for h in range(8):
                        kb.op("dve", lambda h=h: nc.vector.tensor_scalar(
                            out=dg[b][:, h, :], in0=identb[:], scalar1=sgn[b][:, h:h + 1], scalar2=None,
                            op0=ALU.mult), reads=["identb", f"sgn{b}"], writes=[(f"dg{b}", h)])
                    yield
                    nsb = (L + 511) // 512
                    units = [(sbk, h) for sbk in range(nsb) for h in range(8)]
                    pend = None

                    def flush(pend):
                        sbk, h, r, wd, c0 = pend
                        kb.op("pe", lambda: nc.tensor.matmul(
                            psS[:, 0:wd], dg[b][:, h, :], rl[r][:, 0:wd], start=(h == 0), stop=(h == 7)),
                            reads=[(f"dg{b}", h), f"rl{r}"], writes=["psS"])
                        if h == 7:
                            kb.op("dve", lambda: nc.vector.tensor_copy(sc[b3][:, c0:c0 + wd], psS[:, 0:wd]),
                                  reads=["psS"], writes=[f"sc{b3}"])

                    for (sbk, h) in units:
                        c0 = sbk * 512
                        wd = min(512, L - c0)
                        hp = (h % 2) * 64
                        d = nrl[0] % 2
                        r = nrl[0] % 2
                        nrl[0] += 1
                        kb.op("pe", lambda: nc.tensor.matmul(
                            psD[d][:, 0:wd], qiz[b][:, h, :], kiT[:, c0:c0 + wd],
                            start=True, stop=True), reads=[f"qiz{b}", "kiT"], writes=[f"psD{d}"])
                        kb.op("act", lambda: nc.scalar.activation(
                            out=rl[r][:, 0:wd], in_=psD[d][:, 0:wd], func=AF.Relu, scale=absw[b][:, h:h + 1]),
                            reads=[f"psD{d}", f"absw{b}"], writes=[f"rl{r}"])
                        if pend is not None:
                            flush(pend)
                        pend = (sbk, h, r, wd, c0)
                        yield
                    flush(pend)
                    kb.op("dve", lambda: nc.vector.memset(sc[b3][0:64, L - 64:L], NEG), writes=[f"sc{b3}"])
                    yield

                def gen_bisect(i):
                    T0 = i * 128
                    L = T0 + 128
                    b3 = i % 3
                    g_, r_ = divmod(i, 4)
                    gb = g_ % 2
                    c_ = i % 2
                    bsk, htak, htbk = f"bs{c_}", f"hta{c_}", f"htb{c_}"
                    bs_, hta_, htb_ = bs[c_], hta[c_], htb[c_]
                    if i >= 2:
                        use_act = (i % 2 == 0)
                        sg_ = -1.0 if use_act else 1.0
                        kb.op("dve", lambda: nc.vector.tensor_reduce(out=bs_[:, 0:1], in_=sc[b3][:, 0:L], axis=AX.X, op=ALU.max),
                              reads=[f"sc{b3}"], writes=[bsk])
                        kb.op("dve", lambda: nc.vector.tensor_reduce(out=bs_[:, 1:2], in_=sc[b3][:, 0:L - 64], axis=AX.X, op=ALU.min),
                              reads=[f"sc{b3}", bsk], writes=[bsk])
                        kb.op("dve", lambda: nc.vector.tensor_tensor(out=bs_[:, 2:3], in0=bs_[:, 0:1], in1=bs_[:, 1:2], op=ALU.subtract),
                              reads=[bsk], writes=[bsk])
                        kb.op("dve", lambda: nc.vector.tensor_scalar(out=hta_[:], in0=pow2[:], scalar1=bs_[:, 2:3], scalar2=2.0 * sg_,
                                                                     op0=ALU.mult, op1=ALU.mult), reads=[bsk, "pow2"], writes=[htak])
                        kb.op("dve", lambda: nc.vector.tensor_scalar(out=htb_[:], in0=pow2[:], scalar1=bs_[:, 2:3], scalar2=-sg_,
                                                                     op0=ALU.mult, op1=ALU.mult), reads=[bsk, "pow2"], writes=[htbk])
                        kb.op("dve", lambda: nc.vector.scalar_tensor_tensor(out=bs_[:, 5:6], in0=bs_[:, 1:2], scalar=sg_, in1=htb_[:, 0:1],
                                                                            op0=ALU.mult, op1=ALU.subtract),
                              reads=[bsk, htbk], writes=[bsk])
                        yield
                        for it in range(N_BISECT):
                            if use_act:
                                kb.op("act", lambda: nc.scalar.activation(
                                    out=junk[:, 0:L], in_=sc[b3][:, 0:L], func=AF.Sign, bias=bs_[:, 5:6], scale=1.0,
                                    accum_out=bs_[:, 3:4], saturate=False), reads=[f"sc{b3}", bsk], writes=[bsk])
                                Kp = float(512 - L)
                            else:
                                kb.op("dve", lambda: nc.vector.tensor_scalar(
                                    out=junk[:, 0:L], in0=sc[b3][:, 0:L], scalar1=bs_[:, 5:6], scalar2=None, op0=ALU.is_ge,
                                    op1=ALU.add, accum_out=bs_[:, 3:4], saturate=False), reads=[f"sc{b3}", bsk], writes=[bsk])
                                Kp = 256.0
                            kb.op("pool", lambda it=it, Kp=Kp: nc.gpsimd.tensor_scalar(
                                out=bs_[:, 4:5], in0=bs_[:, 3:4], scalar1=Kp, scalar2=hta_[:, it + 1:it + 2], op0=ALU.is_ge, op1=ALU.mult),
                                reads=[bsk, htak], writes=[bsk])
                            kb.op("pool", lambda it=it: nc.gpsimd.tensor_scalar(
                                out=bs_[:, 5:6], in0=bs_[:, 5:6], scalar1=htb_[:, it + 1:it + 2], scalar2=bs_[:, 4:5], op0=ALU.add, op1=ALU.add),
                                reads=[bsk, htbk], writes=[bsk])
                            yield
                        kb.op("dve", lambda: nc.vector.tensor_scalar(out=bs_[:, 4:5], in0=bs_[:, 5:6], scalar1=sg_, scalar2=None, op0=ALU.mult),
                              reads=[bsk], writes=[bsk])
                        kb.op("dve", lambda: nc.vector.scalar_tensor_tensor(
                            out=bs_[:, 6:7], in0=bs_[:, 2:3], scalar=-(2.0 ** -(N_BISECT + 1)), in1=bs_[:, 4:5], op0=ALU.mult, op1=ALU.add),
                            reads=[bsk], writes=[bsk])
                        kb.op("dve", lambda: nc.vector.tensor_scalar(
                            out=nm[:, 0:L], in0=sc[b3][:, 0:L], scalar1=bs_[:, 6:7], scalar2=NEGM, op0=ALU.is_lt, op1=ALU.mult),
                            reads=[f"sc{b3}", bsk], writes=["nm"])
                    else:
                        kb.op("dve", lambda: nc.vector.memset(nm[:, 0:L], 0.0), writes=["nm"])
                        kb.op("dve", lambda: nc.vector.memset(nm[0:64, L - 64:L], NEGM), writes=["nm"])
                    yield
                    for j0 in range(0, i + 1, 4):
                        cnt = min(4, i + 1 - j0)
                        for jj in range(cnt):
                            j = j0 + jj
                            kb.op("pe", lambda j=j, jj=jj: nc.tensor.transpose(ptr[:, jj, :], nm[:, j * 128:(j + 1) * 128], identb[:]),
                                  reads=["nm", "identb"], writes=["ptr"])
                        kb.op("dve", lambda: nc.vector.tensor_copy(nmTg[gb][:, j0:j0 + cnt, r_ * 128:(r_ + 1) * 128], ptr[:, 0:cnt, :],
                                                                   saturate=False),
                              reads=["ptr"], writes=[(f"nmTg{gb}", r_)])
                        yield
                    if r_ < 3:
                        kb.op("pool", lambda: nc.gpsimd.memset(nmTg[gb][:, i + 1:4 * g_ + 4, r_ * 128:(r_ + 1) * 128], NEGM),
                              writes=[(f"nmTg{gb}", r_)])

                def gen_attend(g):
                    G0 = g * 512
                    gq = g % 2
                    gb = g % 2
                    nj = 4 * g + 4
                    mkeys = [(f"nmTg{gb}", r) for r in range(4)]
                    qkeys = [f"qz{gq}"]
                    kb.dma("sp", qz[gq][0:64, 0:8:2, :], cm(hq)[0:64, :, G0:G0 + 512], writes=[f"qz{gq}"])
                    kb.dma("sp", qz[gq][64:128, 1:8:2, :], cm(hq)[64:128, :, G0:G0 + 512], writes=[f"qz{gq}"])
                    pend = None
                    norm = None

                    def flush(pend):
                        h, j, p_ = pend
                        kb.op("pe", lambda: nc.tensor.matmul(
                            psO[0:65, :], va[:, j, h, :], pt[p_][:, :], start=(j == 0), stop=(j == nj - 1)),
                            reads=["va", f"pt{p_}"], writes=["psO"])

                    def normalise(h):
                        ab = h % 2
                        kb.op("dve", lambda: nc.vector.reciprocal(out=rec[64:65, :], in_=psO[64:65, :]),
                              reads=["psO"], writes=["rec"])
                        kb.op("pe", lambda: nc.tensor.matmul(psR[0:64, :], ones32[64:65, :], rec[64:65, :], start=True, stop=True),
                              reads=["ones32", "rec"], writes=["psR"])
                        kb.op("act", lambda: nc.scalar.copy(rec[0:64, :], psO[0:64, :]), reads=["psO"], writes=["num"])
                        kb.op("dve", lambda: nc.vector.scalar_tensor_tensor(
                            out=ao[ab][0:64, :], in0=rec[0:64, :], scalar=vec[0:64, l, 48 + h:48 + h + 1], in1=psR[0:64, :],
                            op0=ALU.mult, op1=ALU.mult), reads=["num", "psR", "vec"], writes=[f"ao{ab}"])
                        kb.dma("sp", hma[h, :, G0:G0 + 512], ao[ab][0:64, :], reads=[f"ao{ab}"])

                    for h in range(8):
                        hp = (h % 2) * 64
                        for j in range(nj):
                            Lb = nL[0] % 2
                            nL[0] += 1
                            p_ = npt[0] % 3
                            npt[0] += 1
                            nears = [(r, 4 * g + r - j) for r in range(4) if 0 <= 4 * g + r - j <= 1]
                            kb.op("pe", lambda: nc.tensor.matmul(
                                psL[Lb][:, :], identb[:], nmTg[gb][:, j, :], start=True, stop=False),
                                reads=["identb"] + mkeys, writes=[f"psL{Lb}"])
                            kb.op("pe", lambda: nc.tensor.matmul(
                                psL[Lb][:, :], kT[:, h // 2, j * 128:(j + 1) * 128],
                                qz[gq][:, h, :], start=False, stop=(len(nears) == 0)),
                                reads=["kT"] + qkeys, writes=[f"psL{Lb}"])
                            for ni, (r, o) in enumerate(nears):
                                kb.op("pe", lambda r=r, o=o, ni=ni: nc.tensor.matmul(
                                    psL[Lb][:, r * 128:(r + 1) * 128], identb[:], bias_hi[:, h, o, :], start=False,
                                    stop=(ni == len(nears) - 1)),
                                    reads=["identb", "bias_hi"], writes=[f"psL{Lb}"])
                            kb.op("act", lambda: nc.scalar.activation(
                                out=pt[p_][:, :], in_=psL[Lb][:, :], func=AF.Exp,
                                bias=rbb[:, 15 * 8 + h:15 * 8 + h + 1], scale=0.125),
                                reads=[f"psL{Lb}", "rbb"], writes=[f"pt{p_}"])
                            if pend is not None:
                                flush(pend)
                            if norm is not None:
                                normalise(norm)
                                norm = None
                            pend = (h, j, p_)
                            if j == nj - 1:
                                norm = h
                            yield
                    flush(pend)
                    normalise(norm)
                    yield

                def run_step(parts):
                    live = [[g, q] for g, q in parts]
                    done = [g is None for g, q in live]
                    used = [0] * len(live)
                    active = [i for i, (g, q) in enumerate(live) if g is not None]
                    while active:
                        nxt = []
                        for i in active:
                            g, q = live[i]
                            try:
                                next(g)
                                used[i] += 1
                                if q is None or used[i] < q:
                                    nxt.append(i)
                            except StopIteration:
                                done[i] = True
                        active = nxt
                    return [None if done[i] else live[i][0] for i in range(len(live))]

                run_step([[gen_score(0), None]])
                oldb = None
                att = None
                for k in range(NT + 8):
                    newb = gen_bisect(k) if k < NT else None
                    parts = [[gen_score(k + 1) if k + 1 < NT else None, None], [oldb, None], [newb, 12]]
                    if att is None and k >= 5 and (k - 5) % 4 == 0 and (k - 5) // 4 < NT // 4:
                        g = (k - 5) // 4
                        att = [gen_attend(g), 4, 8 * (4 * g + 4) + 8]
                    if att is not None:
                        quota = None if att[1] == 1 else (att[2] + 3) // 4
                        parts.append([att[0], quota])
                    if all(p[0] is None for p in parts):
                        continue
                    rem = run_step(parts)
                    oldb = rem[2]
                    if att is not None:
                        att[1] -= 1
                        if rem[3] is None or att[1] == 0:
                            att = None
                kb.barrier()

        def phase_conv(l):
            with ExitStack() as st:
                u = SB(st, "u", [128, 4, S + 32], BF16)
                cw = SB(st, "cw", [128, 4, CONV_K], F32)
                dgc = SB(st, "dgc", [128, 4, CONV_K, 128], BF16)
                y32 = SB(st, "y32", [128, 4, 512], F32)
                yb = SB(st, "yb", [128, 4, 512], BF16)
                ysq = SB(st, "ysq", [128, 4, 512], BF16)
                mean = SB(st, "cmean", [128, 512], F32)
                rstd = SB(st, "crstd", [128, 512], F32)
                t1 = SB(st, "ct1", [128, 4, 512], F32)
                so = [SB(st, f"cso{i}", [128, 4, 512], BF16) for i in range(2)]
                pc = [PS(st, f"pc{i}", [128, 512], F32) for i in range(2)]
                pm = PS(st, "pm", [128, 512], F32)
                pq = PS(st, "pq", [128, 512], F32)
                kb.op("pool", lambda: nc.gpsimd.memset(u[:, :, 0:32], 0.0), writes=["u"])
                kb.dma("sp", u[:, :, 32:], cm(hu), writes=["u"])
                kb.dma("sp", cw[:], conv_wT[l].rearrange("(c p) j -> p c j", p=128), writes=["cw"])
                for c in range(4):
                    for j in range(CONV_K):
                        kb.op("dve", lambda c=c, j=j: nc.vector.tensor_scalar(
                            out=dgc[:, c, j, :], in0=identb[:], scalar1=cw[:, c, j:j + 1], scalar2=None, op0=ALU.mult),
                            reads=["identb", "cw"], writes=[("dgc", c)])
                od = onesdiv["od512"]
                for tt in range(TT):
                    for c in range(4):
                        p = c % 2
                        for j in range(CONV_K):
                            off = 2 + tt * 512 + j
                            kb.op("pe", lambda c=c, j=j, p=p, off=off: nc.tensor.matmul(
                                pc[p][:, :], dgc[:, c, j, :], u[:, c, off:off + 512], start=(j == 0), stop=(j == CONV_K - 1)),
                                reads=[("dgc", c), "u"], writes=[f"pc{p}"])
                        kb.op("act", lambda c=c, p=p: nc.scalar.activation(out=y32[:, c, :], in_=pc[p][:, :], func=AF.Identity,
                                                                           bias=vec[:, l, c:c + 1], scale=1.0),
                              reads=[f"pc{p}", "vec"], writes=[("y32", c)])
                        kb.op("act", lambda c=c: nc.scalar.activation(out=ysq[:, c, :], in_=y32[:, c, :], func=AF.Square),
                              reads=[("y32", c)], writes=[("ysq", c)])
                        kb.op("dve", lambda c=c: nc.vector.tensor_copy(yb[:, c, :], y32[:, c, :]), reads=[("y32", c)],
                              writes=[("yb", c)])
                    for c in range(4):
                        kb.op("pe", lambda c=c: nc.tensor.matmul(pm[:, :], od[:], yb[:, c, :], start=(c == 0), stop=(c == 3)),
                              reads=["od512", ("yb", c)], writes=["pm"])
                    for c in range(4):
                        kb.op("pe", lambda c=c: nc.tensor.matmul(pq[:, :], od[:], ysq[:, c, :], start=(c == 0), stop=(c == 3)),
                              reads=["od512", ("ysq", c)], writes=["pq"])
                    ln_tail(mean, rstd, pm, pq, "c")
                    sb_ = tt % 2
                    kb.op("dve", lambda: nc.vector.tensor_tensor(out=t1[:], in0=y32[:], in1=bcast_mid(mean[:], 4), op=ALU.subtract),
                          reads=[("y32", c) for c in range(4)] + ["cmean"], writes=["ct1"])
                    kb.op("dve", lambda: nc.vector.tensor_tensor(out=t1[:], in0=t1[:], in1=bcast_mid(rstd[:], 4), op=ALU.mult),
                          reads=["ct1", "crstd"], writes=["ct1"])
                    for c in range(4):
                        kb.op("act", lambda c=c: nc.scalar.activation(out=t1[:, c, :], in_=t1[:, c, :], func=AF.Silu,
                                                                      bias=vec[:, l, 8 + c:9 + c], scale=vec[:, l, 4 + c:5 + c]),
                              reads=["ct1", "vec"], writes=["ct1"])
                        kb.op("dve", lambda c=c, sb_=sb_: nc.vector.tensor_scalar(
                            out=so[sb_][:, c, :], in0=t1[:, c, :], scalar1=vec[:, l, 12 + c:13 + c], scalar2=None, op0=ALU.mult),
                            reads=["ct1", "vec"], writes=[f"cso{sb_}"])
                    kb.dma("act", cm(hmc)[:, :, tt * 512:(tt + 1) * 512], so[sb_][:], reads=[f"cso{sb_}"])
                kb.barrier()

        def ln_tail(mean, rstd, pm, pq, tag):
            mk, rk = f"{tag}mean", f"{tag}rstd"
            kb.op("act", lambda: nc.scalar.copy(mean[:], pm[:, :]), reads=["pm"], writes=[mk])
            kb.op("dve", lambda: nc.vector.tensor_tensor(out=rstd[:], in0=mean[:], in1=mean[:], op=ALU.mult),
                  reads=[mk], writes=[rk])
            kb.op("dve", lambda: nc.vector.tensor_tensor(out=rstd[:], in0=pq[:, :], in1=rstd[:], op=ALU.subtract),
                  reads=["pq", rk], writes=[rk])
            kb.op("dve", lambda: nc.vector.tensor_scalar(out=rstd[:], in0=rstd[:], scalar1=LN_EPS, scalar2=None, op0=ALU.add),
                  reads=[rk], writes=[rk])
            kb.op("act", lambda: nc.scalar.activation(out=rstd[:], in_=rstd[:], func=AF.Sqrt), reads=[rk], writes=[rk])
            kb.op("dve", lambda: nc.vector.reciprocal(out=rstd[:], in_=rstd[:]), reads=[rk], writes=[rk])

        def phase_outproj(l):
            with ExitStack() as st:
                woA = SB(st, "woA", [128, 8, D], BF16)
                woC = SB(st, "woC", [128, 4, D], BF16)
                ma = [SB(st, f"ma{i}", [128, 8, 512], BF16) for i in range(2)]
                mc = [SB(st, f"mc{i}", [128, 4, 512], BF16) for i in range(2)]
                fo = [SB(st, f"fo{i}", [128, 8, 512], F32) for i in range(2)]
                pp = [PS(st, f"pp{i}", [128, 512], F32) for i in range(3)]
                kb.dma("pool", woA[0:64, :, :], w_out[l][0:512, :].rearrange("(h d) n -> d h n", d=64), writes=["woA"])
                kb.dma("pool", woC[:], w_out[l][512:1024, :].rearrange("(c p) n -> p c n", p=128), writes=["woC"])
                hma_v = hma.rearrange("h d t -> d h t")
                n = 0
                for tt in range(TT):
                    b = tt % 2
                    kb.dma("sp", ma[b][0:64, :, :], hma_v[:, :, tt * 512:(tt + 1) * 512], writes=[f"ma{b}"])
                    kb.dma("sp", mc[b][:], cm(hmc)[:, :, tt * 512:(tt + 1) * 512], writes=[f"mc{b}"])
                    for dc in range(8):
                        p = n % 3
                        n += 1
                        for h in range(8):
                            kb.op("pe", lambda h=h, dc=dc, p=p, b=b: nc.tensor.matmul(
                                pp[p][:, :], woA[0:64, h, dc * 128:(dc + 1) * 128], ma[b][0:64, h, :], start=(h == 0), stop=False),
                                reads=["woA", f"ma{b}"], writes=[f"pp{p}"])
                        for c in range(4):
                            kb.op("pe", lambda c=c, dc=dc, p=p, b=b: nc.tensor.matmul(
                                pp[p][:, :], woC[:, c, dc * 128:(dc + 1) * 128], mc[b][:, c, :], start=False, stop=(c == 3)),
                                reads=["woC", f"mc{b}"], writes=[f"pp{p}"])
                        if dc % 2 == 0:
                            kb.op("act", lambda dc=dc, p=p, b=b: nc.scalar.copy(fo[b][:, dc, :], pp[p][:, :]),
                                  reads=[f"pp{p}"], writes=[(f"fo{b}", dc)])
                        else:
                            kb.op("dve", lambda dc=dc, p=p, b=b: nc.vector.tensor_copy(fo[b][:, dc, :], pp[p][:, :]),
                                  reads=[f"pp{p}"], writes=[(f"fo{b}", dc)])
                    kb.dma("act", cm(fsc)[:, :, tt * 512:(tt + 1) * 512], fo[b][:], reads=[(f"fo{b}", dc) for dc in range(8)])
                    for dc in range(1, 8):
                        kb.readers.setdefault((f"fo{b}", dc), {}).update(kb.readers.get((f"fo{b}", 0), {}))
                kb.barrier()

        def phase_ln(l, which, dst, router=None):
            gcol = 16 + 16 * which
            with ExitStack() as st:
                x32 = [SB(st, f"lx{i}", [128, 8, 512], F32) for i in range(2)]
                f32_ = [SB(st, f"lf{i}", [128, 8, 512], F32) for i in range(2)]
                zb = SB(st, "zb", [128, 8, 512], BF16)
                zsq = SB(st, "zsq", [128, 8, 512], BF16)
                mean = SB(st, "lmean", [128, 512], F32)
                rstd = SB(st, "lrstd", [128, 512], F32)
                xo = [SB(st, f"lo{i}", [128, 8, 512], F32) for i in range(2)]
                pm = PS(st, "pm", [128, 512], F32)
                pq = PS(st, "pq", [128, 512], F32)
                od = onesdiv["od1024"]
                if router is not None:
                    rw = SB(st, "rw", [128, 8, NE], F32)
                    lg = SB(st, "lg", [128, NE], F32)
                    mx8 = SB(st, "mx8", [128, 8], F32)
                    ex = SB(st, "ex", [128, NE], F32)
                    gs = SB(st, "gs", [128, 4], F32)
                    gt = SB(st, "gt", [128, NE], F32)
                    gT = [SB(st, f"gT{i}", [8, 512], F32) for i in range(2)]
                    pr = PS(st, "pr", [128, NE], F32)
                    pgt = PS(st, "pgt", [8, 512], F32)
                    kb.dma("sp", rw[:], router.rearrange("(c p) e -> p c e", p=128), writes=["rw"])
                for tt in range(TT):
                    b = tt % 2
                    ts_ = slice(tt * 512, (tt + 1) * 512)
                    kb.dma("sp", x32[b][:], cm(xm)[:, :, ts_], writes=[f"lx{b}"])
                    kb.dma("sp", f32_[b][:], cm(fsc)[:, :, ts_], writes=[f"lf{b}"])
                    kb.op("dve", lambda b=b: nc.vector.scalar_tensor_tensor(
                        out=x32[b][:], in0=x32[b][:], scalar=DN_ALPHA, in1=f32_[b][:], op0=ALU.mult, op1=ALU.add),
                        reads=[f"lx{b}", f"lf{b}"], writes=[f"lx{b}"])
                    kb.op("act", lambda b=b: nc.scalar.copy(zb[:], x32[b][:]), reads=[f"lx{b}"], writes=["zb"])
                    kb.op("act", lambda b=b: nc.scalar.activation(out=zsq[:], in_=x32[b][:], func=AF.Square),
                          reads=[f"lx{b}"], writes=["zsq"])
                    for c in range(8):
                        kb.op("pe", lambda c=c: nc.tensor.matmul(pm[:, :], od[:], zb[:, c, :], start=(c == 0), stop=(c == 7)),
                              reads=["od1024", "zb"], writes=["pm"])
                    for c in range(8):
                        kb.op("pe", lambda c=c: nc.tensor.matmul(pq[:, :], od[:], zsq[:, c, :], start=(c == 0), stop=(c == 7)),
                              reads=["od1024", "zsq"], writes=["pq"])
                    ln_tail(mean, rstd, pm, pq, "l")
                    kb.op("dve", lambda b=b: nc.vector.tensor_tensor(out=x32[b][:], in0=x32[b][:], in1=bcast_mid(mean[:], 8),
                                                                     op=ALU.subtract),
                          reads=[f"lx{b}", "lmean"], writes=[f"lx{b}"])
                    kb.op("dve", lambda b=b: nc.vector.tensor_tensor(out=x32[b][:], in0=x32[b][:], in1=bcast_mid(rstd[:], 8),
                                                                     op=ALU.mult),
                          reads=[f"lx{b}", "lrstd"], writes=[f"lx{b}"])
                    for c in range(8):
                        kb.op("act", lambda c=c, b=b: nc.scalar.activation(
                            out=xo[b][:, c, :], in_=x32[b][:, c, :], func=AF.Identity,
                            bias=vec[:, l, gcol + 8 + c:gcol + 9 + c], scale=vec[:, l, gcol + c:gcol + c + 1]),
                            reads=[f"lx{b}", "vec"], writes=[(f"lo{b}", c)])
                    kb.dma("act", cm(dst)[:, :, ts_], xo[b][:], reads=[(f"lo{b}", c) for c in range(8)])
                    for c in range(1, 8):
                        kb.readers.setdefault((f"lo{b}", c), {}).update(kb.readers.get((f"lo{b}", 0), {}))
                    if router is not None:
                        for sub in range(4):
                            for c in range(8):
                                kb.op("pe", lambda c=c, b=b, sub=sub: nc.tensor.matmul(
                                    pr[:, :], xo[b][:, c, sub * 128:(sub + 1) * 128], rw[:, c, :], start=(c == 0), stop=(c == 7)),
                                    reads=[(f"lo{b}", c), "rw"], writes=["pr"])
                            kb.op("dve", lambda: nc.vector.tensor_copy(lg[:], pr[:, :]), reads=["pr"], writes=["lg"])
                            kb.op("dve", lambda: nc.vector.max(out=mx8[:], in_=lg[:]), reads=["lg"], writes=["mx8"])
                            kb.op("dve", lambda: nc.vector.tensor_scalar(out=gs[:, 0:1], in0=mx8[:, 0:1], scalar1=-1.0, scalar2=None,
                                                                         op0=ALU.mult), reads=["mx8"], writes=["gs"])
                            kb.op("act", lambda: nc.scalar.activation(out=ex[:], in_=lg[:], func=AF.Exp, bias=gs[:, 0:1], scale=1.0),
                                  reads=["lg", "gs"], writes=["ex"])
                            kb.op("dve", lambda: nc.vector.scalar_tensor_tensor(
                                out=ex[:], in0=lg[:], scalar=mx8[:, 1:2], in1=ex[:], op0=ALU.is_ge, op1=ALU.mult),
                                reads=["lg", "mx8", "ex"], writes=["ex"])
                            kb.op("dve", lambda: nc.vector.tensor_reduce(out=gs[:, 1:2], in_=ex[:], axis=AX.X, op=ALU.add),
                                  reads=["ex", "gs"], writes=["gs"])
                            kb.op("dve", lambda: nc.vector.reciprocal(out=gs[:, 2:3], in_=gs[:, 1:2]), reads=["gs"], writes=["gs"])
                            kb.op("dve", lambda: nc.vector.tensor_scalar(out=gt[:], in0=ex[:], scalar1=gs[:, 2:3], scalar2=None,
                                                                         op0=ALU.mult), reads=["ex", "gs"], writes=["gt"])
                            kb.op("pe", lambda sub=sub: nc.tensor.transpose(pgt[0:8, sub * 128:(sub + 1) * 128], gt[:, :], ident32[:]),
                                  reads=["gt", "ident32"], writes=["pgt"])
                        kb.op("dve", lambda b=b: nc.vector.tensor_copy(gT[b][:], pgt[:, :]), reads=["pgt"], writes=[f"gT{b}"])
                        kb.dma("act", hg[:, ts_], gT[b][:], reads=[f"gT{b}"])
                kb.barrier()

        def phase_ffn(l):
            moe = (l % 2 == 1)
            m = l // 2
            HW = D_FFE if moe else D_FF
            nexp = NE if moe else 1
            groups = [(c0, min(512, HW - c0)) for c0 in range(0, HW, 512)]
            HT = 2048
            with ExitStack() as st:
                xb = SB(st, "fxb", [128, 8, HT], BF16)
                yacc = SB(st, "yacc", [128, 8, HT], F32)
                wg = [SB(st, f"fwg{i}", [128, 8, 512], BF16) for i in range(2)]
                wu = [SB(st, f"fwu{i}", [128, 8, 512], BF16) for i in range(2)]
                wd = [SB(st, f"fwd{i}", [128, 4, D], BF16) for i in range(2)]
                sgm = [SB(st, f"fsg{i}", [128, 512], BF16) for i in range(2)]
                hT = [SB(st, f"fhT{i}", [128, 4, 512], BF16) for i in range(2)]
                htmp = [SB(st, f"fht{i}", [128, 512], F32) for i in range(2)]
                pA = [PS(st, f"pA{i}", [128, 512], F32) for i in range(2)]
                pB = [PS(st, f"pB{i}", [128, 512], F32) for i in range(2)]
                pY = [PS(st, f"pY{i}", [128, 512], F32) for i in range(2)]
                if moe:
                    sel8 = SB(st, "sel8", [8, 8, 128], F32)
                    kb.op("pool", lambda: nc.gpsimd.memset(sel8[:], 0.0), writes=["sel8"])
                    kb.op("pool", lambda: nc.gpsimd.affine_select(out=sel8[:], in_=sel8[:], pattern=[[-1, 8], [0, 128]],
                                                                  compare_op=ALU.not_equal, fill=1.0, base=0,
                                                                  channel_multiplier=1), reads=["sel8"], writes=["sel8"])
                    gTs = SB(st, "gTs", [8, HT], F32)
                    gbc = SB(st, "gbc", [128, 4, 512], BF16)
                    pG = PS(st, "pG", [128, 512], F32)
                nab = 0
                ny = 0
                nh = 0
                items = []
                for half in range(S // HT):
                    for e in range(nexp):
                        for gi, (c0, wdt) in enumerate(groups):
                            items.append((half, e, gi, c0, wdt))

                def issue_w(k):
                    half, e, gi, c0, wdt = items[k]
                    ws = k % 2
                    if moe:
                        Wg, Wu, Wd = moe_wg[m, e], moe_wu[m, e], moe_wd[m, e]
                    else:
                        Wg, Wu, Wd = ffn_wg[m], ffn_wu[m], ffn_wd[m]
                    nfc = wdt // 128
                    kb.dma("pool", wg[ws][:, :, 0:wdt], cm(Wg)[:, :, c0:c0 + wdt], writes=[f"fwg{ws}"])
                    kb.dma("pool", wu[ws][:, :, 0:wdt], cm(Wu)[:, :, c0:c0 + wdt], writes=[f"fwu{ws}"])
                    kb.dma("pool", wd[ws][:, 0:nfc, :], Wd[c0:c0 + wdt, :].rearrange("(c p) n -> p c n", p=128),
                           writes=[f"fwd{ws}"])

                issue_w(0)
                for k, (half, e, gi, c0, wdt) in enumerate(items):
                    hs = slice(half * HT, (half + 1) * HT)
                    ws = k % 2
                    nfc = wdt // 128
                    if e == 0 and gi == 0:
                        for c in range(8):
                            kb.dma("pool", xb[:, c, :], cm(xm)[:, c, hs], writes=[("fxb", c)])
                        if moe:
                            kb.dma("sp", gTs[:], hg[:, hs], writes=["gTs"])
                    if k + 1 < len(items):
                        issue_w(k + 1)
                    first = (e == 0 and gi == 0)
                    if moe and gi == 0:
                        for tt in range(4):
                            kb.op("pe", lambda e=e, tt=tt: nc.tensor.matmul(
                                pG[:, :], sel8[0:8, e, :], gTs[0:8, tt * 512:(tt + 1) * 512], start=True, stop=True),
                                reads=["sel8", "gTs"], writes=["pG"])
                            kb.op("act", lambda tt=tt: nc.scalar.copy(gbc[:, tt, :], pG[:, :]), reads=["pG"], writes=[("gbc", tt)])
                    for tt in range(4):
                        tsl = slice(tt * 512, (tt + 1) * 512)
                        hb = nh % 2
                        nh += 1
                        for fc in range(nfc):
                            a = nab % 2
                            nab += 1
                            for c in range(8):
                                kb.op("pe", lambda c=c, a=a, ws=ws, fc=fc, tsl=tsl: nc.tensor.matmul(
                                    pA[a][:, :], wg[ws][:, c, fc * 128:(fc + 1) * 128], xb[:, c, tsl],
                                    start=(c == 0), stop=(c == 7)), reads=[f"fwg{ws}", ("fxb", c)], writes=[f"pA{a}"])
                            for c in range(8):
                                kb.op("pe", lambda c=c, a=a, ws=ws, fc=fc, tsl=tsl: nc.tensor.matmul(
                                    pB[a][:, :], wu[ws][:, c, fc * 128:(fc + 1) * 128], xb[:, c, tsl],
                                    start=(c == 0), stop=(c == 7)), reads=[f"fwu{ws}", ("fxb", c)], writes=[f"pB{a}"])
                            kb.op("act", lambda a=a: nc.scalar.activation(out=sgm[a][:], in_=pA[a][:, :], func=AF.Silu),
                                  reads=[f"pA{a}"], writes=[f"fsg{a}"])
                            if moe:
                                kb.op("dve", lambda a=a: nc.vector.tensor_tensor(out=htmp[a][:], in0=pB[a][:, :], in1=sgm[a][:],
                                                                                 op=ALU.mult),
                                      reads=[f"pB{a}", f"fsg{a}"], writes=[f"fht{a}"])
                                kb.op("dve", lambda a=a, hb=hb, fc=fc, tt=tt: nc.vector.tensor_tensor(
                                    out=hT[hb][:, fc, :], in0=htmp[a][:], in1=gbc[:, tt, :], op=ALU.mult),
                                    reads=[f"fht{a}", ("gbc", tt)], writes=[(f"fhT{hb}", fc)])
                            else:
                                kb.op("dve", lambda a=a, hb=hb, fc=fc: nc.vector.tensor_tensor(
                                    out=hT[hb][:, fc, :], in0=pB[a][:, :], in1=sgm[a][:], op=ALU.mult),
                                    reads=[f"pB{a}", f"fsg{a}"], writes=[(f"fhT{hb}", fc)])
                        for dc in range(8):
                            y = ny % 2
                            ny += 1
                            for fc in range(nfc):
                                kb.op("pe", lambda fc=fc, dc=dc, y=y, ws=ws, hb=hb: nc.tensor.matmul(
                                    pY[y][:, :], wd[ws][:, fc, dc * 128:(dc + 1) * 128], hT[hb][:, fc, :],
                                    start=(fc == 0), stop=(fc == nfc - 1)),
                                    reads=[f"fwd{ws}", (f"fhT{hb}", fc)], writes=[f"pY{y}"])
                            if first:
                                kb.op("dve", lambda dc=dc, y=y, tsl=tsl: nc.vector.tensor_copy(yacc[:, dc, tsl], pY[y][:, :]),
                                      reads=[f"pY{y}"], writes=[("yacc", dc, tsl.start)])
                            else:
                                kb.op("dve", lambda dc=dc, y=y, tsl=tsl: nc.vector.tensor_tensor(
                                    out=yacc[:, dc, tsl], in0=pY[y][:, :], in1=yacc[:, dc, tsl], op=ALU.add),
                                    reads=[f"pY{y}", ("yacc", dc, tsl.start)], writes=[("yacc", dc, tsl.start)])
                    if e == nexp - 1 and gi == len(groups) - 1:
                        kb.dma("act", cm(fsc)[:, :, hs], yacc[:],
                               reads=[("yacc", dc, t0) for dc in range(8) for t0 in range(0, HT, 512)])
                kb.barrier()

        stages = []
        for l in range(n_layers):
            last = (l == n_layers - 1)
            stages += [("proj", lambda l=l: phase_proj(l)), ("attn", lambda l=l: phase_attn(l)),
                       ("conv", lambda l=l: phase_conv(l)), ("outproj", lambda l=l: phase_outproj(l)),
                       ("ln1", lambda l=l: phase_ln(l, 0, xm, router=(moe_r[l // 2] if l % 2 == 1 else None))),
                       ("ffn", lambda l=l: phase_ffn(l)),
                       ("ln2", lambda l=l, last=last: phase_ln(l, 1, outT if last else xm))]
        for idx, (name, fn) in enumerate(stages):
            fn()
            if stop_after is not None and (idx + 1) >= stop_after:
                break
        kb.barrier()
        build_program.n_inst = kb.n_inst
    return nc


def _t5_bucket_np(rel):
    nb = 16
    max_exact = 8
    ret = np.where(rel > 0, nb, 0).astype(np.int32)
    n = np.abs(rel)
    nf = np.maximum(n, 1).astype(np.float32)
    large = max_exact + (np.log(nf / max_exact) / math.log(128 / max_exact) * (nb - max_exact)).astype(np.int32)
    large = np.minimum(large, nb - 1)
    return ret + np.where(n < max_exact, n, large)


def _const_onehot():
    s = np.arange(128)[:, None]
    t = np.arange(128)[None, :]
    oh = np.zeros((128, 32, 2, 128), np.float32)
    for o in range(2):
        rel = (s - 128 * o) - t
        bk = _t5_bucket_np(rel)
        for b in range(32):
            oh[:, b, o, :] = (bk == b)
    return oh


def _pack_vecs(inp):
    f = lambda a, n: np.asarray(a, np.float32).reshape(DEPTH, n, 128).transpose(2, 0, 1)
    v = np.zeros((128, DEPTH, NV), np.float32)
    v[:, :, 0:4] = f(inp["conv_b"], 4)
    v[:, :, 4:8] = f(inp["conv_ln_g"], 4)
    v[:, :, 8:12] = f(inp["conv_ln_b"], 4)
    ms = np.asarray(inp["mix_scale"], np.float32)
    v[:, :, 12:16] = f(ms[:, 512:], 4)
    v[:, :, 16:24] = f(inp["ln1_g"], 8)
    v[:, :, 24:32] = f(inp["ln1_b"], 8)
    v[:, :, 32:40] = f(inp["ln2_g"], 8)
    v[:, :, 40:48] = f(inp["ln2_b"], 8)
    v[0:64, :, 48:56] = ms[:, :512].reshape(DEPTH, 8, 64).transpose(2, 0, 1)
    return v


def make_in_maps(inputs, n_cores=8):
    shared = {
        "w_in": np.ascontiguousarray(inputs["w_in"], np.float32),
        "conv_wT": np.ascontiguousarray(np.asarray(inputs["conv_w"], np.float32).transpose(0, 2, 1)),
        "vecs": _pack_vecs(inputs),
        "rel_bias": np.ascontiguousarray(inputs["rel_bias"], np.float32),
        "ohb": _const_onehot(),
        "w_out": np.ascontiguousarray(inputs["w_out"], np.float32),
        "ffn_w_gate": np.ascontiguousarray(inputs["ffn_w_gate"], np.float32),
        "ffn_w_up": np.ascontiguousarray(inputs["ffn_w_up"], np.float32),
        "ffn_w_down": np.ascontiguousarray(inputs["ffn_w_down"], np.float32),
        "moe_router": np.ascontiguousarray(inputs["moe_router"], np.float32),
        "moe_w_gate": np.ascontiguousarray(inputs["moe_w_gate"], np.float32),
        "moe_w_up": np.ascontiguousarray(inputs["moe_w_up"], np.float32),
        "moe_w_down": np.ascontiguousarray(inputs["moe_w_down"], np.float32),
    }
    x = np.asarray(inputs["x"], np.float32)
    maps = []
    for b in range(n_cores):
        m = dict(shared)
        m["xT"] = np.ascontiguousarray(x[b].T)
        maps.append(m)
    return maps


def kernel(**inputs):
    nc = build_program()
    in_maps = make_in_maps(inputs, 8)
    res = run_bass_kernel_spmd(nc, in_maps, core_ids=list(range(8)))
    out = np.stack([np.ascontiguousarray(r["outT"].T) for r in res.results], axis=0)
    return out.astype(np.float32)
```

```python
import math
from contextlib import ExitStack

import numpy as np
import concourse.bass as bass
import concourse.mybir as mybir
from concourse.bass_utils import run_bass_kernel_spmd

F32 = mybir.dt.float32
BF16 = mybir.dt.bfloat16
FP8 = mybir.dt.float8e5
AF = mybir.ActivationFunctionType
ALU = mybir.AluOpType
AX = mybir.AxisListType

D = 1024
S = 4096
DEPTH = 4
NT = S // 128
TT = S // 512
IN_COLS = 3144
OFF_Q, OFF_K, OFF_V, OFF_QI, OFF_KI, OFF_WI, OFF_A, OFF_G = 0, 512, 1024, 1536, 2048, 2112, 2120, 2632
D_FF = 2816
D_FFE = 3584
NE = 8
CONV_K = 31
DN_ALPHA = (2.0 * DEPTH) ** 0.25
LN_EPS = 1e-5
NEGM = -30000.0
NEG = -1e30
N_BISECT = 16
NV = 56

EPOCH = 30000
DMA_RING = 8


class KB:
    def __init__(self, nc, es):
        self.nc = nc
        self.es = es
        self.eng = {"pe": nc.tensor, "act": nc.scalar, "dve": nc.vector, "pool": nc.gpsimd, "sp": nc.sync}
        self.count = {e: 0 for e in self.eng}
        self.dma_n = {}
        self.waited = {e: {} for e in self.eng}
        self.last_write = {}
        self.readers = {}
        self._semh = {}
        self.n_inst = 0

    def _sem(self, key):
        if key not in self._semh:
            name = "s_" + "_".join(str(k) for k in key)
            self._semh[key] = self.es.enter_context(self.nc.semaphore(name))
        return self._semh[key]

    def _wait(self, eng, token):
        key, val = token
        if self.waited[eng].get(key, 0) >= val:
            return
        self.waited[eng][key] = val
        self.eng[eng].wait_ge(self._sem(key), val)

    def _deps(self, eng, reads, writes):
        toks = []
        for b in reads:
            t = self.last_write.get(b)
            if t is not None:
                toks.append(t)
        for b in writes:
            t = self.last_write.get(b)
            if t is not None:
                toks.append(t)
            toks.extend(self.readers.get(b, {}).values())
        for t in toks:
            if eng == "pe" and t[0][0] == "c" and t[0][1] == "pe":
                continue
            self._wait(eng, t)

    def _record(self, token, reads, writes):
        for b in writes:
            self.last_write[b] = token
            self.readers[b] = {}
        for b in reads:
            self.readers.setdefault(b, {})[token[0]] = token

    def op(self, eng, ins_fn, reads=(), writes=()):
        self._deps(eng, reads, writes)
        ins = ins_fn()
        n = self.count[eng]
        self.count[eng] = n + 1
        key = ("c", eng, n // EPOCH)
        ins.then_inc(self._sem(key), 1)
        self._record((key, n % EPOCH + 1), reads, writes)
        self.n_inst += 1
        return ins

    def dma(self, q, out, in_, reads=(), writes=()):
        n = self.dma_n.get(q, 0)
        self.dma_n[q] = n + 1
        slot = n % DMA_RING
        key = ("d", q, slot)
        rnd = n // DMA_RING
        if rnd > 0:
            self._wait(q, (key, 16 * rnd))
        self._deps(q, reads, writes)
        ins = self.eng[q].dma_start(out=out, in_=in_)
        ins.then_inc(self._sem(key), 16)
        self._record((key, 16 * (rnd + 1)), reads, writes)
        self.n_inst += 1
        return ins

    def barrier(self):
        toks = []
        for e in self.eng:
            n = self.count[e]
            if n > 0:
                toks.append((("c", e, (n - 1) // EPOCH), (n - 1) % EPOCH + 1))
        for q, n in self.dma_n.items():
            for slot in range(min(n, DMA_RING)):
                toks.append((("d", q, slot), 16 * ((n - 1 - slot) // DMA_RING + 1)))
        for e in self.eng:
            for t in toks:
                self._wait(e, t)
        self.last_write = {}
        self.readers = {}


def bcast_mid(a, n):
    return bass.AP(a.tensor, a.offset, [list(a.ap[0]), [0, n], list(a.ap[1])])


def build_program(n_layers=DEPTH, debug=False, stop_after=None):
    nc = bass.Bass("TRN2", target_bir_lowering=False)
    dt_in = lambda name, shape: nc.dram_tensor(name, shape, F32, kind="ExternalInput").ap()
    xT_in = dt_in("xT", [D, S])
    w_in = dt_in("w_in", [DEPTH, D, IN_COLS])
    conv_wT = dt_in("conv_wT", [DEPTH, 512, CONV_K])
    vecs = dt_in("vecs", [128, DEPTH, NV])
    rel_bias = dt_in("rel_bias", [32, 8])
    ohb = dt_in("ohb", [128, 32, 2, 128])
    w_out = dt_in("w_out", [DEPTH, D, D])
    ffn_wg = dt_in("ffn_w_gate", [2, D, D_FF])
    ffn_wu = dt_in("ffn_w_up", [2, D, D_FF])
    ffn_wd = dt_in("ffn_w_down", [2, D_FF, D])
    moe_r = dt_in("moe_router", [2, D, NE])
    moe_wg = dt_in("moe_w_gate", [2, NE, D, D_FFE])
    moe_wu = dt_in("moe_w_up", [2, NE, D, D_FFE])
    moe_wd = dt_in("moe_w_down", [2, NE, D_FFE, D])
    outT = nc.dram_tensor("outT", [D, S], F32, kind="ExternalOutput").ap()

    skind = "ExternalOutput" if debug else "Internal"
    scr = lambda name, shape, dt: nc.dram_tensor(name, shape, dt, kind=skind).ap()
    xm = scr("xm", [D, S], F32)
    fsc = scr("fsc", [D, S], F32)
    hq = scr("hq", [512, S], BF16)
    hk = scr("hk", [512, S], BF16)
    hqi = scr("hqi", [512, S], BF16)
    hki = scr("hki", [64, S], BF16)
    hu = scr("hu", [512, S], BF16)
    hv = scr("hv", [S, 520], BF16)
    hw = scr("hw", [S, 8], F32)
    hma = scr("hma", [8, 64, S], BF16)
    hmc = scr("hmc", [512, S], BF16)
    hg = scr("hg", [8, S], F32)

    def cm(a):
        return a.rearrange("(c p) s -> p c s", p=128)

    with ExitStack() as es:
        kb = KB(nc, es)
        uid = [0]

        def SB(st, name, shape, dt):
            uid[0] += 1
            return st.enter_context(nc.sbuf_tensor(f"{name}_{uid[0]}", shape, dt))

        def PS(st, name, shape, dt):
            uid[0] += 1
            return st.enter_context(nc.psum_tensor(f"{name}_{uid[0]}", shape, dt))

        identb = SB(es, "identb", [128, 128], BF16)
        ident32 = SB(es, "ident32", [128, 128], F32)
        onesdiv = {}
        ones32 = SB(es, "ones32", [128, 64], F32)
        vec = SB(es, "vec", [128, DEPTH, NV], F32)
        rbb = SB(es, "rbb", [128, 256], F32)
        pow2 = SB(es, "pow2", [128, N_BISECT + 1], F32)
        bias_hi = SB(es, "bias_hi", [128, 8, 2, 128], BF16)
        negc = SB(es, "negc", [128, 8], F32)

        kb.op("pool", lambda: nc.gpsimd.memset(ident32[:], 0.0), writes=["ident32"])
        kb.op("pool", lambda: nc.gpsimd.affine_select(out=ident32[:], in_=ident32[:], pattern=[[-1, 128]],
                                                      compare_op=ALU.not_equal, fill=1.0, base=0,
                                                      channel_multiplier=1), reads=["ident32"], writes=["ident32"])
        kb.op("dve", lambda: nc.vector.tensor_copy(identb[:], ident32[:]), reads=["ident32"], writes=["identb"])
        kb.op("dve", lambda: nc.vector.memset(ones32[:], 1.0), writes=["ones32"])
        for j in range(N_BISECT + 1):
            kb.op("dve", lambda j=j: nc.vector.memset(pow2[:, j:j + 1], 2.0 ** -(j + 1)), writes=["pow2"])
        for nm_, val in (("od512", 1.0 / 512), ("od1024", 1.0 / 1024)):
            t_ = SB(es, nm_, [128, 128], BF16)
            kb.op("dve", lambda t_=t_, val=val: nc.vector.memset(t_[:], val), writes=[nm_])
            onesdiv[nm_] = t_
        kb.dma("sp", vec[:], vecs, writes=["vec"])
        rb_flat = rel_bias.rearrange("b h -> (b h)")
        kb.dma("sp", rbb[:], bass.AP(rb_flat.tensor, rb_flat.offset, [[0, 128], [1, 256]]), writes=["rbb"])
        with ExitStack() as st:
            oh = SB(st, "oh", [128, 32, 2, 128], F32)
            acc = SB(st, "bacc", [128, 8, 2, 128], F32)
            tmp = SB(st, "btmp", [128, 8, 2, 128], F32)
            kb.dma("sp", oh[:], ohb, writes=["oh"])
            kb.op("dve", lambda: nc.vector.tensor_scalar(out=negc[:], in0=rbb[:, 15 * 8:16 * 8], scalar1=-1.0,
                                                         scalar2=None, op0=ALU.mult), reads=["rbb"], writes=["negc"])
            kb.op("dve", lambda: nc.vector.memset(acc[:], 0.0), writes=["bacc"])
            for h in range(8):
                for b in range(32):
                    kb.op("dve", lambda h=h, b=b: nc.vector.scalar_tensor_tensor(
                        out=acc[:, h], in0=oh[:, b], scalar=rbb[:, b * 8 + h:b * 8 + h + 1], in1=acc[:, h],
                        op0=ALU.mult, op1=ALU.add), reads=["oh", "rbb", "bacc"], writes=["bacc"])
                kb.op("dve", lambda h=h: nc.vector.tensor_scalar(
                    out=acc[:, h], in0=acc[:, h], scalar1=negc[:, h:h + 1], scalar2=8.0, op0=ALU.add, op1=ALU.mult),
                    reads=["bacc", "negc"], writes=["bacc"])
            kb.op("dve", lambda: nc.vector.tensor_copy(bias_hi[:], acc[:]), reads=["bacc"], writes=["bias_hi"])
            kb.barrier()

        with ExitStack() as st:
            xt = [SB(st, f"x0_{i}", [128, S], F32) for i in range(2)]
            for c in range(8):
                kb.dma("sp", xt[c % 2][:], cm(xT_in)[:, c, :], writes=[f"x0_{c % 2}"])
                kb.dma("act", cm(xm)[:, c, :], xt[c % 2][:], reads=[f"x0_{c % 2}"])
            kb.barrier()

        def phase_proj(l):
            with ExitStack() as st:
                xb = SB(st, "xb", [128, 8, S], BF16)
                wb = [SB(st, f"wb{i}", [128, 8, 512], BF16) for i in range(2)]
                wkw = SB(st, "wkw", [128, 8, 72], BF16)
                stg = [SB(st, f"stg{i}", [128, S], BF16) for i in range(2)]
                vst = [SB(st, f"vst{i}", [128, 4, 8, 65], BF16) for i in range(2)]
                wst = SB(st, "wst", [128, NT, 8], F32)
                sg = [SB(st, f"sg{i}", [128, 512], F32) for i in range(2)]
                pa = [PS(st, f"pa{i}", [128, 512], F32) for i in range(3)]
                pg = [PS(st, f"pg{i}", [128, 512], F32) for i in range(2)]
                pw = PS(st, "pw", [128, 8], F32)
                for c in range(8):
                    kb.dma("pool", xb[:, c, :], cm(xm)[:, c, :], writes=[("xb", c)])
                xbk = [("xb", c) for c in range(8)]
                wv = cm(w_in[l])
                nslot = [0]

                def loadw(col0, ncol=512):
                    i = nslot[0] % 2
                    nslot[0] += 1
                    kb.dma("pool", wb[i][:, :, 0:ncol], wv[:, :, col0:col0 + ncol], writes=[f"wb{i}"])
                    return i

                nev = [0]

                def evac(dst, src, reads, writes):
                    if nev[0] % 2 == 0:
                        kb.op("act", lambda: nc.scalar.copy(dst, src), reads=reads, writes=writes)
                    else:
                        kb.op("dve", lambda: nc.vector.tensor_copy(dst, src), reads=reads, writes=writes)
                    nev[0] += 1

                nst = [0]
                npa = [0]
                nxt_w = [loadw(OFF_Q)]
                order = [OFF_K, OFF_QI, OFF_A]
                for gi_, (col0, dst) in enumerate(((OFF_Q, hq), (OFF_K, hk), (OFF_QI, hqi))):
                    wi = nxt_w[0]
                    nxt_w[0] = loadw(order[gi_])
                    for blk in range(4):
                        si = nst[0] % 2
                        nst[0] += 1
                        for tt in range(TT):
                            p = npa[0] % 3
                            npa[0] += 1
                            for c in range(8):
                                kb.op("pe", lambda c=c, p=p, wi=wi, blk=blk, tt=tt: nc.tensor.matmul(
                                    pa[p][:, :], wb[wi][:, c, blk * 128:(blk + 1) * 128], xb[:, c, tt * 512:(tt + 1) * 512],
                                    start=(c == 0), stop=(c == 7)), reads=[f"wb{wi}", ("xb", c)], writes=[f"pa{p}"])
                            evac(stg[si][:, tt * 512:(tt + 1) * 512], pa[p][:, :], [f"pa{p}"], [(f"stg{si}", tt)])
                        kb.dma("sp", dst[blk * 128:(blk + 1) * 128, :], stg[si][:],
                               reads=[(f"stg{si}", tt) for tt in range(TT)], writes=[])
                        for tt in range(TT):
                            kb.readers.setdefault((f"stg{si}", tt), {}).update(kb.readers.get((f"stg{si}", 0), {}))
                wa = nxt_w[0]
                wg_ = loadw(OFF_G)
                for blk in range(4):
                    si = nst[0] % 2
                    nst[0] += 1
                    for tt in range(TT):
                        p = npa[0] % 3
                        npa[0] += 1
                        q = tt % 2
                        for c in range(8):
                            kb.op("pe", lambda c=c, p=p, blk=blk, tt=tt: nc.tensor.matmul(
                                pa[p][:, :], wb[wa][:, c, blk * 128:(blk + 1) * 128], xb[:, c, tt * 512:(tt + 1) * 512],
                                start=(c == 0), stop=(c == 7)), reads=[f"wb{wa}", ("xb", c)], writes=[f"pa{p}"])
                        for c in range(8):
                            kb.op("pe", lambda c=c, q=q, blk=blk, tt=tt: nc.tensor.matmul(
                                pg[q][:, :], wb[wg_][:, c, blk * 128:(blk + 1) * 128], xb[:, c, tt * 512:(tt + 1) * 512],
                                start=(c == 0), stop=(c == 7)), reads=[f"wb{wg_}", ("xb", c)], writes=[f"pg{q}"])
                        kb.op("act", lambda q=q: nc.scalar.activation(out=sg[q][:], in_=pg[q][:, :], func=AF.Sigmoid),
                              reads=[f"pg{q}"], writes=[f"sg{q}"])
                        kb.op("dve", lambda q=q, p=p, si=si, tt=tt: nc.vector.tensor_tensor(
                            out=stg[si][:, tt * 512:(tt + 1) * 512], in0=pa[p][:, :], in1=sg[q][:], op=ALU.mult),
                            reads=[f"pa{p}", f"sg{q}"], writes=[(f"stg{si}", tt)])
                    kb.dma("sp", hu[blk * 128:(blk + 1) * 128, :], stg[si][:],
                           reads=[(f"stg{si}", tt) for tt in range(TT)], writes=[])
                    for tt in range(TT):
                        kb.readers.setdefault((f"stg{si}", tt), {}).update(kb.readers.get((f"stg{si}", 0), {}))
                wvv = loadw(OFF_V)
                kb.dma("pool", wkw[:], wv[:, :, OFF_KI:OFF_KI + 72], writes=["wkw"])
                si = nst[0] % 2
                nst[0] += 1
                for tt in range(TT):
                    p = npa[0] % 3
                    npa[0] += 1
                    for c in range(8):
                        kb.op("pe", lambda c=c, p=p, tt=tt: nc.tensor.matmul(
                            pa[p][0:64, :], wkw[:, c, 0:64], xb[:, c, tt * 512:(tt + 1) * 512],
                            start=(c == 0), stop=(c == 7)), reads=["wkw", ("xb", c)], writes=[f"pa{p}"])
                    evac(stg[si][0:64, tt * 512:(tt + 1) * 512], pa[p][0:64, :], [f"pa{p}"], [(f"stg{si}", tt)])
                kb.dma("sp", hki[:, :], stg[si][0:64, :], reads=[(f"stg{si}", tt) for tt in range(TT)])
                for tt in range(TT):
                    kb.readers.setdefault((f"stg{si}", tt), {}).update(kb.readers.get((f"stg{si}", 0), {}))
                hv4 = hv.rearrange("(n p) c -> p n c", p=128)
                for i_ in range(2):
                    kb.op("pool", lambda i_=i_: nc.gpsimd.memset(vst[i_][:], 1.0), writes=[(f"vst{i_}", k_) for k_ in range(4)])
                for i in range(NT):
                    p = npa[0] % 3
                    npa[0] += 1
                    vi = (i // 4) % 2
                    for c in range(8):
                        kb.op("pe", lambda c=c, p=p, i=i: nc.tensor.matmul(
                            pa[p][:, :], xb[:, c, i * 128:(i + 1) * 128], wb[wvv][:, c, :],
                            start=(c == 0), stop=(c == 7)), reads=[f"wb{wvv}", ("xb", c)], writes=[f"pa{p}"])
                    evac(vst[vi][:, i % 4, :, 0:64], pa[p][:, :].rearrange("p (h d) -> p h d", d=64), [f"pa{p}"], [(f"vst{vi}", i % 4)])
                    for c in range(8):
                        kb.op("pe", lambda c=c, i=i: nc.tensor.matmul(
                            pw[:, :], xb[:, c, i * 128:(i + 1) * 128], wkw[:, c, 64:72],
                            start=(c == 0), stop=(c == 7)), reads=["wkw", ("xb", c)], writes=["pw"])
                    kb.op("dve", lambda i=i: nc.vector.tensor_copy(wst[:, i, :], pw[:, :]), reads=["pw"], writes=["wst"])
                    if i % 4 == 3:
                        g = i // 4
                        kb.dma("sp", hv4[:, 4 * g:4 * g + 4, :], vst[vi][:].rearrange("p n h d -> p n (h d)"),
                               reads=[(f"vst{vi}", k) for k in range(4)])
                        for k in range(1, 4):
                            kb.readers.setdefault((f"vst{vi}", k), {}).update(kb.readers.get((f"vst{vi}", 0), {}))
                kb.dma("sp", hw.rearrange("(n p) e -> p n e", p=128), wst[:], reads=["wst"])
                kb.barrier()

        def phase_attn(l):
            with ExitStack() as st:
                kT = SB(st, "kT", [128, 4, S], BF16)
                kiT = SB(st, "kiT", [128, S], BF16)
                va = SB(st, "va", [128, NT, 8, 65], BF16)
                qz = [SB(st, f"qz{i}", [128, 8, 512], BF16) for i in range(2)]
                qiz = [SB(st, f"qiz{i}", [128, 8, 128], BF16) for i in range(2)]
                wt = [SB(st, f"wt{i}", [128, 8], F32) for i in range(2)]
                sgn = [SB(st, f"sgn{i}", [128, 8], F32) for i in range(2)]
                absw = [SB(st, f"absw{i}", [128, 8], F32) for i in range(2)]
                dg = [SB(st, f"dg{i}", [128, 8, 128], BF16) for i in range(2)]
                sc = [SB(st, f"sc{i}", [128, S], F32) for i in range(3)]
                rl = [SB(st, f"rl{i}", [128, 512], BF16) for i in range(2)]
                junk = SB(st, "junk", [128, S], FP8)
                nm = SB(st, "nm", [128, S], BF16)
                nmTg = [SB(st, f"nmTg{i}", [128, NT, 512], FP8) for i in range(2)]
                bs = [SB(st, f"bs{i}", [128, 8], F32) for i in range(2)]
                hta = [SB(st, f"hta{i}", [128, N_BISECT + 1], F32) for i in range(2)]
                htb = [SB(st, f"htb{i}", [128, N_BISECT + 1], F32) for i in range(2)]
                pt = [SB(st, f"pt{i}", [128, 512], BF16) for i in range(3)]
                rec = SB(st, "rec", [128, 512], F32)
                ao = [SB(st, f"ao{i}", [128, 512], BF16) for i in range(2)]
                psD = [PS(st, f"psD{i}", [128, 512], F32) for i in range(2)]
                psS = PS(st, "psS", [128, 512], F32)
                ptr = PS(st, "ptr", [128, 4, 128], BF16)
                psL = [PS(st, f"psL{i}", [128, 512], F32) for i in range(2)]
                psO = PS(st, "psO", [128, 512], F32)
                psR = PS(st, "psR", [128, 512], F32)

                for c in range(4):
                    kb.dma("sp", kT[:, c, :], cm(hk)[:, c, :], writes=["kT"])
                kb.dma("sp", kiT[0:64, :], hki[:, :], writes=["kiT"])
                kb.dma("sp", kiT[64:128, :], hki[:, :], writes=["kiT"])

                for i_ in range(2):
                    kb.op("pool", lambda i_=i_: nc.gpsimd.memset(qz[i_][:], 0.0), writes=[f"qz{i_}"])
                    kb.op("pool", lambda i_=i_: nc.gpsimd.memset(qiz[i_][:], 0.0), writes=[f"qiz{i_}"])
                hv5 = hv.rearrange("(n p) c -> p n c", p=128)
                for g_ in range(4):
                    kb.dma("sp", va[:, 8 * g_:8 * g_ + 8, :, :].rearrange("p n h d -> p n (h d)"), hv5[:, 8 * g_:8 * g_ + 8, :], writes=["va"])
                hw3 = hw.rearrange("(n p) e -> n p e", p=128)
                hma_v = hma.rearrange("h d t -> d h t")
                nrl = [0]
                npt = [0]
                nL = [0]

                def gen_score(i):
                    T0 = i * 128
                    L = T0 + 128
                    b = i % 2
                    b3 = i % 3
                    if i < 2:
                        return
                    kb.dma("sp", qiz[b][0:64, 0:8:2, :], cm(hqi)[0:64, :, T0:T0 + 128], writes=[f"qiz{b}"])
                    kb.dma("sp", qiz[b][64:128, 1:8:2, :], cm(hqi)[64:128, :, T0:T0 + 128], writes=[f"qiz{b}"])
                    kb.dma("sp", wt[b][:], hw3[i], writes=[f"wt{b}"])
                    kb.op("act", lambda: nc.scalar.activation(out=sgn[b][:], in_=wt[b][:], func=AF.Sign),
                          reads=[f"wt{b}"], writes=[f"sgn{b}"])
                    kb.op("dve", lambda: nc.vector.tensor_tensor(out=absw[b][:], in0=wt[b][:], in1=sgn[b][:], op=ALU.mult),
                          reads=[f"wt{b}", f"sgn{b}"], writes=[f"absw{b}"])
                    for h in range(8):
                        kb.op("dve", lambda h=h: nc.vector.tensor_scalar(
                            out=dg[b][:, h, :], in0=identb[:], scalar1=sgn[b][:, h:h + 1], scalar2=None,
                            op0=ALU.mult), reads=["identb", f"sgn{b}"], writes=[(f"dg{b}", h)])
                    yield
                    nsb = (L + 511) // 512
                    units = [(sbk, h) for sbk in range(nsb) for h in range(8)]
                    pend = None

                    def flush(pend):
                        sbk, h, r, wd, c0 = pend
                        kb.op("pe", lambda: nc.tensor.matmul(
                            psS[:, 0:wd], dg[b][:, h, :], rl[r][:, 0:wd], start=(h == 0), stop=(h == 7)),
                            reads=[(f"dg{b}", h), f"rl{r}"], writes=["psS"])
                        if h == 7:
                            kb.op("dve", lambda: nc.vector.tensor_copy(sc[b3][:, c0:c0 + wd], psS[:, 0:wd]),
                                  reads=["psS"], writes=[f"sc{b3}"])

                    for (sbk, h) in units:
                        c0 = sbk * 512
                        wd = min(512, L - c0)
                        hp = (h % 2) * 64
                        d = nrl[0] % 2
                        r = nrl[0] % 2
                        nrl[0] += 1
                        kb.op("pe", lambda: nc.tensor.matmul(
                            psD[d][:, 0:wd], qiz[b][:, h, :], kiT[:, c0:c0 + wd],
                            start=True, stop=True), reads=[f"qiz{b}", "kiT"], writes=[f"psD{d}"])
                        if nrl[0] % 2 == 0:
                            kb.op("act", lambda: nc.scalar.activation(
                                out=rl[r][:, 0:wd], in_=psD[d][:, 0:wd], func=AF.Relu, scale=absw[b][:, h:h + 1]),
                                reads=[f"psD{d}", f"absw{b}"], writes=[f"rl{r}"])
                        else:
                            kb.op("dve", lambda: nc.vector.tensor_scalar(
                                out=rl[r][:, 0:wd], in0=psD[d][:, 0:wd], scalar1=absw[b][:, h:h + 1], scalar2=0.0,
                                op0=ALU.mult, op1=ALU.max), reads=[f"psD{d}", f"absw{b}"], writes=[f"rl{r}"])
                        if pend is not None:
                            flush(pend)
                        pend = (sbk, h, r, wd, c0)
                        yield
                    flush(pend)
                    kb.op("dve", lambda: nc.vector.memset(sc[b3][0:64, L - 64:L], NEG), writes=[f"sc{b3}"])
                    yield

                def gen_bisect(i):
                    T0 = i * 128
                    L = T0 + 128
                    b3 = i % 3
                    g_, r_ = divmod(i, 4)
                    gb = g_ % 2
                    c_ = i % 2
                    bsk, htak, htbk = f"bs{c_}", f"hta{c_}", f"htb{c_}"
                    bs_, hta_, htb_ = bs[c_], hta[c_], htb[c_]
                    if i >= 2:
                        use_act = (i % 2 == 0)
                        sg_ = -1.0 if use_act else 1.0
                        kb.op("dve", lambda: nc.vector.tensor_reduce(out=bs_[:, 0:1], in_=sc[b3][:, 0:L], axis=AX.X, op=ALU.max),
                              reads=[f"sc{b3}"], writes=[bsk])
                        kb.op("dve", lambda: nc.vector.tensor_reduce(out=bs_[:, 1:2], in_=sc[b3][:, 0:L - 64], axis=AX.X, op=ALU.min),
                              reads=[f"sc{b3}", bsk], writes=[bsk])
                        kb.op("dve", lambda: nc.vector.tensor_tensor(out=bs_[:, 2:3], in0=bs_[:, 0:1], in1=bs_[:, 1:2], op=ALU.subtract),
                              reads=[bsk], writes=[bsk])
                        kb.op("dve", lambda: nc.vector.tensor_scalar(out=hta_[:], in0=pow2[:], scalar1=bs_[:, 2:3], scalar2=2.0 * sg_,
                                                                     op0=ALU.mult, op1=ALU.mult), reads=[bsk, "pow2"], writes=[htak])
                        kb.op("dve", lambda: nc.vector.tensor_scalar(out=htb_[:], in0=pow2[:], scalar1=bs_[:, 2:3], scalar2=-sg_,
                                                                     op0=ALU.mult, op1=ALU.mult), reads=[bsk, "pow2"], writes=[htbk])
                        kb.op("dve", lambda: nc.vector.scalar_tensor_tensor(out=bs_[:, 5:6], in0=bs_[:, 1:2], scalar=sg_, in1=htb_[:, 0:1],
                                                                            op0=ALU.mult, op1=ALU.subtract),
                              reads=[bsk, htbk], writes=[bsk])
                        yield
                        for it in range(N_BISECT):
                            if use_act:
                                kb.op("act", lambda: nc.scalar.activation(
                                    out=junk[:, 0:L], in_=sc[b3][:, 0:L], func=AF.Sign, bias=bs_[:, 5:6], scale=1.0,
                                    accum_out=bs_[:, 3:4], saturate=False), reads=[f"sc{b3}", bsk], writes=[bsk])
                                Kp = float(512 - L)
                            else:
                                kb.op("dve", lambda: nc.vector.tensor_scalar(
                                    out=junk[:, 0:L], in0=sc[b3][:, 0:L], scalar1=bs_[:, 5:6], scalar2=None, op0=ALU.is_ge,
                                    op1=ALU.add, accum_out=bs_[:, 3:4], saturate=False), reads=[f"sc{b3}", bsk], writes=[bsk])
                                Kp = 256.0
                            kb.op("pool", lambda it=it, Kp=Kp: nc.gpsimd.tensor_scalar(
                                out=bs_[:, 4:5], in0=bs_[:, 3:4], scalar1=Kp, scalar2=hta_[:, it + 1:it + 2], op0=ALU.is_ge, op1=ALU.mult),
                                reads=[bsk, htak], writes=[bsk])
                            kb.op("pool", lambda it=it: nc.gpsimd.tensor_scalar(
                                out=bs_[:, 5:6], in0=bs_[:, 5:6], scalar1=htb_[:, it + 1:it + 2], scalar2=bs_[:, 4:5], op0=ALU.add, op1=ALU.add),
                                reads=[bsk, htbk], writes=[bsk])
                            yield
                        kb.op("dve", lambda: nc.vector.tensor_scalar(out=bs_[:, 4:5], in0=bs_[:, 5:6], scalar1=sg_, scalar2=None, op0=ALU.mult),
                              reads=[bsk], writes=[bsk])
                        kb.op("dve", lambda: nc.vector.scalar_tensor_tensor(
                            out=bs_[:, 6:7], in0=bs_[:, 2:3], scalar=-(2.0 ** -(N_BISECT + 1)), in1=bs_[:, 4:5], op0=ALU.mult, op1=ALU.add),
                            reads=[bsk], writes=[bsk])
                        kb.op("dve", lambda: nc.vector.tensor_scalar(
                            out=nm[:, 0:L], in0=sc[b3][:, 0:L], scalar1=bs_[:, 6:7], scalar2=NEGM, op0=ALU.is_lt, op1=ALU.mult),
                            reads=[f"sc{b3}", bsk], writes=["nm"])
                    else:
                        kb.op("dve", lambda: nc.vector.memset(nm[:, 0:L], 0.0), writes=["nm"])
                        kb.op("dve", lambda: nc.vector.memset(nm[0:64, L - 64:L], NEGM), writes=["nm"])
                    yield
                    for j0 in range(0, i + 1, 4):
                        cnt = min(4, i + 1 - j0)
                        for jj in range(cnt):
                            j = j0 + jj
                            kb.op("pe", lambda j=j, jj=jj: nc.tensor.transpose(ptr[:, jj, :], nm[:, j * 128:(j + 1) * 128], identb[:]),
                                  reads=["nm", "identb"], writes=["ptr"])
                        kb.op("dve", lambda: nc.vector.tensor_copy(nmTg[gb][:, j0:j0 + cnt, r_ * 128:(r_ + 1) * 128], ptr[:, 0:cnt, :],
                                                                   saturate=False),
                              reads=["ptr"], writes=[(f"nmTg{gb}", r_)])
                        yield
                    if r_ < 3:
                        kb.op("pool", lambda: nc.gpsimd.memset(nmTg[gb][:, i + 1:4 * g_ + 4, r_ * 128:(r_ + 1) * 128], NEGM),
                              writes=[(f"nmTg{gb}", r_)])

                def gen_attend(g):
                    G0 = g * 512
                    gq = g % 2
                    gb = g % 2
                    nj = 4 * g + 4
                    mkeys = [(f"nmTg{gb}", r) for r in range(4)]
                    qkeys = [f"qz{gq}"]
                    kb.dma("sp", qz[gq][0:64, 0:8:2, :], cm(hq)[0:64, :, G0:G0 + 512], writes=[f"qz{gq}"])
                    kb.dma("sp", qz[gq][64:128, 1:8:2, :], cm(hq)[64:128, :, G0:G0 + 512], writes=[f"qz{gq}"])
                    pend = None
                    norm = None

                    def flush(pend):
                        h, j, p_ = pend
                        kb.op("pe", lambda: nc.tensor.matmul(
                            psO[0:65, :], va[:, j, h, :], pt[p_][:, :], start=(j == 0), stop=(j == nj - 1)),
                            reads=["va", f"pt{p_}"], writes=["psO"])

                    def normalise(h):
                        ab = h % 2
                        kb.op("dve", lambda: nc.vector.reciprocal(out=rec[64:65, :], in_=psO[64:65, :]),
                              reads=["psO"], writes=["rec"])
                        kb.op("pe", lambda: nc.tensor.matmul(psR[0:64, :], ones32[64:65, :], rec[64:65, :], start=True, stop=True),
                              reads=["ones32", "rec"], writes=["psR"])
                        kb.op("act", lambda: nc.scalar.copy(rec[0:64, :], psO[0:64, :]), reads=["psO"], writes=["num"])
                        kb.op("dve", lambda: nc.vector.scalar_tensor_tensor(
                            out=ao[ab][0:64, :], in0=rec[0:64, :], scalar=vec[0:64, l, 48 + h:48 + h + 1], in1=psR[0:64, :],
                            op0=ALU.mult, op1=ALU.mult), reads=["num", "psR", "vec"], writes=[f"ao{ab}"])
                        kb.dma("sp", hma[h, :, G0:G0 + 512], ao[ab][0:64, :], reads=[f"ao{ab}"])

                    for h in range(8):
                        hp = (h % 2) * 64
                        for j in range(nj):
                            Lb = nL[0] % 2
                            nL[0] += 1
                            p_ = npt[0] % 3
                            npt[0] += 1
                            nears = [(r, 4 * g + r - j) for r in range(4) if 0 <= 4 * g + r - j <= 1]
                            kb.op("pe", lambda: nc.tensor.matmul(
                                psL[Lb][:, :], identb[:], nmTg[gb][:, j, :], start=True, stop=False),
                                reads=["identb"] + mkeys, writes=[f"psL{Lb}"])
                            kb.op("pe", lambda: nc.tensor.matmul(
                                psL[Lb][:, :], kT[:, h // 2, j * 128:(j + 1) * 128],
                                qz[gq][:, h, :], start=False, stop=(len(nears) == 0)),
                                reads=["kT"] + qkeys, writes=[f"psL{Lb}"])
                            for ni, (r, o) in enumerate(nears):
                                kb.op("pe", lambda r=r, o=o, ni=ni: nc.tensor.matmul(
                                    psL[Lb][:, r * 128:(r + 1) * 128], identb[:], bias_hi[:, h, o, :], start=False,
                                    stop=(ni == len(nears) - 1)),
                                    reads=["identb", "bias_hi"], writes=[f"psL{Lb}"])
                            kb.op("act", lambda: nc.scalar.activation(
                                out=pt[p_][:, :], in_=psL[Lb][:, :], func=AF.Exp,
                                bias=rbb[:, 15 * 8 + h:15 * 8 + h + 1], scale=0.125),
                                reads=[f"psL{Lb}", "rbb"], writes=[f"pt{p_}"])
                            if pend is not None:
                                flush(pend)
                            if norm is not None:
                                normalise(norm)
                                norm = None
                            pend = (h, j, p_)
                            if j == nj - 1:
                                norm = h
                            yield
                    flush(pend)
                    normalise(norm)
                    yield

                def run_step(parts):
                    live = [list(p) for p in parts]
                    done = [p[0] is None for p in live]
                    used = [0] * len(live)
                    active = [i for i, p in enumerate(live) if p[0] is not None]
                    rnd = 0
                    while active:
                        nxt = []
                        only_strided = all(live[i][2] > 1 for i in active)
                        for i in active:
                            g, q, sd = live[i]
                            if sd > 1 and not only_strided and (rnd % sd) != 0:
                                nxt.append(i)
                                continue
                            try:
                                next(g)
                                used[i] += 1
                                if q is None or used[i] < q:
                                    nxt.append(i)
                            except StopIteration:
                                done[i] = True
                        active = nxt
                        rnd += 1
                    return [None if done[i] else live[i][0] for i in range(len(live))]

                run_step([[gen_score(0), None, 1]])
                oldb = None
                att = None
                for k in range(NT + 8):
                    newb = gen_bisect(k) if k < NT else None
                    sc_units = 3 + 8 * ((k + 2) * 128 + 511) // 512 if k + 1 < NT else 0
                    if att is None and k >= 5 and (k - 5) % 4 == 0 and (k - 5) // 4 < NT // 4:
                        g = (k - 5) // 4
                        att = [gen_attend(g), 4, 8 * (4 * g + 4) + 8]
                    quota = 0
                    if att is not None:
                        quota = None if att[1] == 1 else (att[2] + 3) // 4
                    rounds = max(sc_units, quota if quota is not None else (att[2] + 3) // 4 if att is not None else 0)
                    sd = max(1, rounds // 14)
                    parts = [[gen_score(k + 1) if k + 1 < NT else None, None, 1], [oldb, None, sd], [newb, 12, sd]]
                    if att is not None:
                        parts.append([att[0], quota, 1])
                    if all(p[0] is None for p in parts):
                        continue
                    rem = run_step(parts)
                    oldb = rem[2]
                    if att is not None:
                        att[1] -= 1
                        if rem[3] is None or att[1] == 0:
                            att = None
                kb.barrier()

        def phase_conv(l):
            with ExitStack() as st:
                u = SB(st, "u", [128, 4, S + 32], BF16)
                cw = SB(st, "cw", [128, 4, CONV_K], F32)
                dgc = SB(st, "dgc", [128, 4, CONV_K, 128], BF16)
                y32 = SB(st, "y32", [128, 4, 512], F32)
                yb = SB(st, "yb", [128, 4, 512], BF16)
                ysq = SB(st, "ysq", [128, 4, 512], BF16)
                mean = SB(st, "cmean", [128, 512], F32)
                rstd = SB(st, "crstd", [128, 512], F32)
                t1 = SB(st, "ct1", [128, 4, 512], F32)
                so = [SB(st, f"cso{i}", [128, 4, 512], BF16) for i in range(2)]
                pc = [PS(st, f"pc{i}", [128, 512], F32) for i in range(2)]
                pm = PS(st, "pm", [128, 512], F32)
                pq = PS(st, "pq", [128, 512], F32)
                kb.op("pool", lambda: nc.gpsimd.memset(u[:, :, 0:32], 0.0), writes=["u"])
                kb.dma("sp", u[:, :, 32:], cm(hu), writes=["u"])
                kb.dma("sp", cw[:], conv_wT[l].rearrange("(c p) j -> p c j", p=128), writes=["cw"])
                for c in range(4):
                    for j in range(CONV_K):
                        kb.op("dve", lambda c=c, j=j: nc.vector.tensor_scalar(
                            out=dgc[:, c, j, :], in0=identb[:], scalar1=cw[:, c, j:j + 1], scalar2=None, op0=ALU.mult),
                            reads=["identb", "cw"], writes=[("dgc", c)])
                od = onesdiv["od512"]
                for tt in range(TT):
                    for c in range(4):
                        p = c % 2
                        for j in range(CONV_K):
                            off = 2 + tt * 512 + j
                            kb.op("pe", lambda c=c, j=j, p=p, off=off: nc.tensor.matmul(
                                pc[p][:, :], dgc[:, c, j, :], u[:, c, off:off + 512], start=(j == 0), stop=(j == CONV_K - 1)),
                                reads=[("dgc", c), "u"], writes=[f"pc{p}"])
                        kb.op("act", lambda c=c, p=p: nc.scalar.activation(out=y32[:, c, :], in_=pc[p][:, :], func=AF.Identity,
                                                                           bias=vec[:, l, c:c + 1], scale=1.0),
                              reads=[f"pc{p}", "vec"], writes=[("y32", c)])
                        kb.op("act", lambda c=c: nc.scalar.activation(out=ysq[:, c, :], in_=y32[:, c, :], func=AF.Square),
                              reads=[("y32", c)], writes=[("ysq", c)])
                        kb.op("dve", lambda c=c: nc.vector.tensor_copy(yb[:, c, :], y32[:, c, :]), reads=[("y32", c)],
                              writes=[("yb", c)])
                    for c in range(4):
                        kb.op("pe", lambda c=c: nc.tensor.matmul(pm[:, :], od[:], yb[:, c, :], start=(c == 0), stop=(c == 3)),
                              reads=["od512", ("yb", c)], writes=["pm"])
                    for c in range(4):
                        kb.op("pe", lambda c=c: nc.tensor.matmul(pq[:, :], od[:], ysq[:, c, :], start=(c == 0), stop=(c == 3)),
                              reads=["od512", ("ysq", c)], writes=["pq"])
                    ln_tail(mean, rstd, pm, pq, "c")
                    sb_ = tt % 2
                    kb.op("dve", lambda: nc.vector.tensor_tensor(out=t1[:], in0=y32[:], in1=bcast_mid(mean[:], 4), op=ALU.subtract),
                          reads=[("y32", c) for c in range(4)] + ["cmean"], writes=["ct1"])
                    kb.op("dve", lambda: nc.vector.tensor_tensor(out=t1[:], in0=t1[:], in1=bcast_mid(rstd[:], 4), op=ALU.mult),
                          reads=["ct1", "crstd"], writes=["ct1"])
                    for c in range(4):
                        kb.op("act", lambda c=c: nc.scalar.activation(out=t1[:, c, :], in_=t1[:, c, :], func=AF.Silu,
                                                                      bias=vec[:, l, 8 + c:9 + c], scale=vec[:, l, 4 + c:5 + c]),
                              reads=["ct1", "vec"], writes=["ct1"])
                        kb.op("dve", lambda c=c, sb_=sb_: nc.vector.tensor_scalar(
                            out=so[sb_][:, c, :], in0=t1[:, c, :], scalar1=vec[:, l, 12 + c:13 + c], scalar2=None, op0=ALU.mult),
                            reads=["ct1", "vec"], writes=[f"cso{sb_}"])
                    kb.dma("act", cm(hmc)[:, :, tt * 512:(tt + 1) * 512], so[sb_][:], reads=[f"cso{sb_}"])
                kb.barrier()

        def ln_tail(mean, rstd, pm, pq, tag):
            mk, rk = f"{tag}mean", f"{tag}rstd"
            kb.op("act", lambda: nc.scalar.copy(mean[:], pm[:, :]), reads=["pm"], writes=[mk])
            kb.op("dve", lambda: nc.vector.tensor_tensor(out=rstd[:], in0=mean[:], in1=mean[:], op=ALU.mult),
                  reads=[mk], writes=[rk])
            kb.op("dve", lambda: nc.vector.tensor_tensor(out=rstd[:], in0=pq[:, :], in1=rstd[:], op=ALU.subtract),
                  reads=["pq", rk], writes=[rk])
            kb.op("dve", lambda: nc.vector.tensor_scalar(out=rstd[:], in0=rstd[:], scalar1=LN_EPS, scalar2=None, op0=ALU.add),
                  reads=[rk], writes=[rk])
            kb.op("act", lambda: nc.scalar.activation(out=rstd[:], in_=rstd[:], func=AF.Sqrt), reads=[rk], writes=[rk])
            kb.op("dve", lambda: nc.vector.reciprocal(out=rstd[:], in_=rstd[:]), reads=[rk], writes=[rk])

        def phase_outproj(l):
            with ExitStack() as st:
                woA = SB(st, "woA", [128, 8, D], BF16)
                woC = SB(st, "woC", [128, 4, D], BF16)
                ma = [SB(st, f"ma{i}", [128, 8, 512], BF16) for i in range(2)]
                mc = [SB(st, f"mc{i}", [128, 4, 512], BF16) for i in range(2)]
                fo = [SB(st, f"fo{i}", [128, 8, 512], F32) for i in range(2)]
                pp = [PS(st, f"pp{i}", [128, 512], F32) for i in range(3)]
                kb.dma("pool", woA[0:64, :, :], w_out[l][0:512, :].rearrange("(h d) n -> d h n", d=64), writes=["woA"])
                kb.dma("pool", woC[:], w_out[l][512:1024, :].rearrange("(c p) n -> p c n", p=128), writes=["woC"])
                hma_v = hma.rearrange("h d t -> d h t")
                n = 0
                for tt in range(TT):
                    b = tt % 2
                    kb.dma("sp", ma[b][0:64, :, :], hma_v[:, :, tt * 512:(tt + 1) * 512], writes=[f"ma{b}"])
                    kb.dma("sp", mc[b][:], cm(hmc)[:, :, tt * 512:(tt + 1) * 512], writes=[f"mc{b}"])
                    for dc in range(8):
                        p = n % 3
                        n += 1
                        for h in range(8):
                            kb.op("pe", lambda h=h, dc=dc, p=p, b=b: nc.tensor.matmul(
                                pp[p][:, :], woA[0:64, h, dc * 128:(dc + 1) * 128], ma[b][0:64, h, :], start=(h == 0), stop=False),
                                reads=["woA", f"ma{b}"], writes=[f"pp{p}"])
                        for c in range(4):
                            kb.op("pe", lambda c=c, dc=dc, p=p, b=b: nc.tensor.matmul(
                                pp[p][:, :], woC[:, c, dc * 128:(dc + 1) * 128], mc[b][:, c, :], start=False, stop=(c == 3)),
                                reads=["woC", f"mc{b}"], writes=[f"pp{p}"])
                        if dc % 2 == 0:
                            kb.op("act", lambda dc=dc, p=p, b=b: nc.scalar.copy(fo[b][:, dc, :], pp[p][:, :]),
                                  reads=[f"pp{p}"], writes=[(f"fo{b}", dc)])
                        else:
                            kb.op("dve", lambda dc=dc, p=p, b=b: nc.vector.tensor_copy(fo[b][:, dc, :], pp[p][:, :]),
                                  reads=[f"pp{p}"], writes=[(f"fo{b}", dc)])
                    kb.dma("act", cm(fsc)[:, :, tt * 512:(tt + 1) * 512], fo[b][:], reads=[(f"fo{b}", dc) for dc in range(8)])
                    for dc in range(1, 8):
                        kb.readers.setdefault((f"fo{b}", dc), {}).update(kb.readers.get((f"fo{b}", 0), {}))
                kb.barrier()

        def phase_ln(l, which, dst, router=None):
            gcol = 16 + 16 * which
            with ExitStack() as st:
                x32 = [SB(st, f"lx{i}", [128, 8, 512], F32) for i in range(2)]
                f32_ = [SB(st, f"lf{i}", [128, 8, 512], F32) for i in range(2)]
                zb = SB(st, "zb", [128, 8, 512], BF16)
                zsq = SB(st, "zsq", [128, 8, 512], BF16)
                mean = SB(st, "lmean", [128, 512], F32)
                rstd = SB(st, "lrstd", [128, 512], F32)
                xo = [SB(st, f"lo{i}", [128, 8, 512], F32) for i in range(2)]
                pm = PS(st, "pm", [128, 512], F32)
                pq = PS(st, "pq", [128, 512], F32)
                od = onesdiv["od1024"]
                if router is not None:
                    rw = SB(st, "rw", [128, 8, NE], F32)
                    lg = SB(st, "lg", [128, NE], F32)
                    mx8 = SB(st, "mx8", [128, 8], F32)
                    ex = SB(st, "ex", [128, NE], F32)
                    gs = SB(st, "gs", [128, 4], F32)
                    gt = SB(st, "gt", [128, NE], F32)
                    gT = [SB(st, f"gT{i}", [8, 512], F32) for i in range(2)]
                    pr = PS(st, "pr", [128, NE], F32)
                    pgt = PS(st, "pgt", [8, 512], F32)
                    kb.dma("sp", rw[:], router.rearrange("(c p) e -> p c e", p=128), writes=["rw"])
                for tt in range(TT):
                    b = tt % 2
                    ts_ = slice(tt * 512, (tt + 1) * 512)
                    kb.dma("sp", x32[b][:], cm(xm)[:, :, ts_], writes=[f"lx{b}"])
                    kb.dma("sp", f32_[b][:], cm(fsc)[:, :, ts_], writes=[f"lf{b}"])
                    kb.op("dve", lambda b=b: nc.vector.scalar_tensor_tensor(
                        out=x32[b][:], in0=x32[b][:], scalar=DN_ALPHA, in1=f32_[b][:], op0=ALU.mult, op1=ALU.add),
                        reads=[f"lx{b}", f"lf{b}"], writes=[f"lx{b}"])
                    kb.op("act", lambda b=b: nc.scalar.copy(zb[:], x32[b][:]), reads=[f"lx{b}"], writes=["zb"])
                    kb.op("act", lambda b=b: nc.scalar.activation(out=zsq[:], in_=x32[b][:], func=AF.Square),
                          reads=[f"lx{b}"], writes=["zsq"])
                    for c in range(8):
                        kb.op("pe", lambda c=c: nc.tensor.matmul(pm[:, :], od[:], zb[:, c, :], start=(c == 0), stop=(c == 7)),
                              reads=["od1024", "zb"], writes=["pm"])
                    for c in range(8):
                        kb.op("pe", lambda c=c: nc.tensor.matmul(pq[:, :], od[:], zsq[:, c, :], start=(c == 0), stop=(c == 7)),
                              reads=["od1024", "zsq"], writes=["pq"])
                    ln_tail(mean, rstd, pm, pq, "l")
                    kb.op("dve", lambda b=b: nc.vector.tensor_tensor(out=x32[b][:], in0=x32[b][:], in1=bcast_mid(mean[:], 8),
                                                                     op=ALU.subtract),
                          reads=[f"lx{b}", "lmean"], writes=[f"lx{b}"])
                    kb.op("dve", lambda b=b: nc.vector.tensor_tensor(out=x32[b][:], in0=x32[b][:], in1=bcast_mid(rstd[:], 8),
                                                                     op=ALU.mult),
                          reads=[f"lx{b}", "lrstd"], writes=[f"lx{b}"])
                    for c in range(8):
                        kb.op("act", lambda c=c, b=b: nc.scalar.activation(
                            out=xo[b][:, c, :], in_=x32[b][:, c, :], func=AF.Identity,
                            bias=vec[:, l, gcol + 8 + c:gcol + 9 + c], scale=vec[:, l, gcol + c:gcol + c + 1]),
                            reads=[f"lx{b}", "vec"], writes=[(f"lo{b}", c)])
                    kb.dma("act", cm(dst)[:, :, ts_], xo[b][:], reads=[(f"lo{b}", c) for c in range(8)])
                    for c in range(1, 8):
                        kb.readers.setdefault((f"lo{b}", c), {}).update(kb.readers.get((f"lo{b}", 0), {}))
                    if router is not None:
                        for sub in range(4):
                            for c in range(8):
                                kb.op("pe", lambda c=c, b=b, sub=sub: nc.tensor.matmul(
                                    pr[:, :], xo[b][:, c, sub * 128:(sub + 1) * 128], rw[:, c, :], start=(c == 0), stop=(c == 7)),
                                    reads=[(f"lo{b}", c), "rw"], writes=["pr"])
                            kb.op("dve", lambda: nc.vector.tensor_copy(lg[:], pr[:, :]), reads=["pr"], writes=["lg"])
                            kb.op("dve", lambda: nc.vector.max(out=mx8[:], in_=lg[:]), reads=["lg"], writes=["mx8"])
                            kb.op("dve", lambda: nc.vector.tensor_scalar(out=gs[:, 0:1], in0=mx8[:, 0:1], scalar1=-1.0, scalar2=None,
                                                                         op0=ALU.mult), reads=["mx8"], writes=["gs"])
                            kb.op("act", lambda: nc.scalar.activation(out=ex[:], in_=lg[:], func=AF.Exp, bias=gs[:, 0:1], scale=1.0),
                                  reads=["lg", "gs"], writes=["ex"])
                            kb.op("dve", lambda: nc.vector.scalar_tensor_tensor(
                                out=ex[:], in0=lg[:], scalar=mx8[:, 1:2], in1=ex[:], op0=ALU.is_ge, op1=ALU.mult),
                                reads=["lg", "mx8", "ex"], writes=["ex"])
                            kb.op("dve", lambda: nc.vector.tensor_reduce(out=gs[:, 1:2], in_=ex[:], axis=AX.X, op=ALU.add),
                                  reads=["ex", "gs"], writes=["gs"])
                            kb.op("dve", lambda: nc.vector.reciprocal(out=gs[:, 2:3], in_=gs[:, 1:2]), reads=["gs"], writes=["gs"])
                            kb.op("dve", lambda: nc.vector.tensor_scalar(out=gt[:], in0=ex[:], scalar1=gs[:, 2:3], scalar2=None,
                                                                         op0=ALU.mult), reads=["ex", "gs"], writes=["gt"])
                            kb.op("pe", lambda sub=sub: nc.tensor.transpose(pgt[0:8, sub * 128:(sub + 1) * 128], gt[:, :], ident32[:]),
                                  reads=["gt", "ident32"], writes=["pgt"])
                        kb.op("dve", lambda b=b: nc.vector.tensor_copy(gT[b][:], pgt[:, :]), reads=["pgt"], writes=[f"gT{b}"])
                        kb.dma("act", hg[:, ts_], gT[b][:], reads=[f"gT{b}"])
                kb.barrier()

        def phase_ffn(l):
            moe = (l % 2 == 1)
            m = l // 2
            HW = D_FFE if moe else D_FF
            nexp = NE if moe else 1
            groups = [(c0, min(512, HW - c0)) for c0 in range(0, HW, 512)]
            HT = 2048
            with ExitStack() as st:
                xb = SB(st, "fxb", [128, 8, HT], BF16)
                yacc = SB(st, "yacc", [128, 8, HT], F32)
                wg = [SB(st, f"fwg{i}", [128, 8, 512], BF16) for i in range(2)]
                wu = [SB(st, f"fwu{i}", [128, 8, 512], BF16) for i in range(2)]
                wd = [SB(st, f"fwd{i}", [128, 4, D], BF16) for i in range(2)]
                sgm = [SB(st, f"fsg{i}", [128, 512], BF16) for i in range(2)]
                hT = [SB(st, f"fhT{i}", [128, 4, 512], BF16) for i in range(2)]
                htmp = [SB(st, f"fht{i}", [128, 512], F32) for i in range(2)]
                pA = [PS(st, f"pA{i}", [128, 512], F32) for i in range(2)]
                pB = [PS(st, f"pB{i}", [128, 512], F32) for i in range(2)]
                pY = [PS(st, f"pY{i}", [128, 512], F32) for i in range(2)]
                if moe:
                    sel8 = SB(st, "sel8", [8, 8, 128], F32)
                    kb.op("pool", lambda: nc.gpsimd.memset(sel8[:], 0.0), writes=["sel8"])
                    kb.op("pool", lambda: nc.gpsimd.affine_select(out=sel8[:], in_=sel8[:], pattern=[[-1, 8], [0, 128]],
                                                                  compare_op=ALU.not_equal, fill=1.0, base=0,
                                                                  channel_multiplier=1), reads=["sel8"], writes=["sel8"])
                    gTs = SB(st, "gTs", [8, HT], F32)
                    gbc = SB(st, "gbc", [128, 4, 512], BF16)
                    pG = PS(st, "pG", [128, 512], F32)
                nab = 0
                ny = 0
                nh = 0
                items = []
                for half in range(S // HT):
                    for e in range(nexp):
                        for gi, (c0, wdt) in enumerate(groups):
                            items.append((half, e, gi, c0, wdt))

                def issue_w(k):
                    half, e, gi, c0, wdt = items[k]
                    ws = k % 2
                    if moe:
                        Wg, Wu, Wd = moe_wg[m, e], moe_wu[m, e], moe_wd[m, e]
                    else:
                        Wg, Wu, Wd = ffn_wg[m], ffn_wu[m], ffn_wd[m]
                    nfc = wdt // 128
                    kb.dma("pool", wg[ws][:, :, 0:wdt], cm(Wg)[:, :, c0:c0 + wdt], writes=[f"fwg{ws}"])
                    kb.dma("pool", wu[ws][:, :, 0:wdt], cm(Wu)[:, :, c0:c0 + wdt], writes=[f"fwu{ws}"])
                    kb.dma("pool", wd[ws][:, 0:nfc, :], Wd[c0:c0 + wdt, :].rearrange("(c p) n -> p c n", p=128),
                           writes=[f"fwd{ws}"])

                issue_w(0)
                for k, (half, e, gi, c0, wdt) in enumerate(items):
                    hs = slice(half * HT, (half + 1) * HT)
                    ws = k % 2
                    nfc = wdt // 128
                    if e == 0 and gi == 0:
                        for c in range(8):
                            kb.dma("pool", xb[:, c, :], cm(xm)[:, c, hs], writes=[("fxb", c)])
                        if moe:
                            kb.dma("sp", gTs[:], hg[:, hs], writes=["gTs"])
                    if k + 1 < len(items):
                        issue_w(k + 1)
                    first = (e == 0 and gi == 0)
                    if moe and gi == 0:
                        for tt in range(4):
                            kb.op("pe", lambda e=e, tt=tt: nc.tensor.matmul(
                                pG[:, :], sel8[0:8, e, :], gTs[0:8, tt * 512:(tt + 1) * 512], start=True, stop=True),
                                reads=["sel8", "gTs"], writes=["pG"])
                            kb.op("act", lambda tt=tt: nc.scalar.copy(gbc[:, tt, :], pG[:, :]), reads=["pG"], writes=[("gbc", tt)])
                    for tt in range(4):
                        tsl = slice(tt * 512, (tt + 1) * 512)
                        hb = nh % 2
                        nh += 1
                        for fc in range(nfc):
                            a = nab % 2
                            nab += 1
                            for c in range(8):
                                kb.op("pe", lambda c=c, a=a, ws=ws, fc=fc, tsl=tsl: nc.tensor.matmul(
                                    pA[a][:, :], wg[ws][:, c, fc * 128:(fc + 1) * 128], xb[:, c, tsl],
                                    start=(c == 0), stop=(c == 7)), reads=[f"fwg{ws}", ("fxb", c)], writes=[f"pA{a}"])
                            for c in range(8):
                                kb.op("pe", lambda c=c, a=a, ws=ws, fc=fc, tsl=tsl: nc.tensor.matmul(
                                    pB[a][:, :], wu[ws][:, c, fc * 128:(fc + 1) * 128], xb[:, c, tsl],
                                    start=(c == 0), stop=(c == 7)), reads=[f"fwu{ws}", ("fxb", c)], writes=[f"pB{a}"])
                            kb.op("act", lambda a=a: nc.scalar.activation(out=sgm[a][:], in_=pA[a][:, :], func=AF.Silu),
                                  reads=[f"pA{a}"], writes=[f"fsg{a}"])
                            if moe:
                                kb.op("dve", lambda a=a: nc.vector.tensor_tensor(out=htmp[a][:], in0=pB[a][:, :], in1=sgm[a][:],
                                                                                 op=ALU.mult),
                                      reads=[f"pB{a}", f"fsg{a}"], writes=[f"fht{a}"])
                                kb.op("dve", lambda a=a, hb=hb, fc=fc, tt=tt: nc.vector.tensor_tensor(
                                    out=hT[hb][:, fc, :], in0=htmp[a][:], in1=gbc[:, tt, :], op=ALU.mult),
                                    reads=[f"fht{a}", ("gbc", tt)], writes=[(f"fhT{hb}", fc)])
                            else:
                                kb.op("dve", lambda a=a, hb=hb, fc=fc: nc.vector.tensor_tensor(
                                    out=hT[hb][:, fc, :], in0=pB[a][:, :], in1=sgm[a][:], op=ALU.mult),
                                    reads=[f"pB{a}", f"fsg{a}"], writes=[(f"fhT{hb}", fc)])
                        for dc in range(8):
                            y = ny % 2
                            ny += 1
                            for fc in range(nfc):
                                kb.op("pe", lambda fc=fc, dc=dc, y=y, ws=ws, hb=hb: nc.tensor.matmul(
                                    pY[y][:, :], wd[ws][:, fc, dc * 128:(dc + 1) * 128], hT[hb][:, fc, :],
                                    start=(fc == 0), stop=(fc == nfc - 1)),
                                    reads=[f"fwd{ws}", (f"fhT{hb}", fc)], writes=[f"pY{y}"])
                            if first:
                                kb.op("dve", lambda dc=dc, y=y, tsl=tsl: nc.vector.tensor_copy(yacc[:, dc, tsl], pY[y][:, :]),
                                      reads=[f"pY{y}"], writes=[("yacc", dc, tsl.start)])
                            else:
                                kb.op("dve", lambda dc=dc, y=y, tsl=tsl: nc.vector.tensor_tensor(
                                    out=yacc[:, dc, tsl], in0=pY[y][:, :], in1=yacc[:, dc, tsl], op=ALU.add),
                                    reads=[f"pY{y}", ("yacc", dc, tsl.start)], writes=[("yacc", dc, tsl.start)])
                    if e == nexp - 1 and gi == len(groups) - 1:
                        kb.dma("act", cm(fsc)[:, :, hs], yacc[:],
                               reads=[("yacc", dc, t0) for dc in range(8) for t0 in range(0, HT, 512)])
                kb.barrier()

        stages = []
        for l in range(n_layers):
            last = (l == n_layers - 1)
            stages += [("proj", lambda l=l: phase_proj(l)), ("attn", lambda l=l: phase_attn(l)),
                       ("conv", lambda l=l: phase_conv(l)), ("outproj", lambda l=l: phase_outproj(l)),
                       ("ln1", lambda l=l: phase_ln(l, 0, xm, router=(moe_r[l // 2] if l % 2 == 1 else None))),
                       ("ffn", lambda l=l: phase_ffn(l)),
                       ("ln2", lambda l=l, last=last: phase_ln(l, 1, outT if last else xm))]
        for idx, (name, fn) in enumerate(stages):
            fn()
            if stop_after is not None and (idx + 1) >= stop_after:
                break
        kb.barrier()
        build_program.n_inst = kb.n_inst
    return nc


def _t5_bucket_np(rel):
    nb = 16
    max_exact = 8
    ret = np.where(rel > 0, nb, 0).astype(np.int32)
    n = np.abs(rel)
    nf = np.maximum(n, 1).astype(np.float32)
    large = max_exact + (np.log(nf / max_exact) / math.log(128 / max_exact) * (nb - max_exact)).astype(np.int32)
    large = np.minimum(large, nb - 1)
    return ret + np.where(n < max_exact, n, large)


def _const_onehot():
    s = np.arange(128)[:, None]
    t = np.arange(128)[None, :]
    oh = np.zeros((128, 32, 2, 128), np.float32)
    for o in range(2):
        rel = (s - 128 * o) - t
        bk = _t5_bucket_np(rel)
        for b in range(32):
            oh[:, b, o, :] = (bk == b)
    return oh


def _pack_vecs(inp):
    f = lambda a, n: np.asarray(a, np.float32).reshape(DEPTH, n, 128).transpose(2, 0, 1)
    v = np.zeros((128, DEPTH, NV), np.float32)
    v[:, :, 0:4] = f(inp["conv_b"], 4)
    v[:, :, 4:8] = f(inp["conv_ln_g"], 4)
    v[:, :, 8:12] = f(inp["conv_ln_b"], 4)
    ms = np.asarray(inp["mix_scale"], np.float32)
    v[:, :, 12:16] = f(ms[:, 512:], 4)
    v[:, :, 16:24] = f(inp["ln1_g"], 8)
    v[:, :, 24:32] = f(inp["ln1_b"], 8)
    v[:, :, 32:40] = f(inp["ln2_g"], 8)
    v[:, :, 40:48] = f(inp["ln2_b"], 8)
    v[0:64, :, 48:56] = ms[:, :512].reshape(DEPTH, 8, 64).transpose(2, 0, 1)
    return v


def make_in_maps(inputs, n_cores=8):
    shared = {
        "w_in": np.ascontiguousarray(inputs["w_in"], np.float32),
        "conv_wT": np.ascontiguousarray(np.asarray(inputs["conv_w"], np.float32).transpose(0, 2, 1)),
        "vecs": _pack_vecs(inputs),
        "rel_bias": np.ascontiguousarray(inputs["rel_bias"], np.float32),
        "ohb": _const_onehot(),
        "w_out": np.ascontiguousarray(inputs["w_out"], np.float32),
        "ffn_w_gate": np.ascontiguousarray(inputs["ffn_w_gate"], np.float32),
        "ffn_w_up": np.ascontiguousarray(inputs["ffn_w_up"], np.float32),
        "ffn_w_down": np.ascontiguousarray(inputs["ffn_w_down"], np.float32),
        "moe_router": np.ascontiguousarray(inputs["moe_router"], np.float32),
        "moe_w_gate": np.ascontiguousarray(inputs["moe_w_gate"], np.float32),
        "moe_w_up": np.ascontiguousarray(inputs["moe_w_up"], np.float32),
        "moe_w_down": np.ascontiguousarray(inputs["moe_w_down"], np.float32),
    }
    x = np.asarray(inputs["x"], np.float32)
    maps = []
    for b in range(n_cores):
        m = dict(shared)
        m["xT"] = np.ascontiguousarray(x[b].T)
        maps.append(m)
    return maps


def kernel(**inputs):
    nc = build_program()
    in_maps = make_in_maps(inputs, 8)
    res = run_bass_kernel_spmd(nc, in_maps, core_ids=list(range(8)))
    out = np.stack([np.ascontiguousarray(r["outT"].T) for r in res.results], axis=0)
    return out.astype(np.float32)
```

```python
import math
from contextlib import ExitStack

import numpy as np
import concourse.bass as bass
import concourse.mybir as mybir
from concourse.bass_utils import run_bass_kernel_spmd

F32 = mybir.dt.float32
BF16 = mybir.dt.bfloat16
FP8 = mybir.dt.float8e5
AF = mybir.ActivationFunctionType
ALU = mybir.AluOpType
AX = mybir.AxisListType

D = 1024
S = 4096
DEPTH = 4
NT = S // 128
TT = S // 512
IN_COLS = 3144
OFF_Q, OFF_K, OFF_V, OFF_QI, OFF_KI, OFF_WI, OFF_A, OFF_G = 0, 512, 1024, 1536, 2048, 2112, 2120, 2632
D_FF = 2816
D_FFE = 3584
NE = 8
CONV_K = 31
DN_ALPHA = (2.0 * DEPTH) ** 0.25
LN_EPS = 1e-5
NEGM = -30000.0
NEG = -1e30
N_BISECT = 16
NV = 56

EPOCH = 30000
DMA_RING = 8


class KB:
    def __init__(self, nc, es):
        self.nc = nc
        self.es = es
        self.eng = {"pe": nc.tensor, "act": nc.scalar, "dve": nc.vector, "pool": nc.gpsimd, "sp": nc.sync}
        self.count = {e: 0 for e in self.eng}
        self.dma_n = {}
        self.waited = {e: {} for e in self.eng}
        self.last_write = {}
        self.readers = {}
        self._semh = {}
        self.n_inst = 0

    def _sem(self, key):
        if key not in self._semh:
            name = "s_" + "_".join(str(k) for k in key)
            self._semh[key] = self.es.enter_context(self.nc.semaphore(name))
        return self._semh[key]

    def _wait(self, eng, token):
        key, val = token
        if self.waited[eng].get(key, 0) >= val:
            return
        self.waited[eng][key] = val
        self.eng[eng].wait_ge(self._sem(key), val)

    def _deps(self, eng, reads, writes):
        toks = []
        for b in reads:
            t = self.last_write.get(b)
            if t is not None:
                toks.append(t)
        for b in writes:
            t = self.last_write.get(b)
            if t is not None:
                toks.append(t)
            toks.extend(self.readers.get(b, {}).values())
        for t in toks:
            if eng == "pe" and t[0][0] == "c" and t[0][1] == "pe":
                continue
            self._wait(eng, t)

    def _record(self, token, reads, writes):
        for b in writes:
            self.last_write[b] = token
            self.readers[b] = {}
        for b in reads:
            self.readers.setdefault(b, {})[token[0]] = token

    def op(self, eng, ins_fn, reads=(), writes=()):
        self._deps(eng, reads, writes)
        ins = ins_fn()
        n = self.count[eng]
        self.count[eng] = n + 1
        key = ("c", eng, n // EPOCH)
        ins.then_inc(self._sem(key), 1)
        self._record((key, n % EPOCH + 1), reads, writes)
        self.n_inst += 1
        return ins

    def dma(self, q, out, in_, reads=(), writes=()):
        n = self.dma_n.get(q, 0)
        self.dma_n[q] = n + 1
        slot = n % DMA_RING
        key = ("d", q, slot)
        rnd = n // DMA_RING
        if rnd > 0:
            self._wait(q, (key, 16 * rnd))
        self._deps(q, reads, writes)
        ins = self.eng[q].dma_start(out=out, in_=in_)
        ins.then_inc(self._sem(key), 16)
        self._record((key, 16 * (rnd + 1)), reads, writes)
        self.n_inst += 1
        return ins

    def barrier(self):
        toks = []
        for e in self.eng:
            n = self.count[e]
            if n > 0:
                toks.append((("c", e, (n - 1) // EPOCH), (n - 1) % EPOCH + 1))
        for q, n in self.dma_n.items():
            for slot in range(min(n, DMA_RING)):
                toks.append((("d", q, slot), 16 * ((n - 1 - slot) // DMA_RING + 1)))
        for e in self.eng:
            for t in toks:
                self._wait(e, t)
        self.last_write = {}
        self.readers = {}


def bcast_mid(a, n):
    return bass.AP(a.tensor, a.offset, [list(a.ap[0]), [0, n], list(a.ap[1])])


def build_program(n_layers=DEPTH, debug=False, stop_after=None):
    nc = bass.Bass("TRN2", target_bir_lowering=False)
    dt_in = lambda name, shape: nc.dram_tensor(name, shape, F32, kind="ExternalInput").ap()
    xT_in = dt_in("xT", [D, S])
    w_in = dt_in("w_in", [DEPTH, D, IN_COLS])
    conv_wT = dt_in("conv_wT", [DEPTH, 512, CONV_K])
    vecs = dt_in("vecs", [128, DEPTH, NV])
    rel_bias = dt_in("rel_bias", [32, 8])
    ohb = dt_in("ohb", [128, 32, 2, 128])
    w_out = dt_in("w_out", [DEPTH, D, D])
    ffn_wg = dt_in("ffn_w_gate", [2, D, D_FF])
    ffn_wu = dt_in("ffn_w_up", [2, D, D_FF])
    ffn_wd = dt_in("ffn_w_down", [2, D_FF, D])
    moe_r = dt_in("moe_router", [2, D, NE])
    moe_wg = dt_in("moe_w_gate", [2, NE, D, D_FFE])
    moe_wu = dt_in("moe_w_up", [2, NE, D, D_FFE])
    moe_wd = dt_in("moe_w_down", [2, NE, D_FFE, D])
    outT = nc.dram_tensor("outT", [D, S], F32, kind="ExternalOutput").ap()

    skind = "ExternalOutput" if debug else "Internal"
    scr = lambda name, shape, dt: nc.dram_tensor(name, shape, dt, kind=skind).ap()
    xm = scr("xm", [D, S], F32)
    fsc = scr("fsc", [D, S], F32)
    hq = scr("hq", [512, S], BF16)
    hk = scr("hk", [512, S], BF16)
    hqi = scr("hqi", [512, S], BF16)
    hki = scr("hki", [64, S], BF16)
    hu = scr("hu", [512, S], BF16)
    hv = scr("hv", [S, 520], BF16)
    hw = scr("hw", [S, 8], F32)
    hma = scr("hma", [8, 64, S], BF16)
    hmc = scr("hmc", [512, S], BF16)
    hg = scr("hg", [8, S], F32)

    def cm(a):
        return a.rearrange("(c p) s -> p c s", p=128)

    with ExitStack() as es:
        kb = KB(nc, es)
        uid = [0]

        def SB(st, name, shape, dt):
            uid[0] += 1
            return st.enter_context(nc.sbuf_tensor(f"{name}_{uid[0]}", shape, dt))

        def PS(st, name, shape, dt):
            uid[0] += 1
            return st.enter_context(nc.psum_tensor(f"{name}_{uid[0]}", shape, dt))

        identb = SB(es, "identb", [128, 128], BF16)
        ident32 = SB(es, "ident32", [128, 128], F32)
        onesdiv = {}
        ones32 = SB(es, "ones32", [128, 64], F32)
        vec = SB(es, "vec", [128, DEPTH, NV], F32)
        rbb = SB(es, "rbb", [128, 256], F32)
        pow2 = SB(es, "pow2", [128, N_BISECT + 1], F32)
        bias_hi = SB(es, "bias_hi", [128, 8, 2, 128], BF16)
        negc = SB(es, "negc", [128, 8], F32)

        kb.op("pool", lambda: nc.gpsimd.memset(ident32[:], 0.0), writes=["ident32"])
        kb.op("pool", lambda: nc.gpsimd.affine_select(out=ident32[:], in_=ident32[:], pattern=[[-1, 128]],
                                                      compare_op=ALU.not_equal, fill=1.0, base=0,
                                                      channel_multiplier=1), reads=["ident32"], writes=["ident32"])
        kb.op("dve", lambda: nc.vector.tensor_copy(identb[:], ident32[:]), reads=["ident32"], writes=["identb"])
        kb.op("dve", lambda: nc.vector.memset(ones32[:], 1.0), writes=["ones32"])
        for j in range(N_BISECT + 1):
            kb.op("dve", lambda j=j: nc.vector.memset(pow2[:, j:j + 1], 2.0 ** -(j + 1)), writes=["pow2"])
        for nm_, val in (("od512", 1.0 / 512), ("od1024", 1.0 / 1024)):
            t_ = SB(es, nm_, [128, 128], BF16)
            kb.op("dve", lambda t_=t_, val=val: nc.vector.memset(t_[:], val), writes=[nm_])
            onesdiv[nm_] = t_
        kb.dma("sp", vec[:], vecs, writes=["vec"])
        rb_flat = rel_bias.rearrange("b h -> (b h)")
        kb.dma("sp", rbb[:], bass.AP(rb_flat.tensor, rb_flat.offset, [[0, 128], [1, 256]]), writes=["rbb"])
        with ExitStack() as st:
            oh = SB(st, "oh", [128, 32, 2, 128], F32)
            acc = SB(st, "bacc", [128, 8, 2, 128], F32)
            tmp = SB(st, "btmp", [128, 8, 2, 128], F32)
            kb.dma("sp", oh[:], ohb, writes=["oh"])
            kb.op("dve", lambda: nc.vector.tensor_scalar(out=negc[:], in0=rbb[:, 15 * 8:16 * 8], scalar1=-1.0,
                                                         scalar2=None, op0=ALU.mult), reads=["rbb"], writes=["negc"])
            kb.op("dve", lambda: nc.vector.memset(acc[:], 0.0), writes=["bacc"])
            for h in range(8):
                for b in range(32):
                    kb.op("dve", lambda h=h, b=b: nc.vector.scalar_tensor_tensor(
                        out=acc[:, h], in0=oh[:, b], scalar=rbb[:, b * 8 + h:b * 8 + h + 1], in1=acc[:, h],
                        op0=ALU.mult, op1=ALU.add), reads=["oh", "rbb", "bacc"], writes=["bacc"])
                kb.op("dve", lambda h=h: nc.vector.tensor_scalar(
                    out=acc[:, h], in0=acc[:, h], scalar1=negc[:, h:h + 1], scalar2=8.0, op0=ALU.add, op1=ALU.mult),
                    reads=["bacc", "negc"], writes=["bacc"])
            kb.op("dve", lambda: nc.vector.tensor_copy(bias_hi[:], acc[:]), reads=["bacc"], writes=["bias_hi"])
            kb.barrier()

        with ExitStack() as st:
            xt = [SB(st, f"x0_{i}", [128, S], F32) for i in range(2)]
            for c in range(8):
                kb.dma("sp", xt[c % 2][:], cm(xT_in)[:, c, :], writes=[f"x0_{c % 2}"])
                kb.dma("act", cm(xm)[:, c, :], xt[c % 2][:], reads=[f"x0_{c % 2}"])
            kb.barrier()

        def phase_proj(l):
            with ExitStack() as st:
                xb = SB(st, "xb", [128, 8, S], BF16)
                wb = [SB(st, f"wb{i}", [128, 8, 512], BF16) for i in range(2)]
                wkw = SB(st, "wkw", [128, 8, 72], BF16)
                stg = [SB(st, f"stg{i}", [128, S], BF16) for i in range(2)]
                vst = [SB(st, f"vst{i}", [128, 4, 8, 65], BF16) for i in range(2)]
                wst = SB(st, "wst", [128, NT, 8], F32)
                sg = [SB(st, f"sg{i}", [128, 512], F32) for i in range(2)]
                pa = [PS(st, f"pa{i}", [128, 512], F32) for i in range(3)]
                pg = [PS(st, f"pg{i}", [128, 512], F32) for i in range(2)]
                pw = PS(st, "pw", [128, 8], F32)
                for c in range(8):
                    kb.dma("pool", xb[:, c, :], cm(xm)[:, c, :], writes=[("xb", c)])
                xbk = [("xb", c) for c in range(8)]
                wv = cm(w_in[l])
                nslot = [0]

                def loadw(col0, ncol=512):
                    i = nslot[0] % 2
                    nslot[0] += 1
                    kb.dma("pool", wb[i][:, :, 0:ncol], wv[:, :, col0:col0 + ncol], writes=[f"wb{i}"])
                    return i

                nev = [0]

                def evac(dst, src, reads, writes):
                    if nev[0] % 2 == 0:
                        kb.op("act", lambda: nc.scalar.copy(dst, src), reads=reads, writes=writes)
                    else:
                        kb.op("dve", lambda: nc.vector.tensor_copy(dst, src), reads=reads, writes=writes)
                    nev[0] += 1

                nst = [0]
                npa = [0]
                nxt_w = [loadw(OFF_Q)]
                order = [OFF_K, OFF_QI, OFF_A]
                for gi_, (col0, dst) in enumerate(((OFF_Q, hq), (OFF_K, hk), (OFF_QI, hqi))):
                    wi = nxt_w[0]
                    nxt_w[0] = loadw(order[gi_])
                    for blk in range(4):
                        si = nst[0] % 2
                        nst[0] += 1
                        for tt in range(TT):
                            p = npa[0] % 3
                            npa[0] += 1
                            for c in range(8):
                                kb.op("pe", lambda c=c, p=p, wi=wi, blk=blk, tt=tt: nc.tensor.matmul(
                                    pa[p][:, :], wb[wi][:, c, blk * 128:(blk + 1) * 128], xb[:, c, tt * 512:(tt + 1) * 512],
                                    start=(c == 0), stop=(c == 7)), reads=[f"wb{wi}", ("xb", c)], writes=[f"pa{p}"])
                            evac(stg[si][:, tt * 512:(tt + 1) * 512], pa[p][:, :], [f"pa{p}"], [(f"stg{si}", tt)])
                        kb.dma("sp", dst[blk * 128:(blk + 1) * 128, :], stg[si][:],
                               reads=[(f"stg{si}", tt) for tt in range(TT)], writes=[])
                        for tt in range(TT):
                            kb.readers.setdefault((f"stg{si}", tt), {}).update(kb.readers.get((f"stg{si}", 0), {}))
                wa = nxt_w[0]
                wg_ = loadw(OFF_G)
                for blk in range(4):
                    si = nst[0] % 2
                    nst[0] += 1
                    for tt in range(TT):
                        p = npa[0] % 3
                        npa[0] += 1
                        q = tt % 2
                        for c in range(8):
                            kb.op("pe", lambda c=c, p=p, blk=blk, tt=tt: nc.tensor.matmul(
                                pa[p][:, :], wb[wa][:, c, blk * 128:(blk + 1) * 128], xb[:, c, tt * 512:(tt + 1) * 512],
                                start=(c == 0), stop=(c == 7)), reads=[f"wb{wa}", ("xb", c)], writes=[f"pa{p}"])
                        for c in range(8):
                            kb.op("pe", lambda c=c, q=q, blk=blk, tt=tt: nc.tensor.matmul(
                                pg[q][:, :], wb[wg_][:, c, blk * 128:(blk + 1) * 128], xb[:, c, tt * 512:(tt + 1) * 512],
                                start=(c == 0), stop=(c == 7)), reads=[f"wb{wg_}", ("xb", c)], writes=[f"pg{q}"])
                        kb.op("act", lambda q=q: nc.scalar.activation(out=sg[q][:], in_=pg[q][:, :], func=AF.Sigmoid),
                              reads=[f"pg{q}"], writes=[f"sg{q}"])
                        kb.op("dve", lambda q=q, p=p, si=si, tt=tt: nc.vector.tensor_tensor(
                            out=stg[si][:, tt * 512:(tt + 1) * 512], in0=pa[p][:, :], in1=sg[q][:], op=ALU.mult),
                            reads=[f"pa{p}", f"sg{q}"], writes=[(f"stg{si}", tt)])
                    kb.dma("sp", hu[blk * 128:(blk + 1) * 128, :], stg[si][:],
                           reads=[(f"stg{si}", tt) for tt in range(TT)], writes=[])
                    for tt in range(TT):
                        kb.readers.setdefault((f"stg{si}", tt), {}).update(kb.readers.get((f"stg{si}", 0), {}))
                wvv = loadw(OFF_V)
                kb.dma("pool", wkw[:], wv[:, :, OFF_KI:OFF_KI + 72], writes=["wkw"])
                si = nst[0] % 2
                nst[0] += 1
                for tt in range(TT):
                    p = npa[0] % 3
                    npa[0] += 1
                    for c in range(8):
                        kb.op("pe", lambda c=c, p=p, tt=tt: nc.tensor.matmul(
                            pa[p][0:64, :], wkw[:, c, 0:64], xb[:, c, tt * 512:(tt + 1) * 512],
                            start=(c == 0), stop=(c == 7)), reads=["wkw", ("xb", c)], writes=[f"pa{p}"])
                    evac(stg[si][0:64, tt * 512:(tt + 1) * 512], pa[p][0:64, :], [f"pa{p}"], [(f"stg{si}", tt)])
                kb.dma("sp", hki[:, :], stg[si][0:64, :], reads=[(f"stg{si}", tt) for tt in range(TT)])
                for tt in range(TT):
                    kb.readers.setdefault((f"stg{si}", tt), {}).update(kb.readers.get((f"stg{si}", 0), {}))
                hv4 = hv.rearrange("(n p) c -> p n c", p=128)
                for i_ in range(2):
                    kb.op("pool", lambda i_=i_: nc.gpsimd.memset(vst[i_][:], 1.0), writes=[(f"vst{i_}", k_) for k_ in range(4)])
                for i in range(NT):
                    p = npa[0] % 3
                    npa[0] += 1
                    vi = (i // 4) % 2
                    for c in range(8):
                        kb.op("pe", lambda c=c, p=p, i=i: nc.tensor.matmul(
                            pa[p][:, :], xb[:, c, i * 128:(i + 1) * 128], wb[wvv][:, c, :],
                            start=(c == 0), stop=(c == 7)), reads=[f"wb{wvv}", ("xb", c)], writes=[f"pa{p}"])
                    evac(vst[vi][:, i % 4, :, 0:64], pa[p][:, :].rearrange("p (h d) -> p h d", d=64), [f"pa{p}"], [(f"vst{vi}", i % 4)])
                    for c in range(8):
                        kb.op("pe", lambda c=c, i=i: nc.tensor.matmul(
                            pw[:, :], xb[:, c, i * 128:(i + 1) * 128], wkw[:, c, 64:72],
                            start=(c == 0), stop=(c == 7)), reads=["wkw", ("xb", c)], writes=["pw"])
                    kb.op("dve", lambda i=i: nc.vector.tensor_copy(wst[:, i, :], pw[:, :]), reads=["pw"], writes=["wst"])
                    if i % 4 == 3:
                        g = i // 4
                        kb.dma("sp", hv4[:, 4 * g:4 * g + 4, :], vst[vi][:].rearrange("p n h d -> p n (h d)"),
                               reads=[(f"vst{vi}", k) for k in range(4)])
                        for k in range(1, 4):
                            kb.readers.setdefault((f"vst{vi}", k), {}).update(kb.readers.get((f"vst{vi}", 0), {}))
                kb.dma("sp", hw.rearrange("(n p) e -> p n e", p=128), wst[:], reads=["wst"])
                kb.barrier()

        def phase_attn(l):
            with ExitStack() as st:
                kT = SB(st, "kT", [128, 4, S], BF16)
                kiT = SB(st, "kiT", [128, S], BF16)
                va = SB(st, "va", [128, NT, 8, 65], BF16)
                qz = [SB(st, f"qz{i}", [128, 8, 512], BF16) for i in range(2)]
                qiz = [SB(st, f"qiz{i}", [128, 8, 128], BF16) for i in range(2)]
                wt = [SB(st, f"wt{i}", [128, 8], F32) for i in range(2)]
                sgn = [SB(st, f"sgn{i}", [128, 8], F32) for i in range(2)]
                absw = [SB(st, f"absw{i}", [128, 8], F32) for i in range(2)]
                dg = [SB(st, f"dg{i}", [128, 8, 128], BF16) for i in range(2)]
                sc = [SB(st, f"sc{i}", [128, S], F32) for i in range(3)]
                rl = [SB(st, f"rl{i}", [128, 512], BF16) for i in range(2)]
                junk = SB(st, "junk", [128, S], FP8)
                nm = SB(st, "nm", [128, S], BF16)
                nmTg = [SB(st, f"nmTg{i}", [128, NT, 512], FP8) for i in range(2)]
                bs = [SB(st, f"bs{i}", [128, 8], F32) for i in range(2)]
                hta = [SB(st, f"hta{i}", [128, N_BISECT + 1], F32) for i in range(2)]
                htb = [SB(st, f"htb{i}", [128, N_BISECT + 1], F32) for i in range(2)]
                pt = [SB(st, f"pt{i}", [128, 512], BF16) for i in range(3)]
                rec = SB(st, "rec", [128, 512], F32)
                ao = [SB(st, f"ao{i}", [128, 512], BF16) for i in range(2)]
                psD = [PS(st, f"psD{i}", [128, 512], F32) for i in range(2)]
                psS = PS(st, "psS", [128, 512], F32)
                ptr = PS(st, "ptr", [128, 4, 128], BF16)
                psL = [PS(st, f"psL{i}", [128, 512], F32) for i in range(2)]
                psO = PS(st, "psO", [128, 512], F32)
                psR = PS(st, "psR", [128, 512], F32)

                for c in range(4):
                    kb.dma("sp", kT[:, c, :], cm(hk)[:, c, :], writes=["kT"])
                kb.dma("sp", kiT[0:64, :], hki[:, :], writes=["kiT"])
                kb.dma("sp", kiT[64:128, :], hki[:, :], writes=["kiT"])

                for i_ in range(2):
                    kb.op("pool", lambda i_=i_: nc.gpsimd.memset(qz[i_][:], 0.0), writes=[f"qz{i_}"])
                    kb.op("pool", lambda i_=i_: nc.gpsimd.memset(qiz[i_][:], 0.0), writes=[f"qiz{i_}"])
                hv5 = hv.rearrange("(n p) c -> p n c", p=128)
                for g_ in range(4):
                    kb.dma("sp", va[:, 8 * g_:8 * g_ + 8, :, :].rearrange("p n h d -> p n (h d)"), hv5[:, 8 * g_:8 * g_ + 8, :], writes=["va"])
                hw3 = hw.rearrange("(n p) e -> n p e", p=128)
                hma_v = hma.rearrange("h d t -> d h t")
                nrl = [0]
                npt = [0]
                nL = [0]

                def gen_score(i):
                    T0 = i * 128
                    L = T0 + 128
                    b = i % 2
                    b3 = i % 3
                    if i < 2:
                        return
                    kb.dma("sp", qiz[b][0:64, 0:8:2, :], cm(hqi)[0:64, :, T0:T0 + 128], writes=[f"qiz{b}"])
                    kb.dma("sp", qiz[b][64:128, 1:8:2, :], cm(hqi)[64:128, :, T0:T0 + 128], writes=[f"qiz{b}"])
                    kb.dma("sp", wt[b][:], hw3[i], writes=[f"wt{b}"])
                    kb.op("act", lambda: nc.scalar.activation(out=sgn[b][:], in_=wt[b][:], func=AF.Sign),
                          reads=[f"wt{b}"], writes=[f"sgn{b}"])
                    kb.op("dve", lambda: nc.vector.tensor_tensor(out=absw[b][:], in0=wt[b][:], in1=sgn[b][:], op=ALU.mult),
                          reads=[f"wt{b}", f"sgn{b}"], writes=[f"absw{b}"])
                    for h in range(8):
                        kb.op("dve", lambda h=h: nc.vector.tensor_scalar(
                            out=dg[b][:, h, :], in0=identb[:], scalar1=sgn[b][:, h:h + 1], scalar2=None,
                            op0=ALU.mult), reads=["identb", f"sgn{b}"], writes=[(f"dg{b}", h)])
                    yield
                    nsb = (L + 511) // 512
                    units = [(sbk, h) for sbk in range(nsb) for h in range(8)]
                    pend = None

                    def flush(pend):
                        sbk, h, r, wd, c0 = pend
                        kb.op("pe", lambda: nc.tensor.matmul(
                            psS[:, 0:wd], dg[b][:, h, :], rl[r][:, 0:wd], start=(h == 0), stop=(h == 7)),
                            reads=[(f"dg{b}", h), f"rl{r}"], writes=["psS"])
                        if h == 7:
                            kb.op("dve", lambda: nc.vector.tensor_copy(sc[b3][:, c0:c0 + wd], psS[:, 0:wd]),
                                  reads=["psS"], writes=[f"sc{b3}"])

                    for (sbk, h) in units:
                        c0 = sbk * 512
                        wd = min(512, L - c0)
                        hp = (h % 2) * 64
                        d = nrl[0] % 2
                        r = nrl[0] % 2
                        nrl[0] += 1
                        kb.op("pe", lambda: nc.tensor.matmul(
                            psD[d][:, 0:wd], qiz[b][:, h, :], kiT[:, c0:c0 + wd],
                            start=True, stop=True), reads=[f"qiz{b}", "kiT"], writes=[f"psD{d}"])
                        if nrl[0] % 2 == 0:
                            kb.op("act", lambda: nc.scalar.activation(
                                out=rl[r][:, 0:wd], in_=psD[d][:, 0:wd], func=AF.Relu, scale=absw[b][:, h:h + 1]),
                                reads=[f"psD{d}", f"absw{b}"], writes=[f"rl{r}"])
                        else:
                            kb.op("dve", lambda: nc.vector.tensor_scalar(
                                out=rl[r][:, 0:wd], in0=psD[d][:, 0:wd], scalar1=absw[b][:, h:h + 1], scalar2=0.0,
                                op0=ALU.mult, op1=ALU.max), reads=[f"psD{d}", f"absw{b}"], writes=[f"rl{r}"])
                        if pend is not None:
                            flush(pend)
                        pend = (sbk, h, r, wd, c0)
                        yield
                    flush(pend)
                    kb.op("dve", lambda: nc.vector.memset(sc[b3][0:64, L - 64:L], NEG), writes=[f"sc{b3}"])
                    yield

                def gen_bisect(i):
                    T0 = i * 128
                    L = T0 + 128
                    b3 = i % 3
                    g_, r_ = divmod(i, 4)
                    gb = g_ % 2
                    c_ = i % 2
                    bsk, htak, htbk = f"bs{c_}", f"hta{c_}", f"htb{c_}"
                    bs_, hta_, htb_ = bs[c_], hta[c_], htb[c_]
                    if i >= 2:
                        use_act = (i % 2 == 0)
                        sg_ = -1.0 if use_act else 1.0
                        kb.op("dve", lambda: nc.vector.tensor_reduce(out=bs_[:, 0:1], in_=sc[b3][:, 0:L], axis=AX.X, op=ALU.max),
                              reads=[f"sc{b3}"], writes=[bsk])
                        kb.op("dve", lambda: nc.vector.tensor_reduce(out=bs_[:, 1:2], in_=sc[b3][:, 0:L - 64], axis=AX.X, op=ALU.min),
                              reads=[f"sc{b3}", bsk], writes=[bsk])
                        kb.op("dve", lambda: nc.vector.tensor_tensor(out=bs_[:, 2:3], in0=bs_[:, 0:1], in1=bs_[:, 1:2], op=ALU.subtract),
                              reads=[bsk], writes=[bsk])
                        kb.op("dve", lambda: nc.vector.tensor_scalar(out=hta_[:], in0=pow2[:], scalar1=bs_[:, 2:3], scalar2=2.0 * sg_,
                                                                     op0=ALU.mult, op1=ALU.mult), reads=[bsk, "pow2"], writes=[htak])
                        kb.op("dve", lambda: nc.vector.tensor_scalar(out=htb_[:], in0=pow2[:], scalar1=bs_[:, 2:3], scalar2=-sg_,
                                                                     op0=ALU.mult, op1=ALU.mult), reads=[bsk, "pow2"], writes=[htbk])
                        kb.op("dve", lambda: nc.vector.scalar_tensor_tensor(out=bs_[:, 5:6], in0=bs_[:, 1:2], scalar=sg_, in1=htb_[:, 0:1],
                                                                            op0=ALU.mult, op1=ALU.subtract),
                              reads=[bsk, htbk], writes=[bsk])
                        yield
                        for it in range(N_BISECT):
                            if use_act:
                                kb.op("act", lambda: nc.scalar.activation(
                                    out=junk[:, 0:L], in_=sc[b3][:, 0:L], func=AF.Sign, bias=bs_[:, 5:6], scale=1.0,
                                    accum_out=bs_[:, 3:4], saturate=False), reads=[f"sc{b3}", bsk], writes=[bsk])
                                Kp = float(512 - L)
                            else:
                                kb.op("dve", lambda: nc.vector.tensor_scalar(
                                    out=junk[:, 0:L], in0=sc[b3][:, 0:L], scalar1=bs_[:, 5:6], scalar2=None, op0=ALU.is_ge,
                                    op1=ALU.add, accum_out=bs_[:, 3:4], saturate=False), reads=[f"sc{b3}", bsk], writes=[bsk])
                                Kp = 256.0
                            kb.op("pool", lambda it=it, Kp=Kp: nc.gpsimd.tensor_scalar(
                                out=bs_[:, 4:5], in0=bs_[:, 3:4], scalar1=Kp, scalar2=hta_[:, it + 1:it + 2], op0=ALU.is_ge, op1=ALU.mult),
                                reads=[bsk, htak], writes=[bsk])
                            kb.op("pool", lambda it=it: nc.gpsimd.tensor_scalar(
                                out=bs_[:, 5:6], in0=bs_[:, 5:6], scalar1=htb_[:, it + 1:it + 2], scalar2=bs_[:, 4:5], op0=ALU.add, op1=ALU.add),
                                reads=[bsk, htbk], writes=[bsk])
                            yield
                        kb.op("dve", lambda: nc.vector.tensor_scalar(out=bs_[:, 4:5], in0=bs_[:, 5:6], scalar1=sg_, scalar2=None, op0=ALU.mult),
                              reads=[bsk], writes=[bsk])
                        kb.op("dve", lambda: nc.vector.scalar_tensor_tensor(
                            out=bs_[:, 6:7], in0=bs_[:, 2:3], scalar=-(2.0 ** -(N_BISECT + 1)), in1=bs_[:, 4:5], op0=ALU.mult, op1=ALU.add),
                            reads=[bsk], writes=[bsk])
                        kb.op("dve", lambda: nc.vector.tensor_scalar(
                            out=nm[:, 0:L], in0=sc[b3][:, 0:L], scalar1=bs_[:, 6:7], scalar2=NEGM, op0=ALU.is_lt, op1=ALU.mult),
                            reads=[f"sc{b3}", bsk], writes=["nm"])
                    else:
                        kb.op("dve", lambda: nc.vector.memset(nm[:, 0:L], 0.0), writes=["nm"])
                        kb.op("dve", lambda: nc.vector.memset(nm[0:64, L - 64:L], NEGM), writes=["nm"])
                    yield
                    for j0 in range(0, i + 1, 4):
                        cnt = min(4, i + 1 - j0)
                        for jj in range(cnt):
                            j = j0 + jj
                            kb.op("pe", lambda j=j, jj=jj: nc.tensor.transpose(ptr[:, jj, :], nm[:, j * 128:(j + 1) * 128], identb[:]),
                                  reads=["nm", "identb"], writes=["ptr"])
                        kb.op("dve", lambda: nc.vector.tensor_copy(nmTg[gb][:, j0:j0 + cnt, r_ * 128:(r_ + 1) * 128], ptr[:, 0:cnt, :],
                                                                   saturate=False),
                              reads=["ptr"], writes=[(f"nmTg{gb}", r_)])
                        yield
                    if r_ < 3:
                        kb.op("pool", lambda: nc.gpsimd.memset(nmTg[gb][:, i + 1:4 * g_ + 4, r_ * 128:(r_ + 1) * 128], NEGM),
                              writes=[(f"nmTg{gb}", r_)])

                def gen_attend(g):
                    G0 = g * 512
                    gq = g % 2
                    gb = g % 2
                    nj = 4 * g + 4
                    mkeys = [(f"nmTg{gb}", r) for r in range(4)]
                    qkeys = [f"qz{gq}"]
                    kb.dma("sp", qz[gq][0:64, 0:8:2, :], cm(hq)[0:64, :, G0:G0 + 512], writes=[f"qz{gq}"])
                    kb.dma("sp", qz[gq][64:128, 1:8:2, :], cm(hq)[64:128, :, G0:G0 + 512], writes=[f"qz{gq}"])
                    pend = None
                    norm = None

                    def flush(pend):
                        h, j, p_ = pend
                        kb.op("pe", lambda: nc.tensor.matmul(
                            psO[0:65, :], va[:, j, h, :], pt[p_][:, :], start=(j == 0), stop=(j == nj - 1)),
                            reads=["va", f"pt{p_}"], writes=["psO"])

                    def normalise(h):
                        ab = h % 2
                        kb.op("dve", lambda: nc.vector.reciprocal(out=rec[64:65, :], in_=psO[64:65, :]),
                              reads=["psO"], writes=["rec"])
                        kb.op("pe", lambda: nc.tensor.matmul(psR[0:64, :], ones32[64:65, :], rec[64:65, :], start=True, stop=True),
                              reads=["ones32", "rec"], writes=["psR"])
                        kb.op("act", lambda: nc.scalar.copy(rec[0:64, :], psO[0:64, :]), reads=["psO"], writes=["num"])
                        kb.op("dve", lambda: nc.vector.scalar_tensor_tensor(
                            out=ao[ab][0:64, :], in0=rec[0:64, :], scalar=vec[0:64, l, 48 + h:48 + h + 1], in1=psR[0:64, :],
                            op0=ALU.mult, op1=ALU.mult), reads=["num", "psR", "vec"], writes=[f"ao{ab}"])
                        kb.dma("sp", hma[h, :, G0:G0 + 512], ao[ab][0:64, :], reads=[f"ao{ab}"])

                    for h in range(8):
                        hp = (h % 2) * 64
                        for j in range(nj):
                            Lb = nL[0] % 2
                            nL[0] += 1
                            p_ = npt[0] % 3
                            npt[0] += 1
                            nears = [(r, 4 * g + r - j) for r in range(4) if 0 <= 4 * g + r - j <= 1]
                            kb.op("pe", lambda: nc.tensor.matmul(
                                psL[Lb][:, :], identb[:], nmTg[gb][:, j, :], start=True, stop=False),
                                reads=["identb"] + mkeys, writes=[f"psL{Lb}"])
                            kb.op("pe", lambda: nc.tensor.matmul(
                                psL[Lb][:, :], kT[:, h // 2, j * 128:(j + 1) * 128],
                                qz[gq][:, h, :], start=False, stop=(len(nears) == 0)),
                                reads=["kT"] + qkeys, writes=[f"psL{Lb}"])
                            for ni, (r, o) in enumerate(nears):
                                kb.op("pe", lambda r=r, o=o, ni=ni: nc.tensor.matmul(
                                    psL[Lb][:, r * 128:(r + 1) * 128], identb[:], bias_hi[:, h, o, :], start=False,
                                    stop=(ni == len(nears) - 1)),
                                    reads=["identb", "bias_hi"], writes=[f"psL{Lb}"])
                            kb.op("act", lambda: nc.scalar.activation(
                                out=pt[p_][:, :], in_=psL[Lb][:, :], func=AF.Exp,
                                bias=rbb[:, 15 * 8 + h:15 * 8 + h + 1], scale=0.125),
                                reads=[f"psL{Lb}", "rbb"], writes=[f"pt{p_}"])
                            if pend is not None:
                                flush(pend)
                            if norm is not None:
                                normalise(norm)
                                norm = None
                            pend = (h, j, p_)
                            if j == nj - 1:
                                norm = h
                            yield
                    flush(pend)
                    normalise(norm)
                    yield

                def run_step(parts):
                    live = [list(p) for p in parts]
                    done = [p[0] is None for p in live]
                    used = [0] * len(live)
                    active = [i for i, p in enumerate(live) if p[0] is not None]
                    rnd = 0
                    while active:
                        nxt = []
                        only_strided = all(live[i][2] > 1 for i in active)
                        for i in active:
                            g, q, sd = live[i]
                            if sd > 1 and not only_strided and (rnd % sd) != 0:
                                nxt.append(i)
                                continue
                            try:
                                next(g)
                                used[i] += 1
                                if q is None or used[i] < q:
                                    nxt.append(i)
                            except StopIteration:
                                done[i] = True
                        active = nxt
                        rnd += 1
                    return [None if done[i] else live[i][0] for i in range(len(live))]

                run_step([[gen_score(0), None, 1]])
                oldb = None
                att = None
                for k in range(NT + 8):
                    newb = gen_bisect(k) if k < NT else None
                    sc_units = 3 + 8 * ((k + 2) * 128 + 511) // 512 if k + 1 < NT else 0
                    if att is None and k >= 5 and (k - 5) % 4 == 0 and (k - 5) // 4 < NT // 4:
                        g = (k - 5) // 4
                        att = [gen_attend(g), 4, 8 * (4 * g + 4) + 8]
                    quota = 0
                    if att is not None:
                        quota = None if att[1] == 1 else (att[2] + 3) // 4
                    rounds = max(sc_units, quota if quota is not None else (att[2] + 3) // 4 if att is not None else 0)
                    sd = max(1, rounds // 14)
                    parts = [[gen_score(k + 1) if k + 1 < NT else None, None, 1], [oldb, None, sd], [newb, 12, sd]]
                    if att is not None:
                        parts.append([att[0], quota, 1])
                    if all(p[0] is None for p in parts):
                        continue
                    rem = run_step(parts)
                    oldb = rem[2]
                    if att is not None:
                        att[1] -= 1
                        if rem[3] is None or att[1] == 0:
                            att = None
                kb.barrier()

        def phase_conv(l):
            with ExitStack() as st:
                u = SB(st, "u", [128, 4, S + 32], BF16)
                cw = SB(st, "cw", [128, 4, CONV_K], F32)
                dgc = SB(st, "dgc", [128, 4, CONV_K, 128], BF16)
                y32 = SB(st, "y32", [128, 4, 512], F32)
                yb = SB(st, "yb", [128, 4, 512], BF16)
                ysq = SB(st, "ysq", [128, 4, 512], BF16)
                mean = SB(st, "cmean", [128, 512], F32)
                rstd = SB(st, "crstd", [128, 512], F32)
                t1 = SB(st, "ct1", [128, 4, 512], F32)
                so = [SB(st, f"cso{i}", [128, 4, 512], BF16) for i in range(2)]
                pc = [PS(st, f"pc{i}", [128, 512], F32) for i in range(2)]
                pm = PS(st, "pm", [128, 512], F32)
                pq = PS(st, "pq", [128, 512], F32)
                kb.op("pool", lambda: nc.gpsimd.memset(u[:, :, 0:32], 0.0), writes=["u"])
                kb.dma("sp", u[:, :, 32:], cm(hu), writes=["u"])
                kb.dma("sp", cw[:], conv_wT[l].rearrange("(c p) j -> p c j", p=128), writes=["cw"])
                for c in range(4):
                    for j in range(CONV_K):
                        kb.op("dve", lambda c=c, j=j: nc.vector.tensor_scalar(
                            out=dgc[:, c, j, :], in0=identb[:], scalar1=cw[:, c, j:j + 1], scalar2=None, op0=ALU.mult),
                            reads=["identb", "cw"], writes=[("dgc", c)])
                od = onesdiv["od512"]
                for tt in range(TT):
                    for c in range(4):
                        p = c % 2
                        for j in range(CONV_K):
                            off = 2 + tt * 512 + j
                            kb.op("pe", lambda c=c, j=j, p=p, off=off: nc.tensor.matmul(
                                pc[p][:, :], dgc[:, c, j, :], u[:, c, off:off + 512], start=(j == 0), stop=(j == CONV_K - 1)),
                                reads=[("dgc", c), "u"], writes=[f"pc{p}"])
                        kb.op("act", lambda c=c, p=p: nc.scalar.activation(out=y32[:, c, :], in_=pc[p][:, :], func=AF.Identity,
                                                                           bias=vec[:, l, c:c + 1], scale=1.0),
                              reads=[f"pc{p}", "vec"], writes=[("y32", c)])
                        kb.op("act", lambda c=c: nc.scalar.activation(out=ysq[:, c, :], in_=y32[:, c, :], func=AF.Square),
                              reads=[("y32", c)], writes=[("ysq", c)])
                        kb.op("dve", lambda c=c: nc.vector.tensor_copy(yb[:, c, :], y32[:, c, :]), reads=[("y32", c)],
                              writes=[("yb", c)])
                    for c in range(4):
                        kb.op("pe", lambda c=c: nc.tensor.matmul(pm[:, :], od[:], yb[:, c, :], start=(c == 0), stop=(c == 3)),
                              reads=["od512", ("yb", c)], writes=["pm"])
                    for c in range(4):
                        kb.op("pe", lambda c=c: nc.tensor.matmul(pq[:, :], od[:], ysq[:, c, :], start=(c == 0), stop=(c == 3)),
                              reads=["od512", ("ysq", c)], writes=["pq"])
                    ln_tail(mean, rstd, pm, pq, "c")
                    sb_ = tt % 2
                    kb.op("dve", lambda: nc.vector.tensor_tensor(out=t1[:], in0=y32[:], in1=bcast_mid(mean[:], 4), op=ALU.subtract),
                          reads=[("y32", c) for c in range(4)] + ["cmean"], writes=["ct1"])
                    kb.op("dve", lambda: nc.vector.tensor_tensor(out=t1[:], in0=t1[:], in1=bcast_mid(rstd[:], 4), op=ALU.mult),
                          reads=["ct1", "crstd"], writes=["ct1"])
                    for c in range(4):
                        kb.op("act", lambda c=c: nc.scalar.activation(out=t1[:, c, :], in_=t1[:, c, :], func=AF.Silu,
                                                                      bias=vec[:, l, 8 + c:9 + c], scale=vec[:, l, 4 + c:5 + c]),
                              reads=["ct1", "vec"], writes=["ct1"])
                        kb.op("dve", lambda c=c, sb_=sb_: nc.vector.tensor_scalar(
                            out=so[sb_][:, c, :], in0=t1[:, c, :], scalar1=vec[:, l, 12 + c:13 + c], scalar2=None, op0=ALU.mult),
                            reads=["ct1", "vec"], writes=[f"cso{sb_}"])
                    kb.dma("act", cm(hmc)[:, :, tt * 512:(tt + 1) * 512], so[sb_][:], reads=[f"cso{sb_}"])
                kb.barrier()

        def ln_tail(mean, rstd, pm, pq, tag):
            mk, rk = f"{tag}mean", f"{tag}rstd"
            kb.op("act", lambda: nc.scalar.copy(mean[:], pm[:, :]), reads=["pm"], writes=[mk])
            kb.op("dve", lambda: nc.vector.tensor_tensor(out=rstd[:], in0=mean[:], in1=mean[:], op=ALU.mult),
                  reads=[mk], writes=[rk])
            kb.op("dve", lambda: nc.vector.tensor_tensor(out=rstd[:], in0=pq[:, :], in1=rstd[:], op=ALU.subtract),
                  reads=["pq", rk], writes=[rk])
            kb.op("dve", lambda: nc.vector.tensor_scalar(out=rstd[:], in0=rstd[:], scalar1=LN_EPS, scalar2=None, op0=ALU.add),
                  reads=[rk], writes=[rk])
            kb.op("act", lambda: nc.scalar.activation(out=rstd[:], in_=rstd[:], func=AF.Sqrt), reads=[rk], writes=[rk])
            kb.op("dve", lambda: nc.vector.reciprocal(out=rstd[:], in_=rstd[:]), reads=[rk], writes=[rk])

        def phase_outproj(l, router=None):
            with ExitStack() as st:
                load_x, do_tt = ln_setup(st, l, 0, xm, router)
                woA = SB(st, "woA", [128, 4, D], BF16)
                woC = SB(st, "woC", [128, 4, D], BF16)
                ma = [SB(st, f"ma{i}", [128, 4, 512], BF16) for i in range(2)]
                mc = [SB(st, f"mc{i}", [128, 4, 512], BF16) for i in range(2)]
                fo = [SB(st, f"fo{i}", [128, 8, 512], F32) for i in range(2)]
                pp = [PS(st, f"pp{i}", [128, 512], F32) for i in range(3)]
                kb.dma("pool", woA[:], w_out[l][0:512, :].rearrange("(c p) n -> p c n", p=128), writes=["woA"])
                kb.dma("pool", woC[:], w_out[l][512:1024, :].rearrange("(c p) n -> p c n", p=128), writes=["woC"])
                hma_v = hma.rearrange("(c two) d t -> two d c t", two=2)
                n = 0
                for tt in range(TT):
                    b = tt % 2
                    load_x(tt)
                    kb.dma("sp", ma[b][0:64, :, :], hma_v[0][:, :, tt * 512:(tt + 1) * 512], writes=[f"ma{b}"])
                    kb.dma("sp", ma[b][64:128, :, :], hma_v[1][:, :, tt * 512:(tt + 1) * 512], writes=[f"ma{b}"])
                    kb.dma("sp", mc[b][:], cm(hmc)[:, :, tt * 512:(tt + 1) * 512], writes=[f"mc{b}"])
                    for dc in range(8):
                        p = n % 3
                        n += 1
                        for h in range(4):
                            kb.op("pe", lambda h=h, dc=dc, p=p, b=b: nc.tensor.matmul(
                                pp[p][:, :], woA[:, h, dc * 128:(dc + 1) * 128], ma[b][:, h, :], start=(h == 0), stop=False),
                                reads=["woA", f"ma{b}"], writes=[f"pp{p}"])
                        for c in range(4):
                            kb.op("pe", lambda c=c, dc=dc, p=p, b=b: nc.tensor.matmul(
                                pp[p][:, :], woC[:, c, dc * 128:(dc + 1) * 128], mc[b][:, c, :], start=False, stop=(c == 3)),
                                reads=["woC", f"mc{b}"], writes=[f"pp{p}"])
                        if dc % 2 == 0:
                            kb.op("act", lambda dc=dc, p=p, b=b: nc.scalar.copy(fo[b][:, dc, :], pp[p][:, :]),
                                  reads=[f"pp{p}"], writes=[(f"fo{b}", dc)])
                        else:
                            kb.op("dve", lambda dc=dc, p=p, b=b: nc.vector.tensor_copy(fo[b][:, dc, :], pp[p][:, :]),
                                  reads=[f"pp{p}"], writes=[(f"fo{b}", dc)])
                    do_tt(tt, fo[b][:], [(f"fo{b}", dc) for dc in range(8)])
                kb.barrier()

        def ln_setup(st, l, which, dst, router=None):
            gcol = 16 + 16 * which
            x32 = [SB(st, f"lx{i}", [128, 8, 512], F32) for i in range(2)]
            zb = SB(st, "zb", [128, 8, 512], BF16)
            zsq = SB(st, "zsq", [128, 8, 512], BF16)
            mean = SB(st, "lmean", [128, 512], F32)
            rstd = SB(st, "lrstd", [128, 512], F32)
            xo = [SB(st, f"lo{i}", [128, 8, 512], F32) for i in range(2)]
            pm = PS(st, "pm", [128, 512], F32)
            pq = PS(st, "pq", [128, 512], F32)
            od = onesdiv["od1024"]
            if router is not None:
                rw = SB(st, "rw", [128, 8, NE], F32)
                lg = SB(st, "lg", [128, NE], F32)
                mx8 = SB(st, "mx8", [128, 8], F32)
                ex = SB(st, "ex", [128, NE], F32)
                gs = SB(st, "gs", [128, 4], F32)
                gt = SB(st, "gt", [128, NE], F32)
                gT = [SB(st, f"gT{i}", [8, 512], F32) for i in range(2)]
                pr = PS(st, "pr", [128, NE], F32)
                pgt = PS(st, "pgt", [8, 512], F32)
                kb.dma("sp", rw[:], router.rearrange("(c p) e -> p c e", p=128), writes=["rw"])

            def load_x(tt):
                b = tt % 2
                kb.dma("sp", x32[b][:], cm(xm)[:, :, tt * 512:(tt + 1) * 512], writes=[f"lx{b}"])

            def do_tt(tt, ftile, fkeys):
                b = tt % 2
                ts_ = slice(tt * 512, (tt + 1) * 512)
                kb.op("dve", lambda b=b: nc.vector.scalar_tensor_tensor(
                    out=x32[b][:], in0=x32[b][:], scalar=DN_ALPHA, in1=ftile, op0=ALU.mult, op1=ALU.add),
                    reads=[f"lx{b}"] + list(fkeys), writes=[f"lx{b}"])
                kb.op("act", lambda b=b: nc.scalar.copy(zb[:], x32[b][:]), reads=[f"lx{b}"], writes=["zb"])
                kb.op("act", lambda b=b: nc.scalar.activation(out=zsq[:], in_=x32[b][:], func=AF.Square),
                      reads=[f"lx{b}"], writes=["zsq"])
                for c in range(8):
                    kb.op("pe", lambda c=c: nc.tensor.matmul(pm[:, :], od[:], zb[:, c, :], start=(c == 0), stop=(c == 7)),
                          reads=["od1024", "zb"], writes=["pm"])
                for c in range(8):
                    kb.op("pe", lambda c=c: nc.tensor.matmul(pq[:, :], od[:], zsq[:, c, :], start=(c == 0), stop=(c == 7)),
                          reads=["od1024", "zsq"], writes=["pq"])
                ln_tail(mean, rstd, pm, pq, "l")
                kb.op("dve", lambda b=b: nc.vector.tensor_tensor(out=x32[b][:], in0=x32[b][:], in1=bcast_mid(mean[:], 8),
                                                                 op=ALU.subtract),
                      reads=[f"lx{b}", "lmean"], writes=[f"lx{b}"])
                kb.op("dve", lambda b=b: nc.vector.tensor_tensor(out=x32[b][:], in0=x32[b][:], in1=bcast_mid(rstd[:], 8),
                                                                 op=ALU.mult),
                      reads=[f"lx{b}", "lrstd"], writes=[f"lx{b}"])
                for c in range(8):
                    kb.op("act", lambda c=c, b=b: nc.scalar.activation(
                        out=xo[b][:, c, :], in_=x32[b][:, c, :], func=AF.Identity,
                        bias=vec[:, l, gcol + 8 + c:gcol + 9 + c], scale=vec[:, l, gcol + c:gcol + c + 1]),
                        reads=[f"lx{b}", "vec"], writes=[(f"lo{b}", c)])
                kb.dma("act", cm(dst)[:, :, ts_], xo[b][:], reads=[(f"lo{b}", c) for c in range(8)])
                if router is not None:
                    for sub in range(4):
                        for c in range(8):
                            kb.op("pe", lambda c=c, b=b, sub=sub: nc.tensor.matmul(
                                pr[:, :], xo[b][:, c, sub * 128:(sub + 1) * 128], rw[:, c, :], start=(c == 0), stop=(c == 7)),
                                reads=[(f"lo{b}", c), "rw"], writes=["pr"])
                        kb.op("dve", lambda: nc.vector.tensor_copy(lg[:], pr[:, :]), reads=["pr"], writes=["lg"])
                        kb.op("dve", lambda: nc.vector.max(out=mx8[:], in_=lg[:]), reads=["lg"], writes=["mx8"])
                        kb.op("dve", lambda: nc.vector.tensor_scalar(out=gs[:, 0:1], in0=mx8[:, 0:1], scalar1=-1.0, scalar2=None,
                                                                     op0=ALU.mult), reads=["mx8"], writes=["gs"])
                        kb.op("act", lambda: nc.scalar.activation(out=ex[:], in_=lg[:], func=AF.Exp, bias=gs[:, 0:1], scale=1.0),
                              reads=["lg", "gs"], writes=["ex"])
                        kb.op("dve", lambda: nc.vector.scalar_tensor_tensor(
                            out=ex[:], in0=lg[:], scalar=mx8[:, 1:2], in1=ex[:], op0=ALU.is_ge, op1=ALU.mult),
                            reads=["lg", "mx8", "ex"], writes=["ex"])
                        kb.op("dve", lambda: nc.vector.tensor_reduce(out=gs[:, 1:2], in_=ex[:], axis=AX.X, op=ALU.add),
                              reads=["ex", "gs"], writes=["gs"])
                        kb.op("dve", lambda: nc.vector.reciprocal(out=gs[:, 2:3], in_=gs[:, 1:2]), reads=["gs"], writes=["gs"])
                        kb.op("dve", lambda: nc.vector.tensor_scalar(out=gt[:], in0=ex[:], scalar1=gs[:, 2:3], scalar2=None,
                                                                     op0=ALU.mult), reads=["ex", "gs"], writes=["gt"])
                        kb.op("pe", lambda sub=sub: nc.tensor.transpose(pgt[0:8, sub * 128:(sub + 1) * 128], gt[:, :], ident32[:]),
                              reads=["gt", "ident32"], writes=["pgt"])
                    kb.op("dve", lambda b=b: nc.vector.tensor_copy(gT[b][:], pgt[:, :]), reads=["pgt"], writes=[f"gT{b}"])
                    kb.dma("act", hg[:, ts_], gT[b][:], reads=[f"gT{b}"])

            return load_x, do_tt

        def phase_ln(l, which, dst, router=None):
            with ExitStack() as st:
                f32_ = [SB(st, f"lf{i}", [128, 8, 512], F32) for i in range(2)]
                load_x, do_tt = ln_setup(st, l, which, dst, router)
                for tt in range(TT):
                    b = tt % 2
                    load_x(tt)
                    kb.dma("pool", f32_[b][:], cm(fsc)[:, :, tt * 512:(tt + 1) * 512], writes=[f"lf{b}"])
                    do_tt(tt, f32_[b][:], [f"lf{b}"])
                kb.barrier()

        def phase_ffn(l):
            moe = (l % 2 == 1)
            m = l // 2
            HW = D_FFE if moe else D_FF
            nexp = NE if moe else 1
            groups = [(c0, min(512, HW - c0)) for c0 in range(0, HW, 512)]
            HT = 2048
            with ExitStack() as st:
                xb = SB(st, "fxb", [128, 8, HT], BF16)
                yacc = SB(st, "yacc", [128, 8, HT], F32)
                wg = [SB(st, f"fwg{i}", [128, 8, 512], BF16) for i in range(2)]
                wu = [SB(st, f"fwu{i}", [128, 8, 512], BF16) for i in range(2)]
                wd = [SB(st, f"fwd{i}", [128, 4, D], BF16) for i in range(2)]
                sgm = [SB(st, f"fsg{i}", [128, 512], BF16) for i in range(2)]
                hT = [SB(st, f"fhT{i}", [128, 4, 512], BF16) for i in range(2)]
                htmp = [SB(st, f"fht{i}", [128, 512], F32) for i in range(2)]
                pA = [PS(st, f"pA{i}", [128, 512], F32) for i in range(2)]
                pB = [PS(st, f"pB{i}", [128, 512], F32) for i in range(2)]
                pY = [PS(st, f"pY{i}", [128, 512], F32) for i in range(2)]
                if moe:
                    sel8 = SB(st, "sel8", [8, 8, 128], F32)
                    kb.op("pool", lambda: nc.gpsimd.memset(sel8[:], 0.0), writes=["sel8"])
                    kb.op("pool", lambda: nc.gpsimd.affine_select(out=sel8[:], in_=sel8[:], pattern=[[-1, 8], [0, 128]],
                                                                  compare_op=ALU.not_equal, fill=1.0, base=0,
                                                                  channel_multiplier=1), reads=["sel8"], writes=["sel8"])
                    gTs = SB(st, "gTs", [8, HT], F32)
                    gbc = SB(st, "gbc", [128, 4, 512], BF16)
                    pG = PS(st, "pG", [128, 512], F32)
                nab = 0
                ny = 0
                nh = 0
                items = []
                for half in range(S // HT):
                    for e in range(nexp):
                        for gi, (c0, wdt) in enumerate(groups):
                            items.append((half, e, gi, c0, wdt))

                def issue_w(k):
                    half, e, gi, c0, wdt = items[k]
                    ws = k % 2
                    if moe:
                        Wg, Wu, Wd = moe_wg[m, e], moe_wu[m, e], moe_wd[m, e]
                    else:
                        Wg, Wu, Wd = ffn_wg[m], ffn_wu[m], ffn_wd[m]
                    nfc = wdt // 128
                    kb.dma("pool", wg[ws][:, :, 0:wdt], cm(Wg)[:, :, c0:c0 + wdt], writes=[f"fwg{ws}"])
                    kb.dma("pool", wu[ws][:, :, 0:wdt], cm(Wu)[:, :, c0:c0 + wdt], writes=[f"fwu{ws}"])
                    kb.dma("pool", wd[ws][:, 0:nfc, :], Wd[c0:c0 + wdt, :].rearrange("(c p) n -> p c n", p=128),
                           writes=[f"fwd{ws}"])

                issue_w(0)
                for k, (half, e, gi, c0, wdt) in enumerate(items):
                    hs = slice(half * HT, (half + 1) * HT)
                    ws = k % 2
                    nfc = wdt // 128
                    if e == 0 and gi == 0:
                        for c in range(8):
                            kb.dma("pool", xb[:, c, :], cm(xm)[:, c, hs], writes=[("fxb", c)])
                        if moe:
                            kb.dma("sp", gTs[:], hg[:, hs], writes=["gTs"])
                    if k + 1 < len(items):
                        issue_w(k + 1)
                    first = (e == 0 and gi == 0)
                    if moe and gi == 0:
                        for tt in range(4):
                            kb.op("pe", lambda e=e, tt=tt: nc.tensor.matmul(
                                pG[:, :], sel8[0:8, e, :], gTs[0:8, tt * 512:(tt + 1) * 512], start=True, stop=True),
                                reads=["sel8", "gTs"], writes=["pG"])
                            kb.op("act", lambda tt=tt: nc.scalar.copy(gbc[:, tt, :], pG[:, :]), reads=["pG"], writes=[("gbc", tt)])
                    for tt in range(4):
                        tsl = slice(tt * 512, (tt + 1) * 512)
                        hb = nh % 2
                        nh += 1
                        for fc in range(nfc):
                            a = nab % 2
                            nab += 1
                            for c in range(8):
                                kb.op("pe", lambda c=c, a=a, ws=ws, fc=fc, tsl=tsl: nc.tensor.matmul(
                                    pA[a][:, :], wg[ws][:, c, fc * 128:(fc + 1) * 128], xb[:, c, tsl],
                                    start=(c == 0), stop=(c == 7)), reads=[f"fwg{ws}", ("fxb", c)], writes=[f"pA{a}"])
                            for c in range(8):
                                kb.op("pe", lambda c=c, a=a, ws=ws, fc=fc, tsl=tsl: nc.tensor.matmul(
                                    pB[a][:, :], wu[ws][:, c, fc * 128:(fc + 1) * 128], xb[:, c, tsl],
                                    start=(c == 0), stop=(c == 7)), reads=[f"fwu{ws}", ("fxb", c)], writes=[f"pB{a}"])
                            kb.op("act", lambda a=a: nc.scalar.activation(out=sgm[a][:], in_=pA[a][:, :], func=AF.Silu),
                                  reads=[f"pA{a}"], writes=[f"fsg{a}"])
                            if moe:
                                kb.op("dve", lambda a=a: nc.vector.tensor_tensor(out=htmp[a][:], in0=pB[a][:, :], in1=sgm[a][:],
                                                                                 op=ALU.mult),
                                      reads=[f"pB{a}", f"fsg{a}"], writes=[f"fht{a}"])
                                kb.op("dve", lambda a=a, hb=hb, fc=fc, tt=tt: nc.vector.tensor_tensor(
                                    out=hT[hb][:, fc, :], in0=htmp[a][:], in1=gbc[:, tt, :], op=ALU.mult),
                                    reads=[f"fht{a}", ("gbc", tt)], writes=[(f"fhT{hb}", fc)])
                            else:
                                kb.op("dve", lambda a=a, hb=hb, fc=fc: nc.vector.tensor_tensor(
                                    out=hT[hb][:, fc, :], in0=pB[a][:, :], in1=sgm[a][:], op=ALU.mult),
                                    reads=[f"pB{a}", f"fsg{a}"], writes=[(f"fhT{hb}", fc)])
                        for dc in range(8):
                            y = ny % 2
                            ny += 1
                            for fc in range(nfc):
                                kb.op("pe", lambda fc=fc, dc=dc, y=y, ws=ws, hb=hb: nc.tensor.matmul(
                                    pY[y][:, :], wd[ws][:, fc, dc * 128:(dc + 1) * 128], hT[hb][:, fc, :],
                                    start=(fc == 0), stop=(fc == nfc - 1)),
                                    reads=[f"fwd{ws}", (f"fhT{hb}", fc)], writes=[f"pY{y}"])
                            if first:
                                kb.op("dve", lambda dc=dc, y=y, tsl=tsl: nc.vector.tensor_copy(yacc[:, dc, tsl], pY[y][:, :]),
                                      reads=[f"pY{y}"], writes=[("yacc", dc, tsl.start)])
                            else:
                                kb.op("dve", lambda dc=dc, y=y, tsl=tsl: nc.vector.tensor_tensor(
                                    out=yacc[:, dc, tsl], in0=pY[y][:, :], in1=yacc[:, dc, tsl], op=ALU.add),
                                    reads=[f"pY{y}", ("yacc", dc, tsl.start)], writes=[("yacc", dc, tsl.start)])
                    if e == nexp - 1 and gi == len(groups) - 1:
                        kb.dma("act", cm(fsc)[:, :, hs], yacc[:],
                               reads=[("yacc", dc, t0) for dc in range(8) for t0 in range(0, HT, 512)])
                kb.barrier()

        stages = []
        for l in range(n_layers):
            last = (l == n_layers - 1)
            stages += [("proj", lambda l=l: phase_proj(l)), ("attn", lambda l=l: phase_attn(l)),
                       ("conv", lambda l=l: phase_conv(l)),
                       ("outproj+ln1", lambda l=l: phase_outproj(l, router=(moe_r[l // 2] if l % 2 == 1 else None))),
                       ("ffn", lambda l=l: phase_ffn(l)),
                       ("ln2", lambda l=l, last=last: phase_ln(l, 1, outT if last else xm))]
        for idx, (name, fn) in enumerate(stages):
            fn()
            if stop_after is not None and (idx + 1) >= stop_after:
                break
        kb.barrier()
        build_program.n_inst = kb.n_inst
    return nc


def _t5_bucket_np(rel):
    nb = 16
    max_exact = 8
    ret = np.where(rel > 0, nb, 0).astype(np.int32)
    n = np.abs(rel)
    nf = np.maximum(n, 1).astype(np.float32)
    large = max_exact + (np.log(nf / max_exact) / math.log(128 / max_exact) * (nb - max_exact)).astype(np.int32)
    large = np.minimum(large, nb - 1)
    return ret + np.where(n < max_exact, n, large)


def _const_onehot():
    s = np.arange(128)[:, None]
    t = np.arange(128)[None, :]
    oh = np.zeros((128, 32, 2, 128), np.float32)
    for o in range(2):
        rel = (s - 128 * o) - t
        bk = _t5_bucket_np(rel)
        for b in range(32):
            oh[:, b, o, :] = (bk == b)
    return oh


def _pack_vecs(inp):
    f = lambda a, n: np.asarray(a, np.float32).reshape(DEPTH, n, 128).transpose(2, 0, 1)
    v = np.zeros((128, DEPTH, NV), np.float32)
    v[:, :, 0:4] = f(inp["conv_b"], 4)
    v[:, :, 4:8] = f(inp["conv_ln_g"], 4)
    v[:, :, 8:12] = f(inp["conv_ln_b"], 4)
    ms = np.asarray(inp["mix_scale"], np.float32)
    v[:, :, 12:16] = f(ms[:, 512:], 4)
    v[:, :, 16:24] = f(inp["ln1_g"], 8)
    v[:, :, 24:32] = f(inp["ln1_b"], 8)
    v[:, :, 32:40] = f(inp["ln2_g"], 8)
    v[:, :, 40:48] = f(inp["ln2_b"], 8)
    v[0:64, :, 48:56] = ms[:, :512].reshape(DEPTH, 8, 64).transpose(2, 0, 1)
    return v


def make_in_maps(inputs, n_cores=8):
    shared = {
        "w_in": np.ascontiguousarray(inputs["w_in"], np.float32),
        "conv_wT": np.ascontiguousarray(np.asarray(inputs["conv_w"], np.float32).transpose(0, 2, 1)),
        "vecs": _pack_vecs(inputs),
        "rel_bias": np.ascontiguousarray(inputs["rel_bias"], np.float32),
        "ohb": _const_onehot(),
        "w_out": np.ascontiguousarray(inputs["w_out"], np.float32),
        "ffn_w_gate": np.ascontiguousarray(inputs["ffn_w_gate"], np.float32),
        "ffn_w_up": np.ascontiguousarray(inputs["ffn_w_up"], np.float32),
        "ffn_w_down": np.ascontiguousarray(inputs["ffn_w_down"], np.float32),
        "moe_router": np.ascontiguousarray(inputs["moe_router"], np.float32),
        "moe_w_gate": np.ascontiguousarray(inputs["moe_w_gate"], np.float32),
        "moe_w_up": np.ascontiguousarray(inputs["moe_w_up"], np.float32),
        "moe_w_down": np.ascontiguousarray(inputs["moe_w_down"], np.float32),
    }
    x = np.asarray(inputs["x"], np.float32)
    maps = []
    for b in range(n_cores):
        m = dict(shared)
        m["xT"] = np.ascontiguousarray(x[b].T)
        maps.append(m)
    return maps


def kernel(**inputs):
    nc = build_program()
    in_maps = make_in_maps(inputs, 8)
    res = run_bass_kernel_spmd(nc, in_maps, core_ids=list(range(8)))
    out = np.stack([np.ascontiguousarray(r["outT"].T) for r in res.results], axis=0)
    return out.astype(np.float32)
```

```python
import math
from contextlib import ExitStack

import numpy as np
import concourse.bass as bass
import concourse.mybir as mybir
from concourse.bass_utils import run_bass_kernel_spmd

F32 = mybir.dt.float32
BF16 = mybir.dt.bfloat16
FP8 = mybir.dt.float8e5
AF = mybir.ActivationFunctionType
ALU = mybir.AluOpType
AX = mybir.AxisListType

D = 1024
S = 4096
DEPTH = 4
NT = S // 128
TT = S // 512
IN_COLS = 3144
OFF_Q, OFF_K, OFF_V, OFF_QI, OFF_KI, OFF_WI, OFF_A, OFF_G = 0, 512, 1024, 1536, 2048, 2112, 2120, 2632
D_FF = 2816
D_FFE = 3584
NE = 8
CONV_K = 31
DN_ALPHA = (2.0 * DEPTH) ** 0.25
LN_EPS = 1e-5
NEGM = -30000.0
NEG = -1e30
N_BISECT = 16
NV = 56

EPOCH = 30000
DMA_RING = 8


class KB:
    def __init__(self, nc, es):
        self.nc = nc
        self.es = es
        self.eng = {"pe": nc.tensor, "act": nc.scalar, "dve": nc.vector, "pool": nc.gpsimd, "sp": nc.sync}
        self.count = {e: 0 for e in self.eng}
        self.dma_n = {}
        self.waited = {e: {} for e in self.eng}
        self.last_write = {}
        self.readers = {}
        self._semh = {}
        self.n_inst = 0

    def _sem(self, key):
        if key not in self._semh:
            name = "s_" + "_".join(str(k) for k in key)
            self._semh[key] = self.es.enter_context(self.nc.semaphore(name))
        return self._semh[key]

    def _wait(self, eng, token):
        key, val = token
        if self.waited[eng].get(key, 0) >= val:
            return
        self.waited[eng][key] = val
        self.eng[eng].wait_ge(self._sem(key), val)

    def _deps(self, eng, reads, writes):
        toks = []
        for b in reads:
            t = self.last_write.get(b)
            if t is not None:
                toks.append(t)
        for b in writes:
            t = self.last_write.get(b)
            if t is not None:
                toks.append(t)
            toks.extend(self.readers.get(b, {}).values())
        for t in toks:
            if eng == "pe" and t[0][0] == "c" and t[0][1] == "pe":
                continue
            self._wait(eng, t)

    def _record(self, token, reads, writes):
        for b in writes:
            self.last_write[b] = token
            self.readers[b] = {}
        for b in reads:
            self.readers.setdefault(b, {})[token[0]] = token

    def op(self, eng, ins_fn, reads=(), writes=()):
        self._deps(eng, reads, writes)
        ins = ins_fn()
        n = self.count[eng]
        self.count[eng] = n + 1
        key = ("c", eng, n // EPOCH)
        ins.then_inc(self._sem(key), 1)
        self._record((key, n % EPOCH + 1), reads, writes)
        self.n_inst += 1
        return ins

    def dma(self, q, out, in_, reads=(), writes=()):
        n = self.dma_n.get(q, 0)
        self.dma_n[q] = n + 1
        slot = n % DMA_RING
        key = ("d", q, slot)
        rnd = n // DMA_RING
        if rnd > 0:
            self._wait(q, (key, 16 * rnd))
        self._deps(q, reads, writes)
        ins = self.eng[q].dma_start(out=out, in_=in_)
        ins.then_inc(self._sem(key), 16)
        self._record((key, 16 * (rnd + 1)), reads, writes)
        self.n_inst += 1
        return ins

    def barrier(self):
        toks = []
        for e in self.eng:
            n = self.count[e]
            if n > 0:
                toks.append((("c", e, (n - 1) // EPOCH), (n - 1) % EPOCH + 1))
        for q, n in self.dma_n.items():
            for slot in range(min(n, DMA_RING)):
                toks.append((("d", q, slot), 16 * ((n - 1 - slot) // DMA_RING + 1)))
        for e in self.eng:
            for t in toks:
                self._wait(e, t)
        self.last_write = {}
        self.readers = {}


def bcast_mid(a, n):
    return bass.AP(a.tensor, a.offset, [list(a.ap[0]), [0, n], list(a.ap[1])])


def build_program(n_layers=DEPTH, debug=False, stop_after=None):
    nc = bass.Bass("TRN2", target_bir_lowering=False)
    dt_in = lambda name, shape: nc.dram_tensor(name, shape, F32, kind="ExternalInput").ap()
    xT_in = dt_in("xT", [D, S])
    w_in = dt_in("w_in", [DEPTH, D, IN_COLS])
    conv_wT = dt_in("conv_wT", [DEPTH, 512, CONV_K])
    vecs = dt_in("vecs", [128, DEPTH, NV])
    rel_bias = dt_in("rel_bias", [32, 8])
    ohb = dt_in("ohb", [128, 32, 2, 128])
    w_out = dt_in("w_out", [DEPTH, D, D])
    ffn_wg = dt_in("ffn_w_gate", [2, D, D_FF])
    ffn_wu = dt_in("ffn_w_up", [2, D, D_FF])
    ffn_wd = dt_in("ffn_w_down", [2, D_FF, D])
    moe_r = dt_in("moe_router", [2, D, NE])
    moe_wg = dt_in("moe_w_gate", [2, NE, D, D_FFE])
    moe_wu = dt_in("moe_w_up", [2, NE, D, D_FFE])
    moe_wd = dt_in("moe_w_down", [2, NE, D_FFE, D])
    outT = nc.dram_tensor("outT", [D, S], F32, kind="ExternalOutput").ap()

    skind = "ExternalOutput" if debug else "Internal"
    scr = lambda name, shape, dt: nc.dram_tensor(name, shape, dt, kind=skind).ap()
    xm = scr("xm", [D, S], F32)
    fsc = scr("fsc", [D, S], F32)
    hq = scr("hq", [512, S], BF16)
    hk = scr("hk", [512, S], BF16)
    hqi = scr("hqi", [512, S], BF16)
    hki = scr("hki", [64, S], BF16)
    hu = scr("hu", [512, S], BF16)
    hv = scr("hv", [S, 520], BF16)
    hw = scr("hw", [S, 8], F32)
    hma = scr("hma", [8, 64, S], BF16)
    hmc = scr("hmc", [512, S], BF16)
    hg = scr("hg", [8, S], F32)

    def cm(a):
        return a.rearrange("(c p) s -> p c s", p=128)

    with ExitStack() as es:
        kb = KB(nc, es)
        uid = [0]

        def SB(st, name, shape, dt):
            uid[0] += 1
            return st.enter_context(nc.sbuf_tensor(f"{name}_{uid[0]}", shape, dt))

        def PS(st, name, shape, dt):
            uid[0] += 1
            return st.enter_context(nc.psum_tensor(f"{name}_{uid[0]}", shape, dt))

        identb = SB(es, "identb", [128, 128], BF16)
        ident32 = SB(es, "ident32", [128, 128], F32)
        onesdiv = {}
        ones32 = SB(es, "ones32", [128, 64], F32)
        vec = SB(es, "vec", [128, DEPTH, NV], F32)
        rbb = SB(es, "rbb", [128, 256], F32)
        pow2 = SB(es, "pow2", [128, N_BISECT + 1], F32)
        bias_hi = SB(es, "bias_hi", [128, 8, 2, 128], BF16)
        negc = SB(es, "negc", [128, 8], F32)

        kb.op("pool", lambda: nc.gpsimd.memset(ident32[:], 0.0), writes=["ident32"])
        kb.op("pool", lambda: nc.gpsimd.affine_select(out=ident32[:], in_=ident32[:], pattern=[[-1, 128]],
                                                      compare_op=ALU.not_equal, fill=1.0, base=0,
                                                      channel_multiplier=1), reads=["ident32"], writes=["ident32"])
        kb.op("dve", lambda: nc.vector.tensor_copy(identb[:], ident32[:]), reads=["ident32"], writes=["identb"])
        kb.op("dve", lambda: nc.vector.memset(ones32[:], 1.0), writes=["ones32"])
        epsc = SB(es, "epsc", [128, 1], F32)
        kb.op("dve", lambda: nc.vector.memset(epsc[:], LN_EPS), writes=["epsc"])
        for j in range(N_BISECT + 1):
            kb.op("dve", lambda j=j: nc.vector.memset(pow2[:, j:j + 1], 2.0 ** -(j + 1)), writes=["pow2"])
        for nm_, val in (("od512", 1.0 / 512), ("od1024", 1.0 / 1024)):
            t_ = SB(es, nm_, [128, 128], BF16)
            kb.op("dve", lambda t_=t_, val=val: nc.vector.memset(t_[:], val), writes=[nm_])
            onesdiv[nm_] = t_
        kb.dma("sp", vec[:], vecs, writes=["vec"])
        rb_flat = rel_bias.rearrange("b h -> (b h)")
        kb.dma("sp", rbb[:], bass.AP(rb_flat.tensor, rb_flat.offset, [[0, 128], [1, 256]]), writes=["rbb"])
        with ExitStack() as st:
            oh = SB(st, "oh", [128, 32, 2, 128], F32)
            acc = SB(st, "bacc", [128, 8, 2, 128], F32)
            tmp = SB(st, "btmp", [128, 8, 2, 128], F32)
            kb.dma("sp", oh[:], ohb, writes=["oh"])
            kb.op("dve", lambda: nc.vector.tensor_scalar(out=negc[:], in0=rbb[:, 15 * 8:16 * 8], scalar1=-1.0,
                                                         scalar2=None, op0=ALU.mult), reads=["rbb"], writes=["negc"])
            kb.op("dve", lambda: nc.vector.memset(acc[:], 0.0), writes=["bacc"])
            for h in range(8):
                for b in range(32):
                    kb.op("dve", lambda h=h, b=b: nc.vector.scalar_tensor_tensor(
                        out=acc[:, h], in0=oh[:, b], scalar=rbb[:, b * 8 + h:b * 8 + h + 1], in1=acc[:, h],
                        op0=ALU.mult, op1=ALU.add), reads=["oh", "rbb", "bacc"], writes=["bacc"])
                kb.op("dve", lambda h=h: nc.vector.tensor_scalar(
                    out=acc[:, h], in0=acc[:, h], scalar1=negc[:, h:h + 1], scalar2=8.0, op0=ALU.add, op1=ALU.mult),
                    reads=["bacc", "negc"], writes=["bacc"])
            kb.op("dve", lambda: nc.vector.tensor_copy(bias_hi[:], acc[:]), reads=["bacc"], writes=["bias_hi"])
            kb.barrier()

        with ExitStack() as st:
            xt = [SB(st, f"x0_{i}", [128, S], F32) for i in range(2)]
            for c in range(8):
                kb.dma("sp", xt[c % 2][:], cm(xT_in)[:, c, :], writes=[f"x0_{c % 2}"])
                kb.dma("act", cm(xm)[:, c, :], xt[c % 2][:], reads=[f"x0_{c % 2}"])
            kb.barrier()

        def phase_proj(l):
            with ExitStack() as st:
                xb = SB(st, "xb", [128, 8, S], BF16)
                wb = [SB(st, f"wb{i}", [128, 8, 512], BF16) for i in range(2)]
                wkw = SB(st, "wkw", [128, 8, 72], BF16)
                stg = [SB(st, f"stg{i}", [128, S], BF16) for i in range(2)]
                vst = [SB(st, f"vst{i}", [128, 4, 8, 65], BF16) for i in range(2)]
                wst = SB(st, "wst", [128, NT, 8], F32)
                sg = [SB(st, f"sg{i}", [128, 512], F32) for i in range(2)]
                pa = [PS(st, f"pa{i}", [128, 512], F32) for i in range(3)]
                pg = [PS(st, f"pg{i}", [128, 512], F32) for i in range(2)]
                pw = PS(st, "pw", [128, 8], F32)
                for c in range(8):
                    kb.dma("pool", xb[:, c, :], cm(xm)[:, c, :], writes=[("xb", c)])
                xbk = [("xb", c) for c in range(8)]
                wv = cm(w_in[l])
                nslot = [0]

                def loadw(col0, ncol=512):
                    i = nslot[0] % 2
                    nslot[0] += 1
                    kb.dma("pool", wb[i][:, :, 0:ncol], wv[:, :, col0:col0 + ncol], writes=[f"wb{i}"])
                    return i

                nev = [0]

                def evac(dst, src, reads, writes):
                    if nev[0] % 2 == 0:
                        kb.op("act", lambda: nc.scalar.copy(dst, src), reads=reads, writes=writes)
                    else:
                        kb.op("dve", lambda: nc.vector.tensor_copy(dst, src), reads=reads, writes=writes)
                    nev[0] += 1

                nst = [0]
                npa = [0]
                nxt_w = [loadw(OFF_Q)]
                order = [OFF_K, OFF_QI, OFF_A]
                for gi_, (col0, dst) in enumerate(((OFF_Q, hq), (OFF_K, hk), (OFF_QI, hqi))):
                    wi = nxt_w[0]
                    nxt_w[0] = loadw(order[gi_])
                    for blk in range(4):
                        si = nst[0] % 2
                        nst[0] += 1
                        for tt in range(TT):
                            p = npa[0] % 3
                            npa[0] += 1
                            for c in range(8):
                                kb.op("pe", lambda c=c, p=p, wi=wi, blk=blk, tt=tt: nc.tensor.matmul(
                                    pa[p][:, :], wb[wi][:, c, blk * 128:(blk + 1) * 128], xb[:, c, tt * 512:(tt + 1) * 512],
                                    start=(c == 0), stop=(c == 7)), reads=[f"wb{wi}", ("xb", c)], writes=[f"pa{p}"])
                            evac(stg[si][:, tt * 512:(tt + 1) * 512], pa[p][:, :], [f"pa{p}"], [(f"stg{si}", tt)])
                        kb.dma("sp", dst[blk * 128:(blk + 1) * 128, :], stg[si][:],
                               reads=[(f"stg{si}", tt) for tt in range(TT)], writes=[])
                        for tt in range(TT):
                            kb.readers.setdefault((f"stg{si}", tt), {}).update(kb.readers.get((f"stg{si}", 0), {}))
                wa = nxt_w[0]
                wg_ = loadw(OFF_G)
                for blk in range(4):
                    si = nst[0] % 2
                    nst[0] += 1
                    for tt in range(TT):
                        p = npa[0] % 3
                        npa[0] += 1
                        q = tt % 2
                        for c in range(8):
                            kb.op("pe", lambda c=c, p=p, blk=blk, tt=tt: nc.tensor.matmul(
                                pa[p][:, :], wb[wa][:, c, blk * 128:(blk + 1) * 128], xb[:, c, tt * 512:(tt + 1) * 512],
                                start=(c == 0), stop=(c == 7)), reads=[f"wb{wa}", ("xb", c)], writes=[f"pa{p}"])
                        for c in range(8):
                            kb.op("pe", lambda c=c, q=q, blk=blk, tt=tt: nc.tensor.matmul(
                                pg[q][:, :], wb[wg_][:, c, blk * 128:(blk + 1) * 128], xb[:, c, tt * 512:(tt + 1) * 512],
                                start=(c == 0), stop=(c == 7)), reads=[f"wb{wg_}", ("xb", c)], writes=[f"pg{q}"])
                        kb.op("act", lambda q=q: nc.scalar.activation(out=sg[q][:], in_=pg[q][:, :], func=AF.Sigmoid),
                              reads=[f"pg{q}"], writes=[f"sg{q}"])
                        kb.op("dve", lambda q=q, p=p, si=si, tt=tt: nc.vector.tensor_tensor(
                            out=stg[si][:, tt * 512:(tt + 1) * 512], in0=pa[p][:, :], in1=sg[q][:], op=ALU.mult),
                            reads=[f"pa{p}", f"sg{q}"], writes=[(f"stg{si}", tt)])
                    kb.dma("sp", hu[blk * 128:(blk + 1) * 128, :], stg[si][:],
                           reads=[(f"stg{si}", tt) for tt in range(TT)], writes=[])
                    for tt in range(TT):
                        kb.readers.setdefault((f"stg{si}", tt), {}).update(kb.readers.get((f"stg{si}", 0), {}))
                wvv = loadw(OFF_V)
                kb.dma("pool", wkw[:], wv[:, :, OFF_KI:OFF_KI + 72], writes=["wkw"])
                si = nst[0] % 2
                nst[0] += 1
                for tt in range(TT):
                    p = npa[0] % 3
                    npa[0] += 1
                    for c in range(8):
                        kb.op("pe", lambda c=c, p=p, tt=tt: nc.tensor.matmul(
                            pa[p][0:64, :], wkw[:, c, 0:64], xb[:, c, tt * 512:(tt + 1) * 512],
                            start=(c == 0), stop=(c == 7)), reads=["wkw", ("xb", c)], writes=[f"pa{p}"])
                    evac(stg[si][0:64, tt * 512:(tt + 1) * 512], pa[p][0:64, :], [f"pa{p}"], [(f"stg{si}", tt)])
                kb.dma("sp", hki[:, :], stg[si][0:64, :], reads=[(f"stg{si}", tt) for tt in range(TT)])
                for tt in range(TT):
                    kb.readers.setdefault((f"stg{si}", tt), {}).update(kb.readers.get((f"stg{si}", 0), {}))
                hv4 = hv.rearrange("(n p) c -> p n c", p=128)
                for i_ in range(2):
                    kb.op("pool", lambda i_=i_: nc.gpsimd.memset(vst[i_][:], 1.0), writes=[(f"vst{i_}", k_) for k_ in range(4)])
                for i in range(NT):
                    p = npa[0] % 3
                    npa[0] += 1
                    vi = (i // 4) % 2
                    for c in range(8):
                        kb.op("pe", lambda c=c, p=p, i=i: nc.tensor.matmul(
                            pa[p][:, :], xb[:, c, i * 128:(i + 1) * 128], wb[wvv][:, c, :],
                            start=(c == 0), stop=(c == 7)), reads=[f"wb{wvv}", ("xb", c)], writes=[f"pa{p}"])
                    evac(vst[vi][:, i % 4, :, 0:64], pa[p][:, :].rearrange("p (h d) -> p h d", d=64), [f"pa{p}"], [(f"vst{vi}", i % 4)])
                    for c in range(8):
                        kb.op("pe", lambda c=c, i=i: nc.tensor.matmul(
                            pw[:, :], xb[:, c, i * 128:(i + 1) * 128], wkw[:, c, 64:72],
                            start=(c == 0), stop=(c == 7)), reads=["wkw", ("xb", c)], writes=["pw"])
                    kb.op("dve", lambda i=i: nc.vector.tensor_copy(wst[:, i, :], pw[:, :]), reads=["pw"], writes=["wst"])
                    if i % 4 == 3:
                        g = i // 4
                        kb.dma("sp", hv4[:, 4 * g:4 * g + 4, :], vst[vi][:].rearrange("p n h d -> p n (h d)"),
                               reads=[(f"vst{vi}", k) for k in range(4)])
                        for k in range(1, 4):
                            kb.readers.setdefault((f"vst{vi}", k), {}).update(kb.readers.get((f"vst{vi}", 0), {}))
                kb.dma("sp", hw.rearrange("(n p) e -> p n e", p=128), wst[:], reads=["wst"])
                kb.barrier()

        def phase_attn(l):
            with ExitStack() as st:
                kT = SB(st, "kT", [128, 4, S], BF16)
                kiT = SB(st, "kiT", [128, S], BF16)
                va = SB(st, "va", [128, NT, 8, 65], BF16)
                qz = [SB(st, f"qz{i}", [128, 8, 512], BF16) for i in range(2)]
                qiz = [SB(st, f"qiz{i}", [128, 8, 128], BF16) for i in range(2)]
                wt = [SB(st, f"wt{i}", [128, 8], F32) for i in range(2)]
                sgn = [SB(st, f"sgn{i}", [128, 8], F32) for i in range(2)]
                absw = [SB(st, f"absw{i}", [128, 8], F32) for i in range(2)]
                dg = [SB(st, f"dg{i}", [128, 8, 128], BF16) for i in range(2)]
                sc = [SB(st, f"sc{i}", [128, S], F32) for i in range(3)]
                rl = [SB(st, f"rl{i}", [128, 512], BF16) for i in range(2)]
                junk = SB(st, "junk", [128, S], FP8)
                nm = SB(st, "nm", [128, S], BF16)
                nmTg = [SB(st, f"nmTg{i}", [128, NT, 512], FP8) for i in range(2)]
                bs = [SB(st, f"bs{i}", [128, 8], F32) for i in range(2)]
                hta = [SB(st, f"hta{i}", [128, N_BISECT + 1], F32) for i in range(2)]
                htb = [SB(st, f"htb{i}", [128, N_BISECT + 1], F32) for i in range(2)]
                pt = [SB(st, f"pt{i}", [128, 512], BF16) for i in range(3)]
                rec = SB(st, "rec", [128, 512], F32)
                ao = [SB(st, f"ao{i}", [128, 512], BF16) for i in range(2)]
                psD = [PS(st, f"psD{i}", [128, 512], F32) for i in range(2)]
                psS = PS(st, "psS", [128, 512], F32)
                ptr = PS(st, "ptr", [128, 4, 128], BF16)
                psL = [PS(st, f"psL{i}", [128, 512], F32) for i in range(2)]
                psO = PS(st, "psO", [128, 512], F32)
                psR = PS(st, "psR", [128, 512], F32)

                for c in range(4):
                    kb.dma("sp", kT[:, c, :], cm(hk)[:, c, :], writes=["kT"])
                kb.dma("sp", kiT[0:64, :], hki[:, :], writes=["kiT"])
                kb.dma("sp", kiT[64:128, :], hki[:, :], writes=["kiT"])

                for i_ in range(2):
                    kb.op("pool", lambda i_=i_: nc.gpsimd.memset(qz[i_][:], 0.0), writes=[f"qz{i_}"])
                    kb.op("pool", lambda i_=i_: nc.gpsimd.memset(qiz[i_][:], 0.0), writes=[f"qiz{i_}"])
                hv5 = hv.rearrange("(n p) c -> p n c", p=128)
                for g_ in range(4):
                    kb.dma("sp", va[:, 8 * g_:8 * g_ + 8, :, :].rearrange("p n h d -> p n (h d)"), hv5[:, 8 * g_:8 * g_ + 8, :], writes=["va"])
                hw3 = hw.rearrange("(n p) e -> n p e", p=128)
                hma_v = hma.rearrange("h d t -> d h t")
                nrl = [0]
                npt = [0]
                nL = [0]

                def gen_score(i):
                    T0 = i * 128
                    L = T0 + 128
                    b = i % 2
                    b3 = i % 3
                    if i < 2:
                        return
                    kb.dma("sp", qiz[b][0:64, 0:8:2, :], cm(hqi)[0:64, :, T0:T0 + 128], writes=[f"qiz{b}"])
                    kb.dma("sp", qiz[b][64:128, 1:8:2, :], cm(hqi)[64:128, :, T0:T0 + 128], writes=[f"qiz{b}"])
                    kb.dma("sp", wt[b][:], hw3[i], writes=[f"wt{b}"])
                    kb.op("act", lambda: nc.scalar.activation(out=sgn[b][:], in_=wt[b][:], func=AF.Sign),
                          reads=[f"wt{b}"], writes=[f"sgn{b}"])
                    kb.op("dve", lambda: nc.vector.tensor_tensor(out=absw[b][:], in0=wt[b][:], in1=sgn[b][:], op=ALU.mult),
                          reads=[f"wt{b}", f"sgn{b}"], writes=[f"absw{b}"])
                    for h in range(8):
                        kb.op("dve", lambda h=h: nc.vector.tensor_scalar(
                            out=dg[b][:, h, :], in0=identb[:], scalar1=sgn[b][:, h:h + 1], scalar2=None,
                            op0=ALU.mult), reads=["identb", f"sgn{b}"], writes=[(f"dg{b}", h)])
                    yield
                    nsb = (L + 511) // 512
                    units = [(sbk, h) for sbk in range(nsb) for h in range(8)]
                    pend = None

                    def flush(pend):
                        sbk, h, r, wd, c0 = pend
                        kb.op("pe", lambda: nc.tensor.matmul(
                            psS[:, 0:wd], dg[b][:, h, :], rl[r][:, 0:wd], start=(h == 0), stop=(h == 7)),
                            reads=[(f"dg{b}", h), f"rl{r}"], writes=["psS"])
                        if h == 7:
                            kb.op("dve", lambda: nc.vector.tensor_copy(sc[b3][:, c0:c0 + wd], psS[:, 0:wd]),
                                  reads=["psS"], writes=[f"sc{b3}"])

                    for (sbk, h) in units:
                        c0 = sbk * 512
                        wd = min(512, L - c0)
                        hp = (h % 2) * 64
                        d = nrl[0] % 2
                        r = nrl[0] % 2
                        nrl[0] += 1
                        kb.op("pe", lambda: nc.tensor.matmul(
                            psD[d][:, 0:wd], qiz[b][:, h, :], kiT[:, c0:c0 + wd],
                            start=True, stop=True), reads=[f"qiz{b}", "kiT"], writes=[f"psD{d}"])
                        if nrl[0] % 2 == 0:
                            kb.op("act", lambda: nc.scalar.activation(
                                out=rl[r][:, 0:wd], in_=psD[d][:, 0:wd], func=AF.Relu, scale=absw[b][:, h:h + 1]),
                                reads=[f"psD{d}", f"absw{b}"], writes=[f"rl{r}"])
                        else:
                            kb.op("dve", lambda: nc.vector.tensor_scalar(
                                out=rl[r][:, 0:wd], in0=psD[d][:, 0:wd], scalar1=absw[b][:, h:h + 1], scalar2=0.0,
                                op0=ALU.mult, op1=ALU.max), reads=[f"psD{d}", f"absw{b}"], writes=[f"rl{r}"])
                        if pend is not None:
                            flush(pend)
                        pend = (sbk, h, r, wd, c0)
                        yield
                    flush(pend)
                    kb.op("dve", lambda: nc.vector.memset(sc[b3][0:64, L - 64:L], NEG), writes=[f"sc{b3}"])
                    yield

                def gen_bisect(i):
                    T0 = i * 128
                    L = T0 + 128
                    b3 = i % 3
                    g_, r_ = divmod(i, 4)
                    gb = g_ % 2
                    c_ = i % 2
                    bsk, htak, htbk = f"bs{c_}", f"hta{c_}", f"htb{c_}"
                    bs_, hta_, htb_ = bs[c_], hta[c_], htb[c_]
                    if i >= 2:
                        use_act = (i % 2 == 0)
                        sg_ = -1.0 if use_act else 1.0
                        kb.op("dve", lambda: nc.vector.tensor_reduce(out=bs_[:, 0:1], in_=sc[b3][:, 0:L], axis=AX.X, op=ALU.max),
                              reads=[f"sc{b3}"], writes=[bsk])
                        kb.op("dve", lambda: nc.vector.tensor_reduce(out=bs_[:, 1:2], in_=sc[b3][:, 0:L - 64], axis=AX.X, op=ALU.min),
                              reads=[f"sc{b3}", bsk], writes=[bsk])
                        kb.op("dve", lambda: nc.vector.tensor_tensor(out=bs_[:, 2:3], in0=bs_[:, 0:1], in1=bs_[:, 1:2], op=ALU.subtract),
                              reads=[bsk], writes=[bsk])
                        kb.op("dve", lambda: nc.vector.tensor_scalar(out=hta_[:], in0=pow2[:], scalar1=bs_[:, 2:3], scalar2=2.0 * sg_,
                                                                     op0=ALU.mult, op1=ALU.mult), reads=[bsk, "pow2"], writes=[htak])
                        kb.op("dve", lambda: nc.vector.tensor_scalar(out=htb_[:], in0=pow2[:], scalar1=bs_[:, 2:3], scalar2=-sg_,
                                                                     op0=ALU.mult, op1=ALU.mult), reads=[bsk, "pow2"], writes=[htbk])
                        kb.op("dve", lambda: nc.vector.scalar_tensor_tensor(out=bs_[:, 5:6], in0=bs_[:, 1:2], scalar=sg_, in1=htb_[:, 0:1],
                                                                            op0=ALU.mult, op1=ALU.subtract),
                              reads=[bsk, htbk], writes=[bsk])
                        yield
                        for it in range(N_BISECT):
                            if use_act:
                                kb.op("act", lambda: nc.scalar.activation(
                                    out=junk[:, 0:L], in_=sc[b3][:, 0:L], func=AF.Sign, bias=bs_[:, 5:6], scale=1.0,
                                    accum_out=bs_[:, 3:4], saturate=False), reads=[f"sc{b3}", bsk], writes=[bsk])
                                Kp = float(512 - L)
                            else:
                                kb.op("dve", lambda: nc.vector.tensor_scalar(
                                    out=junk[:, 0:L], in0=sc[b3][:, 0:L], scalar1=bs_[:, 5:6], scalar2=None, op0=ALU.is_ge,
                                    op1=ALU.add, accum_out=bs_[:, 3:4], saturate=False), reads=[f"sc{b3}", bsk], writes=[bsk])
                                Kp = 256.0
                            kb.op("pool", lambda it=it, Kp=Kp: nc.gpsimd.tensor_scalar(
                                out=bs_[:, 4:5], in0=bs_[:, 3:4], scalar1=Kp, scalar2=hta_[:, it + 1:it + 2], op0=ALU.is_ge, op1=ALU.mult),
                                reads=[bsk, htak], writes=[bsk])
                            kb.op("pool", lambda it=it: nc.gpsimd.tensor_scalar(
                                out=bs_[:, 5:6], in0=bs_[:, 5:6], scalar1=htb_[:, it + 1:it + 2], scalar2=bs_[:, 4:5], op0=ALU.add, op1=ALU.add),
                                reads=[bsk, htbk], writes=[bsk])
                            yield
                        kb.op("dve", lambda: nc.vector.tensor_scalar(out=bs_[:, 4:5], in0=bs_[:, 5:6], scalar1=sg_, scalar2=None, op0=ALU.mult),
                              reads=[bsk], writes=[bsk])
                        kb.op("dve", lambda: nc.vector.scalar_tensor_tensor(
                            out=bs_[:, 6:7], in0=bs_[:, 2:3], scalar=-(2.0 ** -(N_BISECT + 1)), in1=bs_[:, 4:5], op0=ALU.mult, op1=ALU.add),
                            reads=[bsk], writes=[bsk])
                        kb.op("dve", lambda: nc.vector.tensor_scalar(
                            out=nm[:, 0:L], in0=sc[b3][:, 0:L], scalar1=bs_[:, 6:7], scalar2=NEGM, op0=ALU.is_lt, op1=ALU.mult),
                            reads=[f"sc{b3}", bsk], writes=["nm"])
                    else:
                        kb.op("dve", lambda: nc.vector.memset(nm[:, 0:L], 0.0), writes=["nm"])
                        kb.op("dve", lambda: nc.vector.memset(nm[0:64, L - 64:L], NEGM), writes=["nm"])
                    yield
                    for j0 in range(0, i + 1, 4):
                        cnt = min(4, i + 1 - j0)
                        for jj in range(cnt):
                            j = j0 + jj
                            kb.op("pe", lambda j=j, jj=jj: nc.tensor.transpose(ptr[:, jj, :], nm[:, j * 128:(j + 1) * 128], identb[:]),
                                  reads=["nm", "identb"], writes=["ptr"])
                        kb.op("dve", lambda: nc.vector.tensor_copy(nmTg[gb][:, j0:j0 + cnt, r_ * 128:(r_ + 1) * 128], ptr[:, 0:cnt, :],
                                                                   saturate=False),
                              reads=["ptr"], writes=[(f"nmTg{gb}", r_)])
                        yield
                    if r_ < 3:
                        kb.op("pool", lambda: nc.gpsimd.memset(nmTg[gb][:, i + 1:4 * g_ + 4, r_ * 128:(r_ + 1) * 128], NEGM),
                              writes=[(f"nmTg{gb}", r_)])

                def gen_attend(g):
                    G0 = g * 512
                    gq = g % 2
                    gb = g % 2
                    nj = 4 * g + 4
                    mkeys = [(f"nmTg{gb}", r) for r in range(4)]
                    qkeys = [f"qz{gq}"]
                    kb.dma("sp", qz[gq][0:64, 0:8:2, :], cm(hq)[0:64, :, G0:G0 + 512], writes=[f"qz{gq}"])
                    kb.dma("sp", qz[gq][64:128, 1:8:2, :], cm(hq)[64:128, :, G0:G0 + 512], writes=[f"qz{gq}"])
                    pend = None
                    norm = None

                    def flush(pend):
                        h, j, p_ = pend
                        kb.op("pe", lambda: nc.tensor.matmul(
                            psO[0:65, :], va[:, j, h, :], pt[p_][:, :], start=(j == 0), stop=(j == nj - 1)),
                            reads=["va", f"pt{p_}"], writes=["psO"])

                    def normalise(h):
                        ab = h % 2
                        kb.op("dve", lambda: nc.vector.reciprocal(out=rec[64:65, :], in_=psO[64:65, :]),
                              reads=["psO"], writes=["rec"])
                        kb.op("pe", lambda: nc.tensor.matmul(psR[0:64, :], ones32[64:65, :], rec[64:65, :], start=True, stop=True),
                              reads=["ones32", "rec"], writes=["psR"])
                        kb.op("act", lambda: nc.scalar.copy(rec[0:64, :], psO[0:64, :]), reads=["psO"], writes=["num"])
                        kb.op("dve", lambda: nc.vector.scalar_tensor_tensor(
                            out=ao[ab][0:64, :], in0=rec[0:64, :], scalar=vec[0:64, l, 48 + h:48 + h + 1], in1=psR[0:64, :],
                            op0=ALU.mult, op1=ALU.mult), reads=["num", "psR", "vec"], writes=[f"ao{ab}"])
                        kb.dma("sp", hma[h, :, G0:G0 + 512], ao[ab][0:64, :], reads=[f"ao{ab}"])

                    for h in range(8):
                        hp = (h % 2) * 64
                        for j in range(nj):
                            Lb = nL[0] % 2
                            nL[0] += 1
                            p_ = npt[0] % 3
                            npt[0] += 1
                            nears = [(r, 4 * g + r - j) for r in range(4) if 0 <= 4 * g + r - j <= 1]
                            kb.op("pe", lambda: nc.tensor.matmul(
                                psL[Lb][:, :], identb[:], nmTg[gb][:, j, :], start=True, stop=False),
                                reads=["identb"] + mkeys, writes=[f"psL{Lb}"])
                            kb.op("pe", lambda: nc.tensor.matmul(
                                psL[Lb][:, :], kT[:, h // 2, j * 128:(j + 1) * 128],
                                qz[gq][:, h, :], start=False, stop=(len(nears) == 0)),
                                reads=["kT"] + qkeys, writes=[f"psL{Lb}"])
                            for ni, (r, o) in enumerate(nears):
                                kb.op("pe", lambda r=r, o=o, ni=ni: nc.tensor.matmul(
                                    psL[Lb][:, r * 128:(r + 1) * 128], identb[:], bias_hi[:, h, o, :], start=False,
                                    stop=(ni == len(nears) - 1)),
                                    reads=["identb", "bias_hi"], writes=[f"psL{Lb}"])
                            kb.op("act", lambda: nc.scalar.activation(
                                out=pt[p_][:, :], in_=psL[Lb][:, :], func=AF.Exp,
                                bias=rbb[:, 15 * 8 + h:15 * 8 + h + 1], scale=0.125),
                                reads=[f"psL{Lb}", "rbb"], writes=[f"pt{p_}"])
                            if pend is not None:
                                flush(pend)
                            if norm is not None:
                                normalise(norm)
                                norm = None
                            pend = (h, j, p_)
                            if j == nj - 1:
                                norm = h
                            yield
                    flush(pend)
                    normalise(norm)
                    yield

                def run_step(parts):
                    live = [list(p) for p in parts]
                    done = [p[0] is None for p in live]
                    used = [0] * len(live)
                    active = [i for i, p in enumerate(live) if p[0] is not None]
                    rnd = 0
                    while active:
                        nxt = []
                        only_strided = all(live[i][2] > 1 for i in active)
                        for i in active:
                            g, q, sd = live[i]
                            if sd > 1 and not only_strided and (rnd % sd) != 0:
                                nxt.append(i)
                                continue
                            try:
                                next(g)
                                used[i] += 1
                                if q is None or used[i] < q:
                                    nxt.append(i)
                            except StopIteration:
                                done[i] = True
                        active = nxt
                        rnd += 1
                    return [None if done[i] else live[i][0] for i in range(len(live))]

                run_step([[gen_score(0), None, 1]])
                oldb = None
                att = None
                for k in range(NT + 8):
                    newb = gen_bisect(k) if k < NT else None
                    sc_units = 3 + 8 * ((k + 2) * 128 + 511) // 512 if k + 1 < NT else 0
                    if att is None and k >= 5 and (k - 5) % 4 == 0 and (k - 5) // 4 < NT // 4:
                        g = (k - 5) // 4
                        att = [gen_attend(g), 4, 8 * (4 * g + 4) + 8]
                    quota = 0
                    if att is not None:
                        quota = None if att[1] == 1 else (att[2] + 3) // 4
                    rounds = max(sc_units, quota if quota is not None else (att[2] + 3) // 4 if att is not None else 0)
                    sd = max(1, rounds // 14)
                    parts = [[gen_score(k + 1) if k + 1 < NT else None, None, 1], [oldb, None, sd], [newb, 12, sd]]
                    if att is not None:
                        parts.append([att[0], quota, 1])
                    if all(p[0] is None for p in parts):
                        continue
                    rem = run_step(parts)
                    oldb = rem[2]
                    if att is not None:
                        att[1] -= 1
                        if rem[3] is None or att[1] == 0:
                            att = None
                kb.barrier()

        def phase_conv(l):
            with ExitStack() as st:
                u = SB(st, "u", [128, 4, S + 32], BF16)
                cw = SB(st, "cw", [128, 4, CONV_K], F32)
                dgc = SB(st, "dgc", [128, 4, CONV_K, 128], BF16)
                y32 = SB(st, "y32", [128, 4, 512], F32)
                yb = SB(st, "yb", [128, 4, 512], BF16)
                ysq = SB(st, "ysq", [128, 4, 512], BF16)
                mean = SB(st, "cmean", [128, 512], F32)
                rstd = SB(st, "crstd", [128, 512], F32)
                t1 = SB(st, "ct1", [128, 4, 512], F32)
                so = [SB(st, f"cso{i}", [128, 4, 512], BF16) for i in range(2)]
                pc = [PS(st, f"pc{i}", [128, 512], F32) for i in range(2)]
                pm = PS(st, "pm", [128, 512], F32)
                pq = PS(st, "pq", [128, 512], F32)
                kb.op("pool", lambda: nc.gpsimd.memset(u[:, :, 0:32], 0.0), writes=["u"])
                kb.dma("sp", u[:, :, 32:], cm(hu), writes=["u"])
                kb.dma("sp", cw[:], conv_wT[l].rearrange("(c p) j -> p c j", p=128), writes=["cw"])
                for c in range(4):
                    for j in range(CONV_K):
                        kb.op("dve", lambda c=c, j=j: nc.vector.tensor_scalar(
                            out=dgc[:, c, j, :], in0=identb[:], scalar1=cw[:, c, j:j + 1], scalar2=None, op0=ALU.mult),
                            reads=["identb", "cw"], writes=[("dgc", c)])
                od = onesdiv["od512"]
                for tt in range(TT):
                    for c in range(4):
                        p = c % 2
                        for j in range(CONV_K):
                            off = 2 + tt * 512 + j
                            kb.op("pe", lambda c=c, j=j, p=p, off=off: nc.tensor.matmul(
                                pc[p][:, :], dgc[:, c, j, :], u[:, c, off:off + 512], start=(j == 0), stop=(j == CONV_K - 1)),
                                reads=[("dgc", c), "u"], writes=[f"pc{p}"])
                        kb.op("act", lambda c=c, p=p: nc.scalar.activation(out=y32[:, c, :], in_=pc[p][:, :], func=AF.Identity,
                                                                           bias=vec[:, l, c:c + 1], scale=1.0),
                              reads=[f"pc{p}", "vec"], writes=[("y32", c)])
                        kb.op("act", lambda c=c: nc.scalar.activation(out=ysq[:, c, :], in_=y32[:, c, :], func=AF.Square),
                              reads=[("y32", c)], writes=[("ysq", c)])
                        kb.op("dve", lambda c=c: nc.vector.tensor_copy(yb[:, c, :], y32[:, c, :]), reads=[("y32", c)],
                              writes=[("yb", c)])
                    for c in range(4):
                        kb.op("pe", lambda c=c: nc.tensor.matmul(pm[:, :], od[:], yb[:, c, :], start=(c == 0), stop=(c == 3)),
                              reads=["od512", ("yb", c)], writes=["pm"])
                    for c in range(4):
                        kb.op("pe", lambda c=c: nc.tensor.matmul(pq[:, :], od[:], ysq[:, c, :], start=(c == 0), stop=(c == 3)),
                              reads=["od512", ("ysq", c)], writes=["pq"])
                    ln_tail(mean, rstd, pm, pq, "c")
                    sb_ = tt % 2
                    kb.op("dve", lambda: nc.vector.tensor_tensor(out=t1[:], in0=y32[:], in1=bcast_mid(mean[:], 4), op=ALU.subtract),
                          reads=[("y32", c) for c in range(4)] + ["cmean"], writes=["ct1"])
                    kb.op("dve", lambda: nc.vector.tensor_tensor(out=t1[:], in0=t1[:], in1=bcast_mid(rstd[:], 4), op=ALU.mult),
                          reads=["ct1", "crstd"], writes=["ct1"])
                    for c in range(4):
                        kb.op("act", lambda c=c: nc.scalar.activation(out=t1[:, c, :], in_=t1[:, c, :], func=AF.Silu,
                                                                      bias=vec[:, l, 8 + c:9 + c], scale=vec[:, l, 4 + c:5 + c]),
                              reads=["ct1", "vec"], writes=["ct1"])
                        kb.op("dve", lambda c=c, sb_=sb_: nc.vector.tensor_scalar(
                            out=so[sb_][:, c, :], in0=t1[:, c, :], scalar1=vec[:, l, 12 + c:13 + c], scalar2=None, op0=ALU.mult),
                            reads=["ct1", "vec"], writes=[f"cso{sb_}"])
                    kb.dma("act", cm(hmc)[:, :, tt * 512:(tt + 1) * 512], so[sb_][:], reads=[f"cso{sb_}"])
                kb.barrier()

        def ln_tail(mean, rstd, pm, pq, tag):
            mk, rk = f"{tag}mean", f"{tag}rstd"
            kb.op("act", lambda: nc.scalar.copy(mean[:], pm[:, :]), reads=["pm"], writes=[mk])
            kb.op("dve", lambda: nc.vector.tensor_tensor(out=rstd[:], in0=mean[:], in1=mean[:], op=ALU.mult),
                  reads=[mk], writes=[rk])
            kb.op("dve", lambda: nc.vector.tensor_tensor(out=rstd[:], in0=pq[:, :], in1=rstd[:], op=ALU.subtract),
                  reads=["pq", rk], writes=[rk])
            kb.op("act", lambda: nc.scalar.activation(out=rstd[:], in_=rstd[:], func=AF.Sqrt, bias=epsc[:, 0:1], scale=1.0),
                  reads=[rk, "epsc"], writes=[rk])
            kb.op("dve", lambda: nc.vector.reciprocal(out=rstd[:], in_=rstd[:]), reads=[rk], writes=[rk])

        def phase_outproj(l, router=None):
            with ExitStack() as st:
                load_x, do_tt = ln_setup(st, l, 0, xm, router)
                woA = SB(st, "woA", [128, 4, D], BF16)
                woC = SB(st, "woC", [128, 4, D], BF16)
                ma = [SB(st, f"ma{i}", [128, 4, 512], BF16) for i in range(2)]
                mc = [SB(st, f"mc{i}", [128, 4, 512], BF16) for i in range(2)]
                fo = [SB(st, f"fo{i}", [128, 8, 512], F32) for i in range(2)]
                pp = [PS(st, f"pp{i}", [128, 512], F32) for i in range(3)]
                kb.dma("pool", woA[:], w_out[l][0:512, :].rearrange("(c p) n -> p c n", p=128), writes=["woA"])
                kb.dma("pool", woC[:], w_out[l][512:1024, :].rearrange("(c p) n -> p c n", p=128), writes=["woC"])
                hma_v = hma.rearrange("(c two) d t -> two d c t", two=2)
                n = 0
                for tt in range(TT):
                    b = tt % 2
                    load_x(tt)
                    kb.dma("sp", ma[b][0:64, :, :], hma_v[0][:, :, tt * 512:(tt + 1) * 512], writes=[f"ma{b}"])
                    kb.dma("sp", ma[b][64:128, :, :], hma_v[1][:, :, tt * 512:(tt + 1) * 512], writes=[f"ma{b}"])
                    kb.dma("sp", mc[b][:], cm(hmc)[:, :, tt * 512:(tt + 1) * 512], writes=[f"mc{b}"])
                    for dc in range(8):
                        p = n % 3
                        n += 1
                        for h in range(4):
                            kb.op("pe", lambda h=h, dc=dc, p=p, b=b: nc.tensor.matmul(
                                pp[p][:, :], woA[:, h, dc * 128:(dc + 1) * 128], ma[b][:, h, :], start=(h == 0), stop=False),
                                reads=["woA", f"ma{b}"], writes=[f"pp{p}"])
                        for c in range(4):
                            kb.op("pe", lambda c=c, dc=dc, p=p, b=b: nc.tensor.matmul(
                                pp[p][:, :], woC[:, c, dc * 128:(dc + 1) * 128], mc[b][:, c, :], start=False, stop=(c == 3)),
                                reads=["woC", f"mc{b}"], writes=[f"pp{p}"])
                        if dc % 2 == 0:
                            kb.op("act", lambda dc=dc, p=p, b=b: nc.scalar.copy(fo[b][:, dc, :], pp[p][:, :]),
                                  reads=[f"pp{p}"], writes=[(f"fo{b}", dc)])
                        else:
                            kb.op("dve", lambda dc=dc, p=p, b=b: nc.vector.tensor_copy(fo[b][:, dc, :], pp[p][:, :]),
                                  reads=[f"pp{p}"], writes=[(f"fo{b}", dc)])
                    do_tt(tt, fo[b][:], [(f"fo{b}", dc) for dc in range(8)])
                kb.barrier()

        def ln_setup(st, l, which, dst, router=None):
            gcol = 16 + 16 * which
            x32 = [SB(st, f"lx{i}", [128, 8, 512], F32) for i in range(2)]
            zb = SB(st, "zb", [128, 8, 512], BF16)
            zsq = SB(st, "zsq", [128, 8, 512], BF16)
            mean = SB(st, "lmean", [128, 512], F32)
            rstd = SB(st, "lrstd", [128, 512], F32)
            xo = [SB(st, f"lo{i}", [128, 8, 512], F32) for i in range(2)]
            pm = PS(st, "pm", [128, 512], F32)
            pq = PS(st, "pq", [128, 512], F32)
            od = onesdiv["od1024"]
            if router is not None:
                rw = SB(st, "rw", [128, 8, NE], F32)
                lg = SB(st, "lg", [128, NE], F32)
                mx8 = SB(st, "mx8", [128, 8], F32)
                ex = SB(st, "ex", [128, NE], F32)
                gs = SB(st, "gs", [128, 4], F32)
                gt = SB(st, "gt", [128, NE], F32)
                gT = [SB(st, f"gT{i}", [8, 512], F32) for i in range(2)]
                pr = PS(st, "pr", [128, NE], F32)
                pgt = PS(st, "pgt", [8, 512], F32)
                kb.dma("sp", rw[:], router.rearrange("(c p) e -> p c e", p=128), writes=["rw"])

            def load_x(tt):
                b = tt % 2
                kb.dma("sp", x32[b][:], cm(xm)[:, :, tt * 512:(tt + 1) * 512], writes=[f"lx{b}"])

            def do_tt(tt, ftile, fkeys):
                b = tt % 2
                ts_ = slice(tt * 512, (tt + 1) * 512)
                kb.op("dve", lambda b=b: nc.vector.scalar_tensor_tensor(
                    out=x32[b][:], in0=x32[b][:], scalar=DN_ALPHA, in1=ftile, op0=ALU.mult, op1=ALU.add),
                    reads=[f"lx{b}"] + list(fkeys), writes=[f"lx{b}"])
                kb.op("act", lambda b=b: nc.scalar.copy(zb[:], x32[b][:]), reads=[f"lx{b}"], writes=["zb"])
                kb.op("act", lambda b=b: nc.scalar.activation(out=zsq[:], in_=x32[b][:], func=AF.Square),
                      reads=[f"lx{b}"], writes=["zsq"])
                for c in range(8):
                    kb.op("pe", lambda c=c: nc.tensor.matmul(pm[:, :], od[:], zb[:, c, :], start=(c == 0), stop=(c == 7)),
                          reads=["od1024", "zb"], writes=["pm"])
                for c in range(8):
                    kb.op("pe", lambda c=c: nc.tensor.matmul(pq[:, :], od[:], zsq[:, c, :], start=(c == 0), stop=(c == 7)),
                          reads=["od1024", "zsq"], writes=["pq"])
                ln_tail(mean, rstd, pm, pq, "l")
                kb.op("dve", lambda b=b: nc.vector.tensor_tensor(out=x32[b][:], in0=x32[b][:], in1=bcast_mid(mean[:], 8),
                                                                 op=ALU.subtract),
                      reads=[f"lx{b}", "lmean"], writes=[f"lx{b}"])
                kb.op("dve", lambda b=b: nc.vector.tensor_tensor(out=x32[b][:], in0=x32[b][:], in1=bcast_mid(rstd[:], 8),
                                                                 op=ALU.mult),
                      reads=[f"lx{b}", "lrstd"], writes=[f"lx{b}"])
                for c in range(8):
                    if c % 2 == 0:
                        kb.op("act", lambda c=c, b=b: nc.scalar.activation(
                            out=xo[b][:, c, :], in_=x32[b][:, c, :], func=AF.Identity,
                            bias=vec[:, l, gcol + 8 + c:gcol + 9 + c], scale=vec[:, l, gcol + c:gcol + c + 1]),
                            reads=[f"lx{b}", "vec"], writes=[(f"lo{b}", c)])
                    else:
                        kb.op("dve", lambda c=c, b=b: nc.vector.tensor_scalar(
                            out=xo[b][:, c, :], in0=x32[b][:, c, :], scalar1=vec[:, l, gcol + c:gcol + c + 1],
                            scalar2=vec[:, l, gcol + 8 + c:gcol + 9 + c], op0=ALU.mult, op1=ALU.add),
                            reads=[f"lx{b}", "vec"], writes=[(f"lo{b}", c)])
                kb.dma("act", cm(dst)[:, :, ts_], xo[b][:], reads=[(f"lo{b}", c) for c in range(8)])
                if router is not None:
                    for sub in range(4):
                        for c in range(8):
                            kb.op("pe", lambda c=c, b=b, sub=sub: nc.tensor.matmul(
                                pr[:, :], xo[b][:, c, sub * 128:(sub + 1) * 128], rw[:, c, :], start=(c == 0), stop=(c == 7)),
                                reads=[(f"lo{b}", c), "rw"], writes=["pr"])
                        kb.op("dve", lambda: nc.vector.tensor_copy(lg[:], pr[:, :]), reads=["pr"], writes=["lg"])
                        kb.op("dve", lambda: nc.vector.max(out=mx8[:], in_=lg[:]), reads=["lg"], writes=["mx8"])
                        kb.op("dve", lambda: nc.vector.tensor_scalar(out=gs[:, 0:1], in0=mx8[:, 0:1], scalar1=-1.0, scalar2=None,
                                                                     op0=ALU.mult), reads=["mx8"], writes=["gs"])
                        kb.op("act", lambda: nc.scalar.activation(out=ex[:], in_=lg[:], func=AF.Exp, bias=gs[:, 0:1], scale=1.0),
                              reads=["lg", "gs"], writes=["ex"])
                        kb.op("dve", lambda: nc.vector.scalar_tensor_tensor(
                            out=ex[:], in0=lg[:], scalar=mx8[:, 1:2], in1=ex[:], op0=ALU.is_ge, op1=ALU.mult),
                            reads=["lg", "mx8", "ex"], writes=["ex"])
                        kb.op("dve", lambda: nc.vector.tensor_reduce(out=gs[:, 1:2], in_=ex[:], axis=AX.X, op=ALU.add),
                              reads=["ex", "gs"], writes=["gs"])
                        kb.op("dve", lambda: nc.vector.reciprocal(out=gs[:, 2:3], in_=gs[:, 1:2]), reads=["gs"], writes=["gs"])
                        kb.op("dve", lambda: nc.vector.tensor_scalar(out=gt[:], in0=ex[:], scalar1=gs[:, 2:3], scalar2=None,
                                                                     op0=ALU.mult), reads=["ex", "gs"], writes=["gt"])
                        kb.op("pe", lambda sub=sub: nc.tensor.transpose(pgt[0:8, sub * 128:(sub + 1) * 128], gt[:, :], ident32[:]),
                              reads=["gt", "ident32"], writes=["pgt"])
                    kb.op("dve", lambda b=b: nc.vector.tensor_copy(gT[b][:], pgt[:, :]), reads=["pgt"], writes=[f"gT{b}"])
                    kb.dma("act", hg[:, ts_], gT[b][:], reads=[f"gT{b}"])

            return load_x, do_tt

        def phase_ln(l, which, dst, router=None):
            with ExitStack() as st:
                f32_ = [SB(st, f"lf{i}", [128, 8, 512], F32) for i in range(2)]
                load_x, do_tt = ln_setup(st, l, which, dst, router)
                for tt in range(TT):
                    b = tt % 2
                    load_x(tt)
                    kb.dma("pool", f32_[b][:], cm(fsc)[:, :, tt * 512:(tt + 1) * 512], writes=[f"lf{b}"])
                    do_tt(tt, f32_[b][:], [f"lf{b}"])
                kb.barrier()

        def phase_ffn(l):
            moe = (l % 2 == 1)
            m = l // 2
            HW = D_FFE if moe else D_FF
            nexp = NE if moe else 1
            groups = [(c0, min(512, HW - c0)) for c0 in range(0, HW, 512)]
            HT = 2048
            with ExitStack() as st:
                xb = SB(st, "fxb", [128, 8, HT], BF16)
                yacc = SB(st, "yacc", [128, 8, HT], F32)
                wg = [SB(st, f"fwg{i}", [128, 8, 512], BF16) for i in range(2)]
                wu = [SB(st, f"fwu{i}", [128, 8, 512], BF16) for i in range(2)]
                wd = [SB(st, f"fwd{i}", [128, 4, D], BF16) for i in range(2)]
                sgm = [SB(st, f"fsg{i}", [128, 512], BF16) for i in range(2)]
                hT = [SB(st, f"fhT{i}", [128, 4, 512], BF16) for i in range(2)]
                htmp = [SB(st, f"fht{i}", [128, 512], F32) for i in range(2)]
                pA = [PS(st, f"pA{i}", [128, 512], F32) for i in range(2)]
                pB = [PS(st, f"pB{i}", [128, 512], F32) for i in range(2)]
                pY = [PS(st, f"pY{i}", [128, 512], F32) for i in range(2)]
                if moe:
                    sel8 = SB(st, "sel8", [8, 8, 128], F32)
                    kb.op("pool", lambda: nc.gpsimd.memset(sel8[:], 0.0), writes=["sel8"])
                    kb.op("pool", lambda: nc.gpsimd.affine_select(out=sel8[:], in_=sel8[:], pattern=[[-1, 8], [0, 128]],
                                                                  compare_op=ALU.not_equal, fill=1.0, base=0,
                                                                  channel_multiplier=1), reads=["sel8"], writes=["sel8"])
                    gTs = SB(st, "gTs", [8, HT], F32)
                    gbc = SB(st, "gbc", [128, 4, 512], BF16)
                    pG = PS(st, "pG", [128, 512], F32)
                nab = 0
                ny = 0
                nh = 0
                items = []
                for half in range(S // HT):
                    for e in range(nexp):
                        for gi, (c0, wdt) in enumerate(groups):
                            items.append((half, e, gi, c0, wdt))

                def issue_w(k):
                    half, e, gi, c0, wdt = items[k]
                    ws = k % 2
                    if moe:
                        Wg, Wu, Wd = moe_wg[m, e], moe_wu[m, e], moe_wd[m, e]
                    else:
                        Wg, Wu, Wd = ffn_wg[m], ffn_wu[m], ffn_wd[m]
                    nfc = wdt // 128
                    kb.dma("pool", wg[ws][:, :, 0:wdt], cm(Wg)[:, :, c0:c0 + wdt], writes=[f"fwg{ws}"])
                    kb.dma("pool", wu[ws][:, :, 0:wdt], cm(Wu)[:, :, c0:c0 + wdt], writes=[f"fwu{ws}"])
                    kb.dma("pool", wd[ws][:, 0:nfc, :], Wd[c0:c0 + wdt, :].rearrange("(c p) n -> p c n", p=128),
                           writes=[f"fwd{ws}"])

                issue_w(0)
                for k, (half, e, gi, c0, wdt) in enumerate(items):
                    hs = slice(half * HT, (half + 1) * HT)
                    ws = k % 2
                    nfc = wdt // 128
                    if e == 0 and gi == 0:
                        for c in range(8):
                            kb.dma("pool", xb[:, c, :], cm(xm)[:, c, hs], writes=[("fxb", c)])
                        if moe:
                            kb.dma("sp", gTs[:], hg[:, hs], writes=["gTs"])
                    if k + 1 < len(items):
                        issue_w(k + 1)
                    first = (e == 0 and gi == 0)
                    if moe and gi == 0:
                        for tt in range(4):
                            kb.op("pe", lambda e=e, tt=tt: nc.tensor.matmul(
                                pG[:, :], sel8[0:8, e, :], gTs[0:8, tt * 512:(tt + 1) * 512], start=True, stop=True),
                                reads=["sel8", "gTs"], writes=["pG"])
                            kb.op("act", lambda tt=tt: nc.scalar.copy(gbc[:, tt, :], pG[:, :]), reads=["pG"], writes=[("gbc", tt)])
                    for tt in range(4):
                        tsl = slice(tt * 512, (tt + 1) * 512)
                        hb = nh % 2
                        nh += 1
                        for fc in range(nfc):
                            a = nab % 2
                            nab += 1
                            for c in range(8):
                                kb.op("pe", lambda c=c, a=a, ws=ws, fc=fc, tsl=tsl: nc.tensor.matmul(
                                    pA[a][:, :], wg[ws][:, c, fc * 128:(fc + 1) * 128], xb[:, c, tsl],
                                    start=(c == 0), stop=(c == 7)), reads=[f"fwg{ws}", ("fxb", c)], writes=[f"pA{a}"])
                            for c in range(8):
                                kb.op("pe", lambda c=c, a=a, ws=ws, fc=fc, tsl=tsl: nc.tensor.matmul(
                                    pB[a][:, :], wu[ws][:, c, fc * 128:(fc + 1) * 128], xb[:, c, tsl],
                                    start=(c == 0), stop=(c == 7)), reads=[f"fwu{ws}", ("fxb", c)], writes=[f"pB{a}"])
                            kb.op("act", lambda a=a: nc.scalar.activation(out=sgm[a][:], in_=pA[a][:, :], func=AF.Silu),
                                  reads=[f"pA{a}"], writes=[f"fsg{a}"])
                            if moe:
                                kb.op("dve", lambda a=a: nc.vector.tensor_tensor(out=htmp[a][:], in0=pB[a][:, :], in1=sgm[a][:],
                                                                                 op=ALU.mult),
                                      reads=[f"pB{a}", f"fsg{a}"], writes=[f"fht{a}"])
                                kb.op("dve", lambda a=a, hb=hb, fc=fc, tt=tt: nc.vector.tensor_tensor(
                                    out=hT[hb][:, fc, :], in0=htmp[a][:], in1=gbc[:, tt, :], op=ALU.mult),
                                    reads=[f"fht{a}", ("gbc", tt)], writes=[(f"fhT{hb}", fc)])
                            else:
                                kb.op("dve", lambda a=a, hb=hb, fc=fc: nc.vector.tensor_tensor(
                                    out=hT[hb][:, fc, :], in0=pB[a][:, :], in1=sgm[a][:], op=ALU.mult),
                                    reads=[f"pB{a}", f"fsg{a}"], writes=[(f"fhT{hb}", fc)])
                        for dc in range(8):
                            y = ny % 2
                            ny += 1
                            for fc in range(nfc):
                                kb.op("pe", lambda fc=fc, dc=dc, y=y, ws=ws, hb=hb: nc.tensor.matmul(
                                    pY[y][:, :], wd[ws][:, fc, dc * 128:(dc + 1) * 128], hT[hb][:, fc, :],
                                    start=(fc == 0), stop=(fc == nfc - 1)),
                                    reads=[f"fwd{ws}", (f"fhT{hb}", fc)], writes=[f"pY{y}"])
                            if first:
                                kb.op("dve", lambda dc=dc, y=y, tsl=tsl: nc.vector.tensor_copy(yacc[:, dc, tsl], pY[y][:, :]),
                                      reads=[f"pY{y}"], writes=[("yacc", dc, tsl.start)])
                            else:
                                kb.op("dve", lambda dc=dc, y=y, tsl=tsl: nc.vector.tensor_tensor(
                                    out=yacc[:, dc, tsl], in0=pY[y][:, :], in1=yacc[:, dc, tsl], op=ALU.add),
                                    reads=[f"pY{y}", ("yacc", dc, tsl.start)], writes=[("yacc", dc, tsl.start)])
                    if e == nexp - 1 and gi == len(groups) - 1:
                        kb.dma("act", cm(fsc)[:, :, hs], yacc[:],
                               reads=[("yacc", dc, t0) for dc in range(8) for t0 in range(0, HT, 512)])
                kb.barrier()

        stages = []
        for l in range(n_layers):
            last = (l == n_layers - 1)
            stages += [("proj", lambda l=l: phase_proj(l)), ("attn", lambda l=l: phase_attn(l)),
                       ("conv", lambda l=l: phase_conv(l)),
                       ("outproj+ln1", lambda l=l: phase_outproj(l, router=(moe_r[l // 2] if l % 2 == 1 else None))),
                       ("ffn", lambda l=l: phase_ffn(l)),
                       ("ln2", lambda l=l, last=last: phase_ln(l, 1, outT if last else xm))]
        for idx, (name, fn) in enumerate(stages):
            fn()
            if stop_after is not None and (idx + 1) >= stop_after:
                break
        kb.barrier()
        build_program.n_inst = kb.n_inst
    return nc


def _t5_bucket_np(rel):
    nb = 16
    max_exact = 8
    ret = np.where(rel > 0, nb, 0).astype(np.int32)
    n = np.abs(rel)
    nf = np.maximum(n, 1).astype(np.float32)
    large = max_exact + (np.log(nf / max_exact) / math.log(128 / max_exact) * (nb - max_exact)).astype(np.int32)
    large = np.minimum(large, nb - 1)
    return ret + np.where(n < max_exact, n, large)


def _const_onehot():
    s = np.arange(128)[:, None]
    t = np.arange(128)[None, :]
    oh = np.zeros((128, 32, 2, 128), np.float32)
    for o in range(2):
        rel = (s - 128 * o) - t
        bk = _t5_bucket_np(rel)
        for b in range(32):
            oh[:, b, o, :] = (bk == b)
    return oh


def _pack_vecs(inp):
    f = lambda a, n: np.asarray(a, np.float32).reshape(DEPTH, n, 128).transpose(2, 0, 1)
    v = np.zeros((128, DEPTH, NV), np.float32)
    v[:, :, 0:4] = f(inp["conv_b"], 4)
    v[:, :, 4:8] = f(inp["conv_ln_g"], 4)
    v[:, :, 8:12] = f(inp["conv_ln_b"], 4)
    ms = np.asarray(inp["mix_scale"], np.float32)
    v[:, :, 12:16] = f(ms[:, 512:], 4)
    v[:, :, 16:24] = f(inp["ln1_g"], 8)
    v[:, :, 24:32] = f(inp["ln1_b"], 8)
    v[:, :, 32:40] = f(inp["ln2_g"], 8)
    v[:, :, 40:48] = f(inp["ln2_b"], 8)
    v[0:64, :, 48:56] = ms[:, :512].reshape(DEPTH, 8, 64).transpose(2, 0, 1)
    return v


def make_in_maps(inputs, n_cores=8):
    shared = {
        "w_in": np.ascontiguousarray(inputs["w_in"], np.float32),
        "conv_wT": np.ascontiguousarray(np.asarray(inputs["conv_w"], np.float32).transpose(0, 2, 1)),
        "vecs": _pack_vecs(inputs),
        "rel_bias": np.ascontiguousarray(inputs["rel_bias"], np.float32),
        "ohb": _const_onehot(),
        "w_out": np.ascontiguousarray(inputs["w_out"], np.float32),
        "ffn_w_gate": np.ascontiguousarray(inputs["ffn_w_gate"], np.float32),
        "ffn_w_up": np.ascontiguousarray(inputs["ffn_w_up"], np.float32),
        "ffn_w_down": np.ascontiguousarray(inputs["ffn_w_down"], np.float32),
        "moe_router": np.ascontiguousarray(inputs["moe_router"], np.float32),
        "moe_w_gate": np.ascontiguousarray(inputs["moe_w_gate"], np.float32),
        "moe_w_up": np.ascontiguousarray(inputs["moe_w_up"], np.float32),
        "moe_w_down": np.ascontiguousarray(inputs["moe_w_down"], np.float32),
    }
    x = np.asarray(inputs["x"], np.float32)
    maps = []
    for b in range(n_cores):
        m = dict(shared)
        m["xT"] = np.ascontiguousarray(x[b].T)
        maps.append(m)
    return maps


def kernel(**inputs):
    nc = build_program()
    in_maps = make_in_maps(inputs, 8)
    res = run_bass_kernel_spmd(nc, in_maps, core_ids=list(range(8)))
    out = np.stack([np.ascontiguousarray(r["outT"].T) for r in res.results], axis=0)
    return out.astype(np.float32)
```
